# Optimizing a Trainium2 kernel written in Bass

```python
import math
import jax
import jax.numpy as jnp
from jax import lax
import numpy as np

D_MODEL = 1024
BATCH = 16
SEQ = 2048
DEPTH = 4

MEM_LEN = 256
HG_HEADS = 4
HG_DK = 128
HG_DV = 128
HG_WIDTH = HG_HEADS * HG_DV
GD_HEADS = 4
GD_DK = 128
GD_DV = 128
GD_WIDTH = GD_HEADS * GD_DV
MIX_WIDTH = HG_WIDTH + GD_WIDTH
CONV_K = 4
GD_CONV_WIDTH = 2 * GD_HEADS * GD_DK + GD_WIDTH
CHUNK = 64
IN_SPLITS = (HG_HEADS * HG_DK, HG_HEADS * HG_DK, HG_WIDTH, HG_WIDTH,
             GD_HEADS * GD_DK, GD_HEADS * GD_DK, GD_WIDTH, GD_HEADS, GD_HEADS, GD_WIDTH)
IN_WIDTH = sum(IN_SPLITS)
MEM_HEADS = 4
MEM_DH = D_MODEL // MEM_HEADS
N_GROUPS = 4
EXPERTS_PER_GROUP = 8
N_EXPERTS = N_GROUPS * EXPERTS_PER_GROUP
TOP_K = 2
D_EXPERT = 512
EXPERT_BLOCK = 128
ALPHA = (2.0 * DEPTH) ** 0.25
BETA = (8.0 * DEPTH) ** -0.25
LN_EPS = 1e-5
RMS_EPS = 1e-6
L2_EPS = 1e-6

kernel_name = 'hybrid_hgrn2_gdn_hmoe_trunk'


def layer_norm(x, g, b):
    xf = x.astype(jnp.float32)
    mu = jnp.mean(xf, axis=-1, keepdims=True)
    var = jnp.mean(jnp.square(xf - mu), axis=-1, keepdims=True)
    return ((xf - mu) * lax.rsqrt(var + LN_EPS) * g + b).astype(x.dtype)


def gated_rms_norm(o, w, gate):
    o = o * lax.rsqrt(jnp.mean(o * o, axis=-1, keepdims=True) + RMS_EPS) * w.astype(jnp.float32)
    return o * gate


def l2_normalize(x):
    return x * lax.rsqrt(jnp.sum(x * x, axis=-1, keepdims=True) + L2_EPS)


def causal_depthwise_conv_silu(x, w):
    y = lax.conv_general_dilated(
        x, w[:, None, :], window_strides=(1,), padding=[(CONV_K - 1, 0)],
        dimension_numbers=('NWC', 'WIO', 'NWC'), feature_group_count=x.shape[-1])
    return jax.nn.silu(y)


def to_chunks(x):
    b, t, h, d = x.shape
    return x.reshape(b, t // CHUNK, CHUNK, h, d).transpose(1, 0, 3, 2, 4)


def from_chunks(x):
    n, b, h, c, d = x.shape
    return x.transpose(1, 0, 3, 2, 4).reshape(b, n * c, h, d)


def hgrn2_chunked(q, k, v, log_f):
    qc, kc, vc, gc = to_chunks(q), to_chunks(k), to_chunks(v), to_chunks(log_f)
    causal = jnp.tril(jnp.ones((CHUNK, CHUNK), dtype=bool))
    bsz, h = q.shape[0], q.shape[2]

    def step(S, inp):
        qi, ki, vi, gi = inp
        cum = jnp.cumsum(gi, axis=2)
        diff = cum[:, :, :, None, :] - cum[:, :, None, :, :]
        decay = jnp.exp(jnp.where(causal[:, :, None], diff, -jnp.inf))
        scores = jnp.einsum('bhtd,bhsd,bhtsd->bhts', qi, ki, decay)
        o = (jnp.einsum('bhtd,bhdv->bhtv', qi * jnp.exp(cum), S)
             + jnp.einsum('bhts,bhsv->bhtv', scores, vi))
        cum_last = cum[:, :, -1, :]
        S = (jnp.exp(cum_last)[..., None] * S
             + jnp.einsum('bhsd,bhsv->bhdv', ki * jnp.exp(cum_last[:, :, None, :] - cum), vi))
        return S, o

    S0 = jnp.zeros((bsz, h, q.shape[-1], v.shape[-1]), jnp.float32)
    _, o = lax.scan(step, S0, (qc, kc, vc, gc))
    return from_chunks(o)


def gated_delta_chunked(q, k, v, g, beta):
    qc, kc, vc = to_chunks(q), to_chunks(k), to_chunks(v)
    gc = to_chunks(g[..., None])[..., 0]
    bc = to_chunks(beta[..., None])[..., 0]
    cum = jnp.cumsum(gc, axis=-1)
    diff = cum[..., :, None] - cum[..., None, :]
    incl = jnp.tril(jnp.ones((CHUNK, CHUNK), dtype=bool))
    strict = jnp.tril(jnp.ones((CHUNK, CHUNK), dtype=bool), -1)
    decay = jnp.exp(jnp.where(incl, diff, -jnp.inf))
    kb = kc * bc[..., None]
    a_mat = jnp.where(strict, jnp.einsum('nbhtd,nbhsd->nbhts', kb, kc) * decay, 0.0)
    u = lax.linalg.triangular_solve(a_mat, vc * bc[..., None], left_side=True, lower=True,
                                    unit_diagonal=True)
    w = lax.linalg.triangular_solve(a_mat, kb * jnp.exp(cum)[..., None], left_side=True,
                                    lower=True, unit_diagonal=True)
    qk = jnp.einsum('nbhtd,nbhsd->nbhts', qc, kc) * decay
    bsz, h = q.shape[0], q.shape[2]

    def step(S, inp):
        qi, ki, ui, wi, ci, qki = inp
        v_new = ui - jnp.einsum('bhtd,bhdv->bhtv', wi, S)
        o = (jnp.einsum('bhtd,bhdv->bhtv', qi * jnp.exp(ci)[..., None], S)
             + jnp.einsum('bhts,bhsv->bhtv', qki, v_new))
        c_last = ci[..., -1:]
        S = (jnp.exp(c_last)[..., None] * S
             + jnp.einsum('bhsd,bhsv->bhdv', ki * jnp.exp(c_last - ci)[..., None], v_new))
        return S, o

    S0 = jnp.zeros((bsz, h, q.shape[-1], v.shape[-1]), jnp.float32)
    _, o = lax.scan(step, S0, (qc, kc, u, w, cum, qk))
    return from_chunks(o)


def hybrid_mixer(x, w_in, w_out, hg_lb, hg_norm_w, gd_conv_w, gd_a_log, gd_dt_bias, gd_norm_w):
    b, t, _ = x.shape
    proj = (x @ w_in).astype(jnp.float32)
    offs = np.cumsum(IN_SPLITS)[:-1].tolist()
    hq, hf, hi, hz, gq, gk, gv, ga, gb, gz = jnp.split(proj, offs, axis=-1)

    lb = hg_lb.astype(jnp.float32).reshape(HG_HEADS, HG_DK)
    zf = hf.reshape(b, t, HG_HEADS, HG_DK)
    log_f = jnp.logaddexp(jnp.log(lb), jnp.log1p(-lb) + jax.nn.log_sigmoid(zf))
    k_hg = (1.0 - lb) * jax.nn.sigmoid(-zf)
    o_hg = hgrn2_chunked(hq.reshape(b, t, HG_HEADS, HG_DK), k_hg,
                         hi.reshape(b, t, HG_HEADS, HG_DV), log_f)
    y_hg = gated_rms_norm(o_hg, hg_norm_w, jax.nn.sigmoid(hz.reshape(b, t, HG_HEADS, HG_DV)))

    qkv = causal_depthwise_conv_silu(jnp.concatenate([gq, gk, gv], axis=-1),
                                     gd_conv_w.astype(jnp.float32))
    cq, ck, cv = jnp.split(qkv, [GD_HEADS * GD_DK, 2 * GD_HEADS * GD_DK], axis=-1)
    q_gd = l2_normalize(cq.reshape(b, t, GD_HEADS, GD_DK)) * (GD_DK ** -0.5)
    k_gd = l2_normalize(ck.reshape(b, t, GD_HEADS, GD_DK))
    v_gd = cv.reshape(b, t, GD_HEADS, GD_DV)
    g_gd = -jnp.exp(gd_a_log.astype(jnp.float32)) * jax.nn.softplus(ga + gd_dt_bias.astype(jnp.float32))
    beta = jax.nn.sigmoid(gb)
    o_gd = gated_delta_chunked(q_gd, k_gd, v_gd, g_gd, beta)
    y_gd = gated_rms_norm(o_gd, gd_norm_w, jax.nn.silu(gz.reshape(b, t, GD_HEADS, GD_DV)))

    y = jnp.concatenate([y_hg.reshape(b, t, HG_WIDTH), y_gd.reshape(b, t, GD_WIDTH)], axis=-1)
    return y.astype(x.dtype) @ w_out


def memory_cross_attention(x, mem_n, wq, wk, wv, wo):
    b, t, _ = x.shape
    m = mem_n.shape[1]
    q = (x @ wq).reshape(b, t, MEM_HEADS, MEM_DH)
    k = (mem_n @ wk).reshape(b, m, MEM_HEADS, MEM_DH)
    v = (mem_n @ wv).reshape(b, m, MEM_HEADS, MEM_DH)
    s = jnp.einsum('bthd,bmhd->bhtm', q, k).astype(jnp.float32) * (MEM_DH ** -0.5)
    p = jax.nn.softmax(s, axis=-1).astype(v.dtype)
    o = jnp.einsum('bhtm,bmhd->bthd', p, v).reshape(b, t, D_MODEL)
    return o @ wo


def hierarchical_moe(x, w_group, b_group, w_router, b_router, w_gate, w_up, w_down):
    b, t, d = x.shape
    n_tok = b * t
    xt = x.reshape(n_tok, d)
    tok = jnp.arange(n_tok)
    group_logits = (xt @ w_group).astype(jnp.float32) + b_group.astype(jnp.float32)
    group_p = jax.nn.softmax(group_logits, axis=-1)
    g_sel = jnp.argmax(group_logits, axis=-1)
    g_gate = group_p[tok, g_sel]
    exp_logits = ((xt @ w_router).astype(jnp.float32) + b_router.astype(jnp.float32)
                  ).reshape(n_tok, N_GROUPS, EXPERTS_PER_GROUP)
    local_logits = exp_logits[tok, g_sel]
    top_logits, top_local = lax.top_k(local_logits, TOP_K)
    gate = jax.nn.softmax(top_logits, axis=-1) * g_gate[:, None]
    expert_id = g_sel[:, None] * EXPERTS_PER_GROUP + top_local

    m = n_tok * TOP_K
    flat_e = expert_id.reshape(m)
    flat_tok = jnp.arange(m) // TOP_K
    flat_w = gate.reshape(m)
    order = jnp.argsort(flat_e)
    se, stok, sw = flat_e[order], flat_tok[order], flat_w[order]
    counts = jnp.zeros((N_EXPERTS,), jnp.int32).at[flat_e].add(1)
    starts = jnp.cumsum(counts) - counts
    pcounts = (counts + EXPERT_BLOCK - 1) // EXPERT_BLOCK * EXPERT_BLOCK
    pends = jnp.cumsum(pcounts)
    pstarts = pends - pcounts
    dest = pstarts[se] + jnp.arange(m) - starts[se]
    n_blocks = -(-m // EXPERT_BLOCK) + N_EXPERTS
    buf = jnp.zeros((n_blocks * EXPERT_BLOCK, d), x.dtype).at[dest].set(xt[stok])
    block_expert = jnp.minimum(
        jnp.searchsorted(pends, jnp.arange(n_blocks) * EXPERT_BLOCK, side='right'), N_EXPERTS - 1)

    def expert_block(args):
        xb, e = args
        h = jax.nn.silu(xb @ w_gate[e]) * (xb @ w_up[e])
        return h @ w_down[e]

    ybuf = lax.map(expert_block, (buf.reshape(n_blocks, EXPERT_BLOCK, d), block_expert))
    y = ybuf.reshape(n_blocks * EXPERT_BLOCK, d)[dest] * sw[:, None].astype(x.dtype)
    out = jnp.zeros((n_tok, d), x.dtype).at[stok].add(y)
    return out.reshape(b, t, d)


def setup_inputs(seed: int = 0) -> dict:
    key = jax.random.key(seed)
    ks = jax.random.split(key, 28)
    f32 = jnp.float32
    D = D_MODEL

    def nrm(k, shape, scale):
        return jax.random.normal(k, shape, f32) * scale

    x = nrm(ks[0], (BATCH, SEQ, D), 1.0)
    mem = nrm(ks[1], (BATCH, MEM_LEN, D), 1.0)
    w_in = nrm(ks[2], (DEPTH, D, IN_WIDTH), D ** -0.5)
    hg_lb_logits = nrm(ks[3], (DEPTH, HG_HEADS * HG_DK), 0.5)
    hg_norm_w = 1.0 + nrm(ks[4], (DEPTH, HG_DV), 0.02)
    gd_conv_w = nrm(ks[5], (DEPTH, CONV_K, GD_CONV_WIDTH), CONV_K ** -0.5)
    gd_a_log = jnp.log(jax.random.uniform(ks[6], (DEPTH, GD_HEADS), f32, 1.0, 16.0))
    dt = jnp.exp(jax.random.uniform(ks[7], (DEPTH, GD_HEADS), f32, math.log(1e-3), math.log(1e-1)))
    gd_dt_bias = dt + jnp.log(-jnp.expm1(-dt))
    gd_norm_w = 1.0 + nrm(ks[8], (DEPTH, GD_DV), 0.02)
    w_out = nrm(ks[9], (DEPTH, MIX_WIDTH, D), MIX_WIDTH ** -0.5 * BETA)
    mem_ln_g = 1.0 + nrm(ks[10], (D,), 0.02)
    mem_ln_b = nrm(ks[11], (D,), 0.02)
    w_mq = nrm(ks[12], (DEPTH, D, D), D ** -0.5)
    w_mk = nrm(ks[13], (DEPTH, D, D), D ** -0.5)
    w_mv = nrm(ks[14], (DEPTH, D, D), D ** -0.5)
    w_mo = nrm(ks[15], (DEPTH, D, D), D ** -0.5 * BETA)
    w_group = nrm(ks[16], (DEPTH, D, N_GROUPS), D ** -0.5)
    b_group = nrm(ks[17], (DEPTH, N_GROUPS), 0.01)
    w_router = nrm(ks[18], (DEPTH, D, N_EXPERTS), D ** -0.5)
    b_router = nrm(ks[19], (DEPTH, N_EXPERTS), 0.01)
    w_gate = nrm(ks[20], (DEPTH, N_EXPERTS, D, D_EXPERT), D ** -0.5)
    w_up = nrm(ks[21], (DEPTH, N_EXPERTS, D, D_EXPERT), D ** -0.5)
    w_down = nrm(ks[22], (DEPTH, N_EXPERTS, D_EXPERT, D), D_EXPERT ** -0.5 * BETA)
    ln_g = 1.0 + nrm(ks[23], (DEPTH, 3, D), 0.02)
    ln_b = nrm(ks[24], (DEPTH, 3, D), 0.02)
    return {'x': x, 'mem': mem, 'w_in': w_in, 'hg_lb_logits': hg_lb_logits,
            'hg_norm_w': hg_norm_w, 'gd_conv_w': gd_conv_w, 'gd_a_log': gd_a_log,
            'gd_dt_bias': gd_dt_bias, 'gd_norm_w': gd_norm_w, 'w_out': w_out,
            'mem_ln_g': mem_ln_g, 'mem_ln_b': mem_ln_b, 'w_mq': w_mq, 'w_mk': w_mk,
            'w_mv': w_mv, 'w_mo': w_mo, 'w_group': w_group, 'b_group': b_group,
            'w_router': w_router, 'b_router': b_router, 'w_gate': w_gate, 'w_up': w_up,
            'w_down': w_down, 'ln_g': ln_g, 'ln_b': ln_b}


def reference(x, mem, w_in, hg_lb_logits, hg_norm_w, gd_conv_w, gd_a_log, gd_dt_bias,
              gd_norm_w, w_out, mem_ln_g, mem_ln_b, w_mq, w_mk, w_mv, w_mo, w_group, b_group,
              w_router, b_router, w_gate, w_up, w_down, ln_g, ln_b):
    p_lb = jax.nn.softmax(hg_lb_logits.astype(jnp.float32), axis=0)
    lb_all = jnp.cumsum(p_lb, axis=0)
    lb_all = lb_all - lb_all[0]
    mem_n = layer_norm(mem, mem_ln_g, mem_ln_b)
    for l in range(DEPTH):
        mix = hybrid_mixer(x, w_in[l], w_out[l], lb_all[l], hg_norm_w[l], gd_conv_w[l],
                           gd_a_log[l], gd_dt_bias[l], gd_norm_w[l])
        x = layer_norm(ALPHA * x + mix, ln_g[l, 0], ln_b[l, 0])
        xa = memory_cross_attention(x, mem_n, w_mq[l], w_mk[l], w_mv[l], w_mo[l])
        x = layer_norm(ALPHA * x + xa, ln_g[l, 1], ln_b[l, 1])
        ff = hierarchical_moe(x, w_group[l], b_group[l], w_router[l], b_router[l],
                              w_gate[l], w_up[l], w_down[l])
        x = layer_norm(ALPHA * x + ff, ln_g[l, 2], ln_b[l, 2])
    return x
```

```python
from contextlib import ExitStack
import numpy as np
import concourse.bass as bass
import concourse.mybir as mybir
from concourse.bass_utils import run_bass_kernel_spmd

F32 = mybir.dt.float32
BF16 = mybir.dt.bfloat16
AF = mybir.ActivationFunctionType
ALU = mybir.AluOpType
AX = mybir.AxisListType

P = 128
D = 1024
KD = 8
DEPTH = 4
MEM_LEN = 256
IN_WIDTH = 4104
N_EXP = 32
D_EXP = 512
ALPHA = (2.0 * DEPTH) ** 0.25
LN_EPS = 1e-5
RMS_EPS = 1e-6
L2_EPS = 1e-6
BIG = 1.0e30


class Buf:
    __slots__ = ("t", "w", "r", "sem", "cnt", "name")

    def __init__(self, t, name=""):
        self.t = t
        self.w = None
        self.r = []
        self.sem = None
        self.cnt = 0
        self.name = name

    def __getitem__(self, k):
        return self.t[k]


class Ctx:
    ENG = ("pe", "act", "dve", "pool", "sp")

    def __init__(self, nc):
        self.nc = nc
        self.engs = {"pe": nc.tensor, "act": nc.scalar, "dve": nc.vector,
                     "pool": nc.gpsimd, "sp": nc.sync}
        self.sem = {e: nc.alloc_semaphore("s_" + e) for e in self.ENG}
        self.cnt = {e: 0 for e in self.ENG}
        self.seen = {e: {} for e in self.ENG}
        self.dma_bufs = []
        self.nsem = 5
        self.ninst = 0

    def _wait(self, eng, ev):
        if ev is None:
            return
        sem, val = ev
        if eng == "pe" and sem is self.sem["pe"]:
            return
        if self.seen[eng].get(sem.num, 0) >= val:
            return
        self.engs[eng].wait_ge(sem, val)
        self.seen[eng][sem.num] = val
        self.ninst += 1

    def _deps(self, eng, reads, writes):
        for b in reads:
            self._wait(eng, b.w)
        for b in writes:
            self._wait(eng, b.w)
            for ev in b.r:
                self._wait(eng, ev)

    def _commit(self, ev, reads, writes):
        for b in reads:
            b.r.append(ev)
            if len(b.r) > 40:
                last = {}
                for s, v in b.r:
                    if v > last.get(s.num, (None, 0))[1]:
                        last[s.num] = (s, v)
                b.r = list(last.values())
        for b in writes:
            b.w = ev
            b.r = []

    def op(self, eng, fn, reads=(), writes=()):
        self._deps(eng, reads, writes)
        ins = fn(self.engs[eng])
        self.cnt[eng] += 1
        self.ninst += 1
        ins.then_inc(self.sem[eng], 1)
        self._commit((self.sem[eng], self.cnt[eng]), reads, writes)
        return ins

    def dma(self, eng, out, in_, dst, src, **kw):
        self._deps(eng, [src], [dst])
        if dst.sem is None:
            dst.sem = self.nc.alloc_semaphore("d%d" % self.nsem)
            self.nsem += 1
            self.dma_bufs.append(dst)
        ins = self.engs[eng].dma_start(out=out, in_=in_, **kw)
        dst.cnt += 16
        self.ninst += 1
        ins.then_inc(dst.sem, 16)
        self._commit((dst.sem, dst.cnt), [src], [dst])
        return ins

    def barrier(self):
        for e in self.ENG:
            for e2 in self.ENG:
                if e2 != e and self.cnt[e2] > 0:
                    self._wait(e, (self.sem[e2], self.cnt[e2]))
            for b in self.dma_bufs:
                if b.cnt:
                    self._wait(e, (b.sem, b.cnt))


class K:
    pass


def build(NSEQ=2, S=2048, L=4, phases=("mix", "attn", "moe"), MEM=MEM_LEN):
    nc = bass.Bass("TRN2", target_bir_lowering=False)
    T = NSEQ * S
    NT = T // P
    c = Ctx(nc)
    k = K()
    k.nc, k.c, k.NSEQ, k.S, k.L, k.T, k.NT, k.MEM = nc, c, NSEQ, S, L, T, NT, MEM

    def din(name, shape):
        return Buf(nc.dram_tensor(name, list(shape), F32, kind="ExternalInput"), name)

    k.x = din("x", [T, D])
    k.mem = din("mem", [NSEQ * MEM, D])
    k.w_in = din("w_in", [L, D, IN_WIDTH])
    k.hg_lb_logits = din("hg_lb_logits", [DEPTH, 512])
    k.hg_norm_w = din("hg_norm_w", [L, 128])
    k.gd_conv_w = din("gd_conv_w", [L, 4, 1536])
    k.gd_a_log = din("gd_a_log", [L, 4])
    k.gd_dt_bias = din("gd_dt_bias", [L, 4])
    k.gd_norm_w = din("gd_norm_w", [L, 128])
    k.w_out = din("w_out", [L, D, D])
    k.mem_ln_g = din("mem_ln_g", [D])
    k.mem_ln_b = din("mem_ln_b", [D])
    k.w_mq = din("w_mq", [L, D, D])
    k.w_mk = din("w_mk", [L, D, D])
    k.w_mv = din("w_mv", [L, D, D])
    k.w_mo = din("w_mo", [L, D, D])
    k.w_group = din("w_group", [L, D, 4])
    k.b_group = din("b_group", [L, 4])
    k.w_router = din("w_router", [L, D, N_EXP])
    k.b_router = din("b_router", [L, N_EXP])
    k.w_gate = din("w_gate", [L, N_EXP, D, D_EXP])
    k.w_up = din("w_up", [L, N_EXP, D, D_EXP])
    k.w_down = din("w_down", [L, N_EXP, D_EXP, D])
    k.ln_g = din("ln_g", [L, 3, D])
    k.ln_b = din("ln_b", [L, 3, D])
    k.out = Buf(nc.dram_tensor("out", [T, D], F32, kind="ExternalOutput"), "out")
    k.XA = Buf(nc.dram_tensor("XA", [T, D], F32, kind="Internal"), "XA")
    k.XB = Buf(nc.dram_tensor("XB", [T, D], F32, kind="Internal"), "XB")
    k.XTA = Buf(nc.dram_tensor("XTA", [NT, P, KD, P], BF16, kind="Internal"), "XTA")
    k.XTB = Buf(nc.dram_tensor("XTB", [NT, P, KD, P], BF16, kind="Internal"), "XTB")

    def sb(name, shape, dt=F32):
        return Buf(nc.alloc_sbuf_tensor(name, list(shape), dt), name)

    k.idf = sb("idf", [P, P])
    k.idb = sb("idb", [P, P], BF16)
    k.onesf = sb("onesf", [P, P])
    k.onesb = sb("onesb", [P, P], BF16)
    k.cst = sb("cst", [P, 8])
    k.memT = sb("memT", [P, KD, NSEQ * MEM], BF16)
    c.op("pool", lambda e: e.memset(k.idf[:], 0.0), writes=[k.idf])
    c.op("pool", lambda e: e.affine_select(out=k.idf[:], in_=k.idf[:], pattern=[[-1, P]], base=0,
                                           channel_multiplier=1, compare_op=ALU.not_equal, fill=1.0),
         reads=[k.idf], writes=[k.idf])
    c.op("dve", lambda e: e.tensor_copy(out=k.idb[:], in_=k.idf[:]), reads=[k.idf], writes=[k.idb])
    c.op("pool", lambda e: e.memset(k.onesf[:], 1.0), writes=[k.onesf])
    c.op("pool", lambda e: e.memset(k.onesb[:], 1.0), writes=[k.onesb])
    for j, v in enumerate([LN_EPS, RMS_EPS, L2_EPS, 1.0, 0.0]):
        c.op("pool", lambda e: e.memset(k.cst[:, j:j + 1], v), writes=[k.cst])

    k.ep_xo = [sb("ep_xo%d" % i, [P, D]) for i in range(2)]
    k.ep_y = sb("ep_y", [P, D])
    k.ep_xn = [sb("ep_xn%d" % i, [P, D]) for i in range(2)]
    k.ep_xb = sb("ep_xb", [P, D], BF16)
    k.ep_xT = [sb("ep_xT%d" % i, [P, KD, P], BF16) for i in range(2)]
    k.ep_st = sb("ep_st", [P, 2, 6])
    k.ep_mv = sb("ep_mv", [P, 4])
    k.ep_g = sb("ep_g", [P, D])
    k.ep_b = sb("ep_b", [P, D])
    k.ep_n = 0
    k.pT = Buf(nc.alloc_psum_tensor("pT", [P, KD, P], BF16), "pT")
    k.PA = [Buf(nc.alloc_psum_tensor("PA0", [P, 2, 512], F32), "PA0")]

    prologue(k)
    if "mix" in phases:
        mix_consts(k)
    xin, xtin = k.x, k.XTA
    cur = 0
    xs = [k.XA, k.XB]
    xts = [k.XTB, k.XTA]
    for l in range(L):
        for pi, ph in enumerate(("mix", "attn", "moe")):
            if ph not in phases:
                continue
            last = (l == L - 1) and ph == [p_ for p_ in ("mix", "attn", "moe") if p_ in phases][-1]
            xout = k.out if last else xs[cur]
            xtout = xts[cur]
            c.barrier()
            load_ln(k, l, pi)
            if ph == "mix":
                phase_mix(k, l, xin, xtin, xout, xtout)
            elif ph == "attn":
                phase_attn(k, l, xin, xtin, xout, xtout)
            else:
                phase_moe(k, l, xin, xtin, xout, xtout)
            xin, xtin = xout, xtout
            cur ^= 1
    c.barrier()
    return nc


def load_ln(k, l, idx):
    c = k.c
    c.dma("sp", k.ep_g[:], k.ln_g[l, idx, :].partition_broadcast(P), k.ep_g, k.ln_g)
    c.dma("sp", k.ep_b[:], k.ln_b[l, idx, :].partition_broadcast(P), k.ep_b, k.ln_b)


def ln_rows(k, src_ap, src_buf, dst, g, b, mv, st):
    c = k.c
    for h in range(2):
        c.op("dve", lambda e: e.bn_stats(out=st[:, h, :], in_=src_ap[:, h * 512:(h + 1) * 512]),
             reads=[src_buf], writes=[st])
    c.op("dve", lambda e: e.bn_aggr(out=mv[:, 0:2], in_=st[:].rearrange("p a b -> p (a b)")),
         reads=[st], writes=[mv])
    c.op("act", lambda e: e.activation(out=mv[:, 2:3], in_=mv[:, 1:2], func=AF.Sqrt, bias=k.cst[:, 0:1]),
         reads=[mv, k.cst], writes=[mv])
    c.op("dve", lambda e: e.reciprocal(out=mv[:, 3:4], in_=mv[:, 2:3]), reads=[mv], writes=[mv])
    c.op("dve", lambda e: e.tensor_scalar(out=dst[:], in0=src_ap, scalar1=mv[:, 0:1], scalar2=mv[:, 3:4],
                                          op0=ALU.subtract, op1=ALU.mult),
         reads=[src_buf, mv], writes=[dst])
    c.op("pool", lambda e: e.tensor_mul(out=dst[:], in0=dst[:], in1=g[:]), reads=[dst, g], writes=[dst])
    c.op("pool", lambda e: e.tensor_add(out=dst[:], in0=dst[:], in1=b[:]), reads=[dst, b], writes=[dst])


def to_xT(k, xn, ti, xtout):
    c = k.c
    n = k.ep_n
    xT = k.ep_xT[n % 2]
    c.op("act", lambda e: e.activation(out=k.ep_xb[:], in_=xn[:], func=AF.Copy), reads=[xn], writes=[k.ep_xb])
    for kk in range(KD):
        c.op("pe", lambda e: e.transpose(out=k.pT[:, kk, :], in_=k.ep_xb[:, kk * P:(kk + 1) * P], identity=k.idb[:]),
             reads=[k.ep_xb, k.idb], writes=[k.pT])
    c.op("dve", lambda e: e.tensor_copy(out=xT[:], in_=k.pT[:]), reads=[k.pT], writes=[xT])
    c.dma("sp", xtout[ti], xT[:], xtout, xT)


def epilogue(k, sub_ap, sub_buf, ti, xin, xout, xtout):
    c = k.c
    n = k.ep_n
    xo = k.ep_xo[n % 2]
    xn = k.ep_xn[n % 2]
    c.dma("sp", xo[:], xin[ti * P:(ti + 1) * P, :], xo, xin)
    c.op("dve", lambda e: e.scalar_tensor_tensor(out=k.ep_y[:], in0=xo[:], scalar=ALPHA, in1=sub_ap,
                                                 op0=ALU.mult, op1=ALU.add),
         reads=[xo, sub_buf], writes=[k.ep_y])
    ln_rows(k, k.ep_y[:], k.ep_y, xn, k.ep_g, k.ep_b, k.ep_mv, k.ep_st)
    c.dma("sp", xout[ti * P:(ti + 1) * P, :], xn[:], xout, xn)
    to_xT(k, xn, ti, xtout)
    k.ep_n += 1


def prologue(k):
    c, nc = k.c, k.nc
    for ti in range(k.NT):
        xn = k.ep_xn[k.ep_n % 2]
        c.dma("sp", xn[:], k.x[ti * P:(ti + 1) * P, :], xn, k.x)
        to_xT(k, xn, ti, k.XTA)
        k.ep_n += 1
    c.dma("sp", k.ep_g[:], k.mem_ln_g[:].partition_broadcast(P), k.ep_g, k.mem_ln_g)
    c.dma("sp", k.ep_b[:], k.mem_ln_b[:].partition_broadcast(P), k.ep_b, k.mem_ln_b)
    for mi in range(k.NSEQ * k.MEM // P):
        xo = k.ep_xo[mi % 2]
        xn = k.ep_xn[mi % 2]
        c.dma("sp", xo[:], k.mem[mi * P:(mi + 1) * P, :], xo, k.mem)
        ln_rows(k, xo[:], xo, xn, k.ep_g, k.ep_b, k.ep_mv, k.ep_st)
        c.op("act", lambda e: e.activation(out=k.ep_xb[:], in_=xn[:], func=AF.Copy), reads=[xn], writes=[k.ep_xb])
        for kk in range(KD):
            c.op("pe", lambda e: e.transpose(out=k.pT[:, kk, :], in_=k.ep_xb[:, kk * P:(kk + 1) * P], identity=k.idb[:]),
                 reads=[k.ep_xb, k.idb], writes=[k.pT])
        c.op("dve", lambda e: e.tensor_copy(out=k.memT[:, :, mi * P:(mi + 1) * P], in_=k.pT[:]),
             reads=[k.pT], writes=[k.memT])


def load_xT(k, dst, xtin, t0, n):
    c = k.c
    nt = n // P
    src = xtin[t0 // P:t0 // P + nt].rearrange("i p k t -> p i k t")
    for i in range(nt):
        c.dma("sp", dst[:, :, i * P:(i + 1) * P], xtin[t0 // P + i], dst, xtin)


def load_w_bf(k, dst, dram_ap, src_buf):
    k.c.dma("pool", dst[:], dram_ap.rearrange("(k p) n -> p k n", p=P), dst, src_buf)


def phase_attn(k, l, xin, xtin, xout, xtout):
    c, nc = k.c, k.nc
    MEM, NSEQ, S = k.MEM, k.NSEQ, k.S
    MC = MEM // P
    with ExitStack() as es_:
        def sbt(name, shape, dt=F32):
            return Buf(es_.enter_context(nc.sbuf_tensor("%s_L%d" % (name, l), list(shape), dt)), name)

        def pst(name, shape, dt=F32):
            return Buf(es_.enter_context(nc.psum_tensor("%s_L%d" % (name, l), list(shape), dt)), name)

        wq, wo = sbt("a_wq", [P, KD, D], BF16), sbt("a_wo", [P, KD, D], BF16)
        kT = sbt("a_kT", [P, NSEQ, KD, MEM], BF16)
        v = sbt("a_v", [P, NSEQ * MC, D], BF16)
        xTs = [sbt("a_xT0", [P, KD, 512], BF16), sbt("a_xT1", [P, KD, 512], BF16)]
        qT = sbt("a_qT", [P, KD, 512], BF16)
        es = [sbt("a_e0", [P, MC, 512], BF16), sbt("a_e1", [P, MC, 512], BF16)]
        rs = sbt("a_rs", [P, 512], F32)
        oT = sbt("a_oT", [P, KD, 512], BF16)
        ps = [pst("a_p0", [P, 512]), pst("a_p1", [P, 512]), pst("a_p2", [P, 512])]
        PAs = [k.PA[0], pst("a_PA1", [P, 2, 512])]
        pn = [0]

        def nextp():
            pn[0] += 1
            return ps[pn[0] % 3]

        load_w_bf(k, wq, k.w_mk[l], k.w_mk)
        load_w_bf(k, wo, k.w_mv[l], k.w_mv)
        for s in range(NSEQ):
            for fb in range(KD):
                pp = nextp()
                for kk in range(KD):
                    c.op("pe", lambda e: e.matmul(pp[:, 0:MEM], lhsT=wq[:, kk, fb * P:(fb + 1) * P],
                                                  rhs=k.memT[:, kk, s * MEM:(s + 1) * MEM],
                                                  start=(kk == 0), stop=(kk == KD - 1)),
                         reads=[wq, k.memT], writes=[pp])
                c.op("act", lambda e: e.activation(out=kT[:, s, fb, :], in_=pp[:, 0:MEM], func=AF.Copy, scale=1.0 / 16.0),
                     reads=[pp], writes=[kT])
            for mc in range(MC):
                for half in range(2):
                    pp = nextp()
                    for kk in range(KD):
                        c.op("pe", lambda e: e.matmul(pp[:], lhsT=k.memT[:, kk, s * MEM + mc * P:s * MEM + (mc + 1) * P],
                                                      rhs=wo[:, kk, half * 512:(half + 1) * 512],
                                                      start=(kk == 0), stop=(kk == KD - 1)),
                             reads=[wo, k.memT], writes=[pp])
                    c.op("dve", lambda e: e.tensor_copy(out=v[:, s * MC + mc, half * 512:(half + 1) * 512], in_=pp[:]),
                         reads=[pp], writes=[v])
        load_w_bf(k, wq, k.w_mq[l], k.w_mq)
        load_w_bf(k, wo, k.w_mo[l], k.w_mo)
        nst = k.T // 512
        for st in range(nst):
            s = (st * 512) // S
            xT = xTs[st % 2]
            load_xT(k, xT, xtin, st * 512, 512)
            for fb in range(KD):
                pp = nextp()
                for kk in range(KD):
                    c.op("pe", lambda e: e.matmul(pp[:], lhsT=wq[:, kk, fb * P:(fb + 1) * P], rhs=xT[:, kk, :],
                                                  start=(kk == 0), stop=(kk == KD - 1)),
                         reads=[wq, xT], writes=[pp])
                if fb % 2 == 0:
                    c.op("act", lambda e: e.activation(out=qT[:, fb, :], in_=pp[:], func=AF.Copy), reads=[pp], writes=[qT])
                else:
                    c.op("dve", lambda e: e.tensor_copy(out=qT[:, fb, :], in_=pp[:]), reads=[pp], writes=[qT])
            for h in range(4):
                ee = es[h % 2]
                for mc in range(MC):
                    pp = nextp()
                    for j in range(2):
                        c.op("pe", lambda e: e.matmul(pp[:], lhsT=kT[:, s, 2 * h + j, mc * P:(mc + 1) * P],
                                                      rhs=qT[:, 2 * h + j, :], start=(j == 0), stop=(j == 1)),
                             reads=[kT, qT], writes=[pp])
                    c.op("act", lambda e: e.activation(out=ee[:, mc, :], in_=pp[:], func=AF.Exp), reads=[pp], writes=[ee])
                pp = nextp()
                for mc in range(MC):
                    c.op("pe", lambda e: e.matmul(pp[:], lhsT=k.onesb[:], rhs=ee[:, mc, :], start=(mc == 0), stop=(mc == MC - 1)),
                         reads=[k.onesb, ee], writes=[pp])
                c.op("dve", lambda e: e.reciprocal(out=rs[:], in_=pp[:]), reads=[pp], writes=[rs])
                for j in range(2):
                    pp = nextp()
                    for mc in range(MC):
                        c.op("pe", lambda e: e.matmul(pp[:], lhsT=v[:, s * MC + mc, (2 * h + j) * P:(2 * h + j + 1) * P],
                                                      rhs=ee[:, mc, :], start=(mc == 0), stop=(mc == MC - 1)),
                             reads=[v, ee], writes=[pp])
                    c.op("dve", lambda e: e.tensor_tensor(out=oT[:, 2 * h + j, :], in0=pp[:], in1=rs[:], op=ALU.mult),
                         reads=[pp, rs], writes=[oT])
            for i in range(4):
                pa = PAs[(st * 4 + i) % 2]
                for half in range(2):
                    for fb in range(KD):
                        c.op("pe", lambda e: e.matmul(pa[:, half, :], lhsT=oT[:, fb, i * P:(i + 1) * P],
                                                      rhs=wo[:, fb, half * 512:(half + 1) * 512],
                                                      start=(fb == 0), stop=(fb == KD - 1)),
                             reads=[oT, wo], writes=[pa])
                epilogue(k, pa[:].rearrange("p a b -> p (a b)"), pa, st * 4 + i, xin, xout, xtout)
        c.barrier()


def phase_moe(k, l, xin, xtin, xout, xtout):
    c, nc = k.c, k.nc
    TB = min(1024, k.T)
    NTB = TB // P
    with ExitStack() as es_:
        def sbt(name, shape, dt=F32):
            return Buf(es_.enter_context(nc.sbuf_tensor("%s_L%d" % (name, l), list(shape), dt)), name)

        def pst(name, shape, dt=F32):
            return Buf(es_.enter_context(nc.psum_tensor("%s_L%d" % (name, l), list(shape), dt)), name)

        xT = sbt("m_xT", [P, KD, TB], BF16)
        yacc = sbt("m_yacc", [P, NTB, D], F32)
        wgs = [sbt("m_wg0", [P, KD, D_EXP], BF16), sbt("m_wg1", [P, KD, D_EXP], BF16)]
        wus = [sbt("m_wu0", [P, KD, D_EXP], BF16), sbt("m_wu1", [P, KD, D_EXP], BF16)]
        wds = [sbt("m_wd0", [P, 4, D], BF16), sbt("m_wd1", [P, 4, D], BF16)]
        wr = sbt("m_wr", [P, KD, 36], BF16)
        br, lg, sm = sbt("m_br", [P, 36]), sbt("m_lg", [P, 36]), sbt("m_sm", [P, 16])
        t0, t1, t2 = sbt("m_t0", [P, 32]), sbt("m_t1", [P, 32]), sbt("m_t2", [P, 32])
        gate = sbt("m_gate", [P, NTB, 32])
        sg = sbt("m_sg", [P, 512])
        hTs = [sbt("m_hT0", [P, 4, 512], BF16), sbt("m_hT1", [P, 4, 512], BF16)]
        ps = [pst("m_p0", [P, 512]), pst("m_p1", [P, 512]), pst("m_p2", [P, 512])]
        PAs = [k.PA[0], pst("m_PA1", [P, 2, 512])]
        pn = [0]

        def nextp():
            pn[0] += 1
            return ps[pn[0] % 3]

        c.dma("pool", wr[:, :, 0:4], k.w_group[l].rearrange("(k p) n -> p k n", p=P), wr, k.w_group)
        c.dma("pool", wr[:, :, 4:36], k.w_router[l].rearrange("(k p) n -> p k n", p=P), wr, k.w_router)
        c.dma("sp", br[:, 0:4], k.b_group[l, :].partition_broadcast(P), br, k.b_group)
        c.dma("sp", br[:, 4:36], k.b_router[l, :].partition_broadcast(P), br, k.b_router)
        for blk in range(k.T // TB):
            tb0 = blk * TB
            load_xT(k, xT, xtin, tb0, TB)
            for ti in range(NTB):
                pp = nextp()
                for kk in range(KD):
                    c.op("pe", lambda e: e.matmul(pp[:, 0:36], lhsT=xT[:, kk, ti * P:(ti + 1) * P], rhs=wr[:, kk, :],
                                                  start=(kk == 0), stop=(kk == KD - 1)), reads=[xT, wr], writes=[pp])
                c.op("dve", lambda e: e.tensor_tensor(out=lg[:], in0=pp[:, 0:36], in1=br[:], op=ALU.add),
                     reads=[pp, br], writes=[lg])
                c.op("dve", lambda e: e.reduce_max(out=sm[:, 0:1], in_=lg[:, 0:4], axis=AX.X), reads=[lg], writes=[sm])
                c.op("dve", lambda e: e.tensor_scalar(out=t0[:, 0:4], in0=lg[:, 0:4], scalar1=sm[:, 0:1], scalar2=None,
                                                      op0=ALU.is_equal), reads=[lg, sm], writes=[t0])
                c.op("dve", lambda e: e.tensor_scalar(out=sm[:, 1:2], in0=sm[:, 0:1], scalar1=-1.0, scalar2=None,
                                                      op0=ALU.mult), reads=[sm], writes=[sm])
                c.op("act", lambda e: e.activation(out=t1[:, 0:4], in_=lg[:, 0:4], func=AF.Exp, bias=sm[:, 1:2],
                                                   accum_out=sm[:, 2:3]), reads=[lg, sm], writes=[t1, sm])
                c.op("dve", lambda e: e.reciprocal(out=sm[:, 3:4], in_=sm[:, 2:3]), reads=[sm], writes=[sm])
                c.op("dve", lambda e: e.tensor_scalar(out=t0[:, 4:8], in0=t0[:, 0:4], scalar1=-1.0, scalar2=BIG,
                                                      op0=ALU.add, op1=ALU.mult), reads=[t0], writes=[t0])
                c.op("dve", lambda e: e.tensor_tensor(out=t1[:].rearrange("p (g e) -> p g e", g=4),
                                                      in0=lg[:, 4:36].rearrange("p (g e) -> p g e", g=4),
                                                      in1=t0[:, 4:8].unsqueeze(2).to_broadcast([P, 4, 8]), op=ALU.add),
                     reads=[lg, t0], writes=[t1])
                c.op("dve", lambda e: e.reduce_max(out=sm[:, 4:5], in_=t1[:], axis=AX.X), reads=[t1], writes=[sm])
                c.op("dve", lambda e: e.tensor_scalar(out=t0[:], in0=t1[:], scalar1=sm[:, 4:5], scalar2=None,
                                                      op0=ALU.is_equal), reads=[t1, sm], writes=[t0])
                c.op("dve", lambda e: e.scalar_tensor_tensor(out=t1[:], in0=t0[:], scalar=-BIG, in1=t1[:],
                                                             op0=ALU.mult, op1=ALU.add), reads=[t0, t1], writes=[t1])
                c.op("dve", lambda e: e.reduce_max(out=sm[:, 5:6], in_=t1[:], axis=AX.X), reads=[t1], writes=[sm])
                c.op("dve", lambda e: e.tensor_scalar(out=t2[:], in0=t1[:], scalar1=sm[:, 5:6], scalar2=None,
                                                      op0=ALU.is_equal), reads=[t1, sm], writes=[t2])
                c.op("dve", lambda e: e.tensor_tensor(out=sm[:, 6:7], in0=sm[:, 4:5], in1=sm[:, 5:6], op=ALU.subtract),
                     reads=[sm], writes=[sm])
                c.op("act", lambda e: e.activation(out=sm[:, 7:8], in_=sm[:, 6:7], func=AF.Sigmoid), reads=[sm], writes=[sm])
                c.op("dve", lambda e: e.tensor_tensor(out=sm[:, 8:9], in0=sm[:, 7:8], in1=sm[:, 3:4], op=ALU.mult),
                     reads=[sm], writes=[sm])
                c.op("dve", lambda e: e.tensor_tensor(out=sm[:, 9:10], in0=sm[:, 3:4], in1=sm[:, 8:9], op=ALU.subtract),
                     reads=[sm], writes=[sm])
                c.op("dve", lambda e: e.tensor_scalar(out=t0[:], in0=t0[:], scalar1=sm[:, 8:9], scalar2=None, op0=ALU.mult),
                     reads=[t0, sm], writes=[t0])
                c.op("dve", lambda e: e.scalar_tensor_tensor(out=gate[:, ti, :], in0=t2[:], scalar=sm[:, 9:10], in1=t0[:],
                                                             op0=ALU.mult, op1=ALU.add), reads=[t2, sm, t0], writes=[gate])
            for ex in range(N_EXP):
                wg, wu, wd = wgs[ex % 2], wus[ex % 2], wds[ex % 2]
                load_w_bf(k, wg, k.w_gate[l, ex], k.w_gate)
                load_w_bf(k, wu, k.w_up[l, ex], k.w_up)
                load_w_bf(k, wd, k.w_down[l, ex], k.w_down)
                for j in range(TB // 512):
                    hT = hTs[j % 2]
                    for fb in range(4):
                        pg = nextp()
                        for kk in range(KD):
                            c.op("pe", lambda e: e.matmul(pg[:], lhsT=wg[:, kk, fb * P:(fb + 1) * P],
                                                          rhs=xT[:, kk, j * 512:(j + 1) * 512],
                                                          start=(kk == 0), stop=(kk == KD - 1)), reads=[wg, xT], writes=[pg])
                        c.op("act", lambda e: e.activation(out=sg[:], in_=pg[:], func=AF.Silu), reads=[pg], writes=[sg])
                        pu = nextp()
                        for kk in range(KD):
                            c.op("pe", lambda e: e.matmul(pu[:], lhsT=wu[:, kk, fb * P:(fb + 1) * P],
                                                          rhs=xT[:, kk, j * 512:(j + 1) * 512],
                                                          start=(kk == 0), stop=(kk == KD - 1)), reads=[wu, xT], writes=[pu])
                        c.op("dve", lambda e: e.tensor_tensor(out=hT[:, fb, :], in0=pu[:], in1=sg[:], op=ALU.mult),
                             reads=[pu, sg], writes=[hT])
                    for i in range(4):
                        ti = j * 4 + i
                        pa = PAs[ti % 2]
                        for half in range(2):
                            for fb in range(4):
                                c.op("pe", lambda e: e.matmul(pa[:, half, :], lhsT=hT[:, fb, i * P:(i + 1) * P],
                                                              rhs=wd[:, fb, half * 512:(half + 1) * 512],
                                                              start=(fb == 0), stop=(fb == 3)), reads=[hT, wd], writes=[pa])
                        pav = pa[:].rearrange("p a b -> p (a b)")
                        if ex == 0:
                            c.op("dve", lambda e: e.tensor_scalar(out=yacc[:, ti, :], in0=pav, scalar1=gate[:, ti, ex:ex + 1],
                                                                  scalar2=None, op0=ALU.mult),
                                 reads=[pa, gate], writes=[yacc])
                        else:
                            c.op("dve", lambda e: e.scalar_tensor_tensor(out=yacc[:, ti, :], in0=pav,
                                                                         scalar=gate[:, ti, ex:ex + 1], in1=yacc[:, ti, :],
                                                                         op0=ALU.mult, op1=ALU.add),
                                 reads=[pa, gate, yacc], writes=[yacc])
            for ti in range(NTB):
                epilogue(k, yacc[:, ti, :], yacc, tb0 // P + ti, xin, xout, xtout)
        c.barrier()


def mix_consts(k):
    c, nc = k.c, k.nc

    def sb(name, shape, dt=F32):
        return Buf(nc.alloc_sbuf_tensor(name, list(shape), dt), name)

    k.lb = sb("lb", [P, DEPTH, 4])
    k.oml = sb("oml", [P, DEPTH, 4])
    k.mk_incl = sb("mk_incl", [64, 4, 64])
    k.mk_strict = sb("mk_strict", [64, 4, 64])
    k.ones256 = sb("ones256", [P, 256])
    lgt = sb("lgt", [P, DEPTH, 4])
    tmp = sb("lbtmp", [P, 4])
    with nc.allow_non_contiguous_dma(reason="tiny param transposes"):
        c.dma("sp", lgt[:], k.hg_lb_logits[:, :].rearrange("l (h d) -> d l h", h=4), lgt, k.hg_lb_logits)
    c.op("dve", lambda e: e.tensor_tensor(out=tmp[:], in0=lgt[:, 0, :], in1=lgt[:, 1, :], op=ALU.max), reads=[lgt], writes=[tmp])
    c.op("dve", lambda e: e.tensor_tensor(out=tmp[:], in0=tmp[:], in1=lgt[:, 2, :], op=ALU.max), reads=[lgt, tmp], writes=[tmp])
    c.op("dve", lambda e: e.tensor_tensor(out=tmp[:], in0=tmp[:], in1=lgt[:, 3, :], op=ALU.max), reads=[lgt, tmp], writes=[tmp])
    c.op("dve", lambda e: e.tensor_tensor(out=lgt[:], in0=lgt[:], in1=tmp[:].unsqueeze(1).to_broadcast([P, DEPTH, 4]),
                                          op=ALU.subtract), reads=[lgt, tmp], writes=[lgt])
    c.op("act", lambda e: e.activation(out=lgt[:], in_=lgt[:], func=AF.Exp), reads=[lgt], writes=[lgt])
    c.op("dve", lambda e: e.tensor_tensor(out=tmp[:], in0=lgt[:, 0, :], in1=lgt[:, 1, :], op=ALU.add), reads=[lgt], writes=[tmp])
    c.op("dve", lambda e: e.tensor_tensor(out=tmp[:], in0=tmp[:], in1=lgt[:, 2, :], op=ALU.add), reads=[lgt, tmp], writes=[tmp])
    c.op("dve", lambda e: e.tensor_tensor(out=tmp[:], in0=tmp[:], in1=lgt[:, 3, :], op=ALU.add), reads=[lgt, tmp], writes=[tmp])
    c.op("dve", lambda e: e.reciprocal(out=tmp[:], in_=tmp[:]), reads=[tmp], writes=[tmp])
    c.op("dve", lambda e: e.tensor_tensor(out=lgt[:], in0=lgt[:], in1=tmp[:].unsqueeze(1).to_broadcast([P, DEPTH, 4]),
                                          op=ALU.mult), reads=[lgt, tmp], writes=[lgt])
    c.op("dve", lambda e: e.memset(k.lb[:, 0, :], 0.0), writes=[k.lb])
    c.op("dve", lambda e: e.tensor_copy(out=k.lb[:, 1, :], in_=lgt[:, 1, :]), reads=[lgt], writes=[k.lb])
    c.op("dve", lambda e: e.tensor_tensor(out=k.lb[:, 2, :], in0=k.lb[:, 1, :], in1=lgt[:, 2, :], op=ALU.add),
         reads=[lgt, k.lb], writes=[k.lb])
    c.op("dve", lambda e: e.tensor_tensor(out=k.lb[:, 3, :], in0=k.lb[:, 2, :], in1=lgt[:, 3, :], op=ALU.add),
         reads=[lgt, k.lb], writes=[k.lb])
    c.op("dve", lambda e: e.tensor_scalar(out=k.oml[:], in0=k.lb[:], scalar1=-1.0, scalar2=1.0, op0=ALU.mult, op1=ALU.add),
         reads=[k.lb], writes=[k.oml])
    for mk, op_ in ((k.mk_incl, ALU.is_ge), (k.mk_strict, ALU.is_gt)):
        c.op("pool", lambda e: e.memset(mk[:], 1.0), writes=[mk])
        c.op("pool", lambda e: e.affine_select(out=mk[:], in_=mk[:], pattern=[[0, 4], [1, 64]], base=0,
                                               channel_multiplier=-1, compare_op=op_, fill=0.0),
             reads=[mk], writes=[mk])
    c.op("pool", lambda e: e.memset(k.ones256[:], 1.0), writes=[k.ones256])


def phase_mix(k, l, xin, xtin, xout, xtout):
    c, nc = k.c, k.nc
    S, NSEQ = k.S, k.NSEQ
    ST = 256
    NCH = ST // 64
    with ExitStack() as es_:
        def sbt(name, shape, dt=F32):
            return Buf(es_.enter_context(nc.sbuf_tensor("%s_L%d" % (name, l), list(shape), dt)), name)

        def pst(name, shape, dt=F32):
            return es_.enter_context(nc.psum_tensor("%s_L%d" % (name, l), list(shape), dt))

        win = sbt("x_win", [P, KD, IN_WIDTH], BF16)
        wout = sbt("x_wout", [P, KD, D], BF16)
        xT = sbt("x_xT", [P, KD, ST], BF16)
        yT = sbt("x_yT", [P, 8, ST], BF16)
        nw = sbt("x_nw", [P, 2])
        cw = sbt("x_cw", [P, 12, 4])
        dtb = sbt("x_dtb", [P, 4])
        negA = sbt("x_negA", [P, 4])
        fa, flf, fk, fcum, fd, fe = (sbt("x_fa", [P, ST]), sbt("x_flf", [P, ST]), sbt("x_fk", [P, ST]),
                                     sbt("x_fcum", [P, ST]), sbt("x_fd", [P, ST]), sbt("x_fe", [P, ST]))
        qt, kt = sbt("x_qt", [P, ST], BF16), sbt("x_kt", [P, ST], BF16)
        Vt = sbt("x_Vt", [64, NCH, 512], BF16)
        hs = sbt("x_hs", [P, 16])
        hsc = sbt("x_hsc", [P, 12])
        AT = sbt("x_AT", [64, 64], BF16)
        ktok = sbt("x_ktok", [64, P], BF16)
        Ssc = sbt("x_Ssc", [P, P], BF16)
        Sh = sbt("x_Sh", [P, 4, P])
        Sg = sbt("x_Sg", [P, 4, P])
        Sgb = sbt("x_Sgb", [P, 4, P], BF16)
        tmpS = sbt("x_tmpS", [P, P])
        osb = sbt("x_osb", [P, ST])
        sq = sbt("x_sq", [P, ST])
        rst = sbt("x_rst", [P, ST])
        gz = sbt("x_gz", [P, ST])
        hist = sbt("x_hist", [P, 12, 3])
        xp = sbt("x_xp", [P, ST + 3])
        yc = sbt("x_yc", [P, ST])
        ys = sbt("x_ys", [P, ST])
        gqT = sbt("x_gqT", [P, 4, ST], BF16)
        gkT = sbt("x_gkT", [P, 4, ST], BF16)
        gvF = sbt("x_gvF", [P, 4, ST], BF16)
        og = sbt("x_og", [P, 4, ST])
        ga = sbt("x_ga", [64, 16])
        gb = sbt("x_gb", [64, 16])
        ecl = sbt("x_ecl", [P, 8])
        Dg = sbt("x_Dg", [64, 4, 64])
        expR = sbt("x_expR", [P, 4, 64], BF16)
        qeT = sbt("x_qeT", [P, 4, 64], BF16)
        decS = sbt("x_decS", [64, 4, 64])
        decI = sbt("x_decI", [64, 4, 64])
        qkT = sbt("x_qkT", [64, 4, 64], BF16)
        Ab = [sbt("x_A0", [64, 4, 64]), sbt("x_A1", [64, 4, 64])]
        Bb = [sbt("x_B0", [64, 4, 64]), sbt("x_B1", [64, 4, 64])]
        Pb = [sbt("x_P0", [64, 4, 64]), sbt("x_P1", [64, 4, 64])]
        TT = sbt("x_TT", [64, 4, 64], BF16)
        vtok = sbt("x_vtok", [64, 4, P], BF16)
        ktk = sbt("x_ktk", [64, 4, P], BF16)
        ke = sbt("x_ke", [64, 4, P], BF16)
        kdec = sbt("x_kdec", [64, 4, P], BF16)
        ub = sbt("x_ub", [64, 4, P])
        wT = sbt("x_wT", [P, 4, 64], BF16)
        vnew = sbt("x_vnew", [64, P], BF16)
        pj = [Buf(pst("x_pj0", [P, 512])), Buf(pst("x_pj1", [P, 512]))]
        mh = []
        for i in range(3):
            mh.append(Buf(pst("x_m%d" % i, [P, 512])))
        cnt = {"pj": 0, "m": 0}

        def nextpj():
            cnt["pj"] += 1
            return pj[cnt["pj"] % 2]

        def nextm():
            cnt["m"] += 1
            return mh[cnt["m"] % 3]

        idf64 = k.idf[0:64, 0:64]

        load_w_bf(k, win, k.w_in[l], k.w_in)
        load_w_bf(k, wout, k.w_out[l], k.w_out)
        with nc.allow_non_contiguous_dma(reason="tiny param transposes"):
            c.dma("sp", nw[:, 0:1], k.hg_norm_w[l:l + 1, :].rearrange("o v -> v o"), nw, k.hg_norm_w)
            c.dma("sp", nw[:, 1:2], k.gd_norm_w[l:l + 1, :].rearrange("o v -> v o"), nw, k.gd_norm_w)
            for j in range(4):
                c.dma("sp", cw[:, :, j], k.gd_conv_w[l, j, :].rearrange("(b c) -> c b", c=P), cw, k.gd_conv_w)
        c.dma("sp", dtb[:], k.gd_dt_bias[l, :].partition_broadcast(P), dtb, k.gd_dt_bias)
        c.dma("sp", negA[:], k.gd_a_log[l, :].partition_broadcast(P), negA, k.gd_a_log)
        c.op("act", lambda e: e.activation(out=negA[:], in_=negA[:], func=AF.Exp), reads=[negA], writes=[negA])
        c.op("dve", lambda e: e.tensor_scalar(out=negA[:], in0=negA[:], scalar1=-1.0, scalar2=None, op0=ALU.mult),
             reads=[negA], writes=[negA])

        def proj_F(col0, n=P):
            pp = nextpj()
            for kk in range(KD):
                c.op("pe", lambda e: e.matmul(pp[0:n, 0:ST], lhsT=win[:, kk, col0:col0 + n], rhs=xT[:, kk, :],
                                              start=(kk == 0), stop=(kk == KD - 1)), reads=[win, xT], writes=[pp])
            return pp

        def rms_gate(o_ap, o_buf, gate_col0, gate_fn, nwcol, ydst):
            c.op("act", lambda e: e.activation(out=sq[:], in_=o_ap, func=AF.Square), reads=[o_buf], writes=[sq])
            pm = nextm()
            c.op("pe", lambda e: e.matmul(pm[:, 0:ST], lhsT=k.onesf[:], rhs=sq[:], start=True, stop=True),
                 reads=[k.onesf, sq], writes=[pm])
            c.op("act", lambda e: e.activation(out=rst[:], in_=pm[:, 0:ST], func=AF.Sqrt, bias=k.cst[:, 1:2], scale=1.0 / 128.0),
                 reads=[pm, k.cst], writes=[rst])
            c.op("dve", lambda e: e.reciprocal(out=rst[:], in_=rst[:]), reads=[rst], writes=[rst])
            pz = proj_F(gate_col0)
            c.op("act", lambda e: e.activation(out=gz[:], in_=pz[:, 0:ST], func=gate_fn), reads=[pz], writes=[gz])
            c.op("pool", lambda e: e.tensor_mul(out=rst[:], in0=rst[:], in1=gz[:]), reads=[rst, gz], writes=[rst])
            c.op("dve", lambda e: e.scalar_tensor_tensor(out=ydst, in0=o_ap, scalar=nw[:, nwcol:nwcol + 1], in1=rst[:],
                                                         op0=ALU.mult, op1=ALU.mult), reads=[o_buf, nw, rst], writes=[yT])

        nst_seq = S // ST
        for s in range(NSEQ):
            c.op("pool", lambda e: e.memset(Sh[:], 0.0), writes=[Sh])
            c.op("pool", lambda e: e.memset(Sg[:], 0.0), writes=[Sg])
            c.op("pool", lambda e: e.memset(Sgb[:], 0.0), writes=[Sgb])
            c.op("pool", lambda e: e.memset(hist[:], 0.0), writes=[hist])
            for sti in range(nst_seq):
                t0 = s * S + sti * ST
                load_xT(k, xT, xtin, t0, ST)
                for ch in range(NCH):
                    pp = nextpj()
                    for kk in range(KD):
                        c.op("pe", lambda e: e.matmul(pp[0:64, :], lhsT=xT[:, kk, ch * 64:(ch + 1) * 64], rhs=win[:, kk, 1024:1536],
                                                      start=(kk == 0), stop=(kk == KD - 1)), reads=[win, xT], writes=[pp])
                    c.op("act", lambda e: e.activation(out=Vt[:, ch, :], in_=pp[0:64, :], func=AF.Copy), reads=[pp], writes=[Vt])
                for h in range(4):
                    pf = proj_F(512 + h * P)
                    c.op("act", lambda e: e.activation(out=fa[:], in_=pf[:, 0:ST], func=AF.Sigmoid), reads=[pf], writes=[fa])
                    c.op("dve", lambda e: e.tensor_scalar(out=fa[:], in0=fa[:], scalar1=k.oml[:, l, h:h + 1], scalar2=None,
                                                          op0=ALU.mult), reads=[fa, k.oml], writes=[fa])
                    c.op("act", lambda e: e.activation(out=flf[:], in_=fa[:], func=AF.Ln, bias=k.lb[:, l, h:h + 1]),
                         reads=[fa, k.lb], writes=[flf])
                    c.op("pool", lambda e: e.tensor_scalar(out=fk[:], in0=fa[:], scalar1=-1.0, scalar2=k.oml[:, l, h:h + 1],
                                                           op0=ALU.mult, op1=ALU.add), reads=[fa, k.oml], writes=[fk])
                    c.op("dve", lambda e: e.tensor_tensor_scan(out=fcum[:], data0=k.ones256[:, 0:ST], data1=flf[:], initial=0.0,
                                                               op0=ALU.mult, op1=ALU.add), reads=[k.ones256, flf], writes=[fcum])
                    cv = fcum[:].rearrange("p (c t) -> p c t", t=64)
                    c.op("dve", lambda e: e.memset(hs[:, 0:1], 0.0), writes=[hs])
                    c.op("dve", lambda e: e.tensor_copy(out=hs[:, 1:NCH], in_=cv[:, 0:NCH - 1, 63]), reads=[fcum], writes=[hs])
                    c.op("dve", lambda e: e.tensor_tensor(out=hs[:, 4:4 + NCH], in0=cv[:, :, 31], in1=hs[:, 0:NCH], op=ALU.subtract),
                         reads=[fcum, hs], writes=[hs])
                    c.op("dve", lambda e: e.tensor_tensor(out=hs[:, 8:8 + NCH], in0=cv[:, :, 63], in1=hs[:, 0:NCH], op=ALU.subtract),
                         reads=[fcum, hs], writes=[hs])
                    c.op("dve", lambda e: e.tensor_tensor(out=hs[:, 12:12 + NCH], in0=cv[:, :, 63], in1=cv[:, :, 31], op=ALU.subtract),
                         reads=[fcum, hs], writes=[hs])
                    c.op("act", lambda e: e.activation(out=hsc[:], in_=hs[:, 4:16], func=AF.Exp), reads=[hs], writes=[hsc])
                    c.op("dve", lambda e: e.tensor_tensor(out=fd[:].rearrange("p (c t) -> p c t", t=64), in0=cv,
                                                          in1=cv[:, :, 31:32].to_broadcast([P, NCH, 64]), op=ALU.subtract),
                         reads=[fcum], writes=[fd])
                    c.op("act", lambda e: e.activation(out=fe[:], in_=fd[:], func=AF.Exp), reads=[fd], writes=[fe])
                    pq = proj_F(h * P)
                    c.op("dve", lambda e: e.tensor_tensor(out=qt[:], in0=pq[:, 0:ST], in1=fe[:], op=ALU.mult),
                         reads=[pq, fe], writes=[qt])
                    c.op("act", lambda e: e.activation(out=fe[:], in_=fd[:], func=AF.Exp, scale=-1.0), reads=[fd], writes=[fe])
                    c.op("dve", lambda e: e.tensor_tensor(out=kt[:], in0=fk[:], in1=fe[:], op=ALU.mult),
                         reads=[fk, fe], writes=[kt])
                    for ch in range(NCH):
                        cs = slice(ch * 64, (ch + 1) * 64)
                        pm = nextm()
                        c.op("pe", lambda e: e.matmul(pm[0:64, 0:64], lhsT=kt[:, cs], rhs=qt[:, cs], start=True, stop=True),
                             reads=[kt, qt], writes=[pm])
                        c.op("dve", lambda e: e.tensor_tensor(out=AT[:], in0=pm[0:64, 0:64], in1=k.mk_incl[:, 0, :], op=ALU.mult),
                             reads=[pm, k.mk_incl], writes=[AT])
                        c.op("pe", lambda e: e.transpose(out=k.pT[0:64, 0, :], in_=kt[:, cs], identity=k.idb[:]),
                             reads=[kt, k.idb], writes=[k.pT])
                        c.op("act", lambda e: e.activation(out=ktok[:], in_=k.pT[0:64, 0, :], func=AF.Copy), reads=[k.pT], writes=[ktok])
                        c.op("dve", lambda e: e.tensor_scalar(out=Ssc[:], in0=Sh[:, h, :], scalar1=hsc[:, ch:ch + 1], scalar2=None,
                                                              op0=ALU.mult), reads=[Sh, hsc], writes=[Ssc])
                        po = nextm()
                        c.op("pe", lambda e: e.matmul(po[:, 0:64], lhsT=Vt[:, ch, h * P:(h + 1) * P], rhs=AT[:], start=True, stop=False),
                             reads=[Vt, AT], writes=[po])
                        c.op("pe", lambda e: e.matmul(po[:, 0:64], lhsT=Ssc[:], rhs=qt[:, cs], start=False, stop=True),
                             reads=[Ssc, qt], writes=[po])
                        c.op("act", lambda e: e.activation(out=osb[:, cs], in_=po[:, 0:64], func=AF.Copy), reads=[po], writes=[osb])
                        pd = nextm()
                        c.op("pe", lambda e: e.matmul(pd[:, 0:P], lhsT=ktok[:], rhs=Vt[:, ch, h * P:(h + 1) * P], start=True, stop=True),
                             reads=[ktok, Vt], writes=[pd])
                        c.op("act", lambda e: e.activation(out=tmpS[:], in_=pd[:, 0:P], func=AF.Copy, scale=hsc[:, 8 + ch:9 + ch]),
                             reads=[pd, hsc], writes=[tmpS])
                        c.op("dve", lambda e: e.scalar_tensor_tensor(out=Sh[:, h, :], in0=Sh[:, h, :], scalar=hsc[:, 4 + ch:5 + ch],
                                                                     in1=tmpS[:], op0=ALU.mult, op1=ALU.add),
                             reads=[Sh, hsc, tmpS], writes=[Sh])
                    rms_gate(osb[:], osb, 1536 + h * P, AF.Sigmoid, 0, yT[:, h, :])
                for b in range(12):
                    pp = proj_F(2048 + b * P)
                    c.op("act", lambda e: e.activation(out=xp[:, 3:3 + ST], in_=pp[:, 0:ST], func=AF.Copy), reads=[pp], writes=[xp])
                    c.op("pool", lambda e: e.tensor_copy(out=xp[:, 0:3], in_=hist[:, b, :]), reads=[hist], writes=[xp])
                    c.op("dve", lambda e: e.tensor_scalar(out=yc[:], in0=xp[:, 3:3 + ST], scalar1=cw[:, b, 3:4], scalar2=None,
                                                          op0=ALU.mult), reads=[xp, cw], writes=[yc])
                    for j in (2, 1, 0):
                        eng = "dve"
                        c.op(eng, lambda e: e.scalar_tensor_tensor(out=yc[:], in0=xp[:, j:j + ST], scalar=cw[:, b, j:j + 1], in1=yc[:],
                                                                   op0=ALU.mult, op1=ALU.add), reads=[xp, cw, yc], writes=[yc])
                    c.op("pool", lambda e: e.tensor_copy(out=hist[:, b, :], in_=xp[:, ST:ST + 3]), reads=[xp], writes=[hist])
                    if b >= 8:
                        c.op("act", lambda e: e.activation(out=gvF[:, b - 8, :], in_=yc[:], func=AF.Silu), reads=[yc], writes=[gvF])
                    else:
                        dst = gqT if b < 4 else gkT
                        c.op("act", lambda e: e.activation(out=ys[:], in_=yc[:], func=AF.Silu), reads=[yc], writes=[ys])
                        c.op("act", lambda e: e.activation(out=sq[:], in_=ys[:], func=AF.Square), reads=[ys], writes=[sq])
                        pm = nextm()
                        c.op("pe", lambda e: e.matmul(pm[:, 0:ST], lhsT=k.onesf[:], rhs=sq[:], start=True, stop=True),
                             reads=[k.onesf, sq], writes=[pm])
                        c.op("act", lambda e: e.activation(out=rst[:], in_=pm[:, 0:ST], func=AF.Sqrt, bias=k.cst[:, 2:3]),
                             reads=[pm, k.cst], writes=[rst])
                        c.op("dve", lambda e: e.reciprocal(out=rst[:], in_=rst[:]), reads=[rst], writes=[rst])
                        sc = (128.0 ** -0.5) if b < 4 else 1.0
                        c.op("dve", lambda e: e.scalar_tensor_tensor(out=dst[:, b % 4, :], in0=ys[:], scalar=sc, in1=rst[:],
                                                                     op0=ALU.mult, op1=ALU.mult), reads=[ys, rst], writes=[dst])
                for ch in range(NCH):
                    cs = slice(ch * 64, (ch + 1) * 64)
                    pg = nextm()
                    for kk in range(KD):
                        c.op("pe", lambda e: e.matmul(pg[0:64, 0:8], lhsT=xT[:, kk, cs], rhs=win[:, kk, 3584:3592],
                                                      start=(kk == 0), stop=(kk == KD - 1)), reads=[win, xT], writes=[pg])
                    c.op("dve", lambda e: e.tensor_tensor(out=ga[:, 0:4], in0=pg[0:64, 0:4], in1=dtb[0:64, :], op=ALU.add),
                         reads=[pg, dtb], writes=[ga])
                    c.op("act", lambda e: e.activation(out=ga[:, 4:8], in_=ga[:, 0:4], func=AF.Exp), reads=[ga], writes=[ga])
                    c.op("act", lambda e: e.activation(out=ga[:, 4:8], in_=ga[:, 4:8], func=AF.Ln, bias=k.cst[0:64, 3:4]),
                         reads=[ga, k.cst], writes=[ga])
                    c.op("dve", lambda e: e.tensor_tensor(out=ga[:, 8:12], in0=ga[:, 4:8], in1=negA[0:64, :], op=ALU.mult),
                         reads=[ga, negA], writes=[ga])
                    c.op("act", lambda e: e.activation(out=ga[:, 12:16], in_=pg[0:64, 4:8], func=AF.Sigmoid), reads=[pg], writes=[ga])
                    c.op("dve", lambda e: e.tensor_scalar(out=gb[:, 0:4], in0=ga[:, 12:16], scalar1=-1.0, scalar2=None, op0=ALU.mult),
                         reads=[ga], writes=[gb])
                    pc = nextm()
                    c.op("pe", lambda e: e.matmul(pc[0:64, 0:4], lhsT=k.mk_incl[:, 0, :], rhs=ga[:, 8:12], start=True, stop=True),
                         reads=[k.mk_incl, ga], writes=[pc])
                    c.op("dve", lambda e: e.tensor_copy(out=gb[:, 4:8], in_=pc[0:64, 0:4]), reads=[pc], writes=[gb])
                    pt_ = nextm()
                    c.op("pe", lambda e: e.matmul(pt_[:, 0:4], lhsT=k.onesf[0:64, :], rhs=ga[:, 8:12], start=True, stop=True),
                         reads=[k.onesf, ga], writes=[pt_])
                    c.op("dve", lambda e: e.tensor_copy(out=ecl[:, 0:4], in_=pt_[:, 0:4]), reads=[pt_], writes=[ecl])
                    c.op("act", lambda e: e.activation(out=ecl[:, 4:8], in_=ecl[:, 0:4], func=AF.Exp), reads=[ecl], writes=[ecl])
                    c.op("act", lambda e: e.activation(out=gb[:, 8:12], in_=gb[:, 4:8], func=AF.Exp), reads=[gb], writes=[gb])
                    c.op("dve", lambda e: e.tensor_tensor(out=gb[:, 12:16], in0=ecl[0:64, 0:4], in1=gb[:, 4:8], op=ALU.subtract),
                         reads=[ecl, gb], writes=[gb])
                    c.op("act", lambda e: e.activation(out=gb[:, 12:16], in_=gb[:, 12:16], func=AF.Exp), reads=[gb], writes=[gb])
                    for h in range(4):
                        c.op("dve", lambda e: e.tensor_scalar(out=Dg[:, h, :], in0=idf64, scalar1=gb[:, 4 + h:5 + h], scalar2=None,
                                                              op0=ALU.mult), reads=[k.idf, gb], writes=[Dg])
                    pr = nextm()
                    c.op("pe", lambda e: e.matmul(pr[:, 0:256], lhsT=k.onesf[0:64, :], rhs=Dg[:].rearrange("s h t -> s (h t)"),
                                                  start=True, stop=True), reads=[k.onesf, Dg], writes=[pr])
                    c.op("act", lambda e: e.activation(out=expR[:].rearrange("p h t -> p (h t)"), in_=pr[:, 0:256], func=AF.Exp),
                         reads=[pr], writes=[expR])
                    c.op("dve", lambda e: e.tensor_tensor(out=qeT[:], in0=gqT[:, :, cs], in1=expR[:], op=ALU.mult),
                         reads=[gqT, expR], writes=[qeT])
                    for h in range(4):
                        c.op("dve", lambda e: e.tensor_scalar(out=decS[:, h, :], in0=pr[0:64, h * 64:(h + 1) * 64],
                                                              scalar1=gb[:, 4 + h:5 + h], scalar2=0.0, op0=ALU.subtract, op1=ALU.min),
                             reads=[pr, gb], writes=[decS])
                    c.op("act", lambda e: e.activation(out=decS[:], in_=decS[:], func=AF.Exp), reads=[decS], writes=[decS])
                    c.op("pool", lambda e: e.tensor_mul(out=decI[:], in0=decS[:], in1=k.mk_incl[:]), reads=[decS, k.mk_incl], writes=[decI])
                    c.op("pool", lambda e: e.tensor_mul(out=decS[:], in0=decS[:], in1=k.mk_strict[:]), reads=[decS, k.mk_strict], writes=[decS])
                    pG = nextm()
                    pQ = nextm()
                    for h in range(4):
                        c.op("pe", lambda e: e.matmul(pG[0:64, h * 64:(h + 1) * 64], lhsT=gkT[:, h, cs], rhs=gkT[:, h, cs], start=True, stop=True),
                             reads=[gkT], writes=[pG])
                    for h in range(4):
                        c.op("pe", lambda e: e.matmul(pQ[0:64, h * 64:(h + 1) * 64], lhsT=gkT[:, h, cs], rhs=gqT[:, h, cs], start=True, stop=True),
                             reads=[gkT, gqT], writes=[pQ])
                    B0, A0, P0 = Bb[0], Ab[0], Pb[0]
                    c.op("dve", lambda e: e.tensor_tensor(out=B0[:].rearrange("s h t -> s (h t)"), in0=pG[0:64, 0:256],
                                                          in1=decS[:].rearrange("s h t -> s (h t)"), op=ALU.mult),
                         reads=[pG, decS], writes=[B0])
                    c.op("dve", lambda e: e.tensor_tensor(out=B0[:], in0=B0[:], in1=ga[:, 12:16].unsqueeze(2).to_broadcast([64, 4, 64]),
                                                          op=ALU.mult), reads=[B0, ga], writes=[B0])
                    c.op("dve", lambda e: e.tensor_tensor(out=qkT[:].rearrange("s h t -> s (h t)"), in0=pQ[0:64, 0:256],
                                                          in1=decI[:].rearrange("s h t -> s (h t)"), op=ALU.mult),
                         reads=[pQ, decI], writes=[qkT])
                    pA = nextm()
                    for h in range(4):
                        c.op("pe", lambda e: e.transpose(out=pA[0:64, h * 64:(h + 1) * 64], in_=B0[:, h, :], identity=idf64),
                             reads=[B0, k.idf], writes=[pA])
                    c.op("act", lambda e: e.activation(out=A0[:].rearrange("s h t -> s (h t)"), in_=pA[0:64, 0:256], func=AF.Copy),
                         reads=[pA], writes=[A0])
                    c.op("pool", lambda e: e.tensor_sub(out=P0[:], in0=k.mk_incl[:], in1=k.mk_strict[:]), reads=[k.mk_incl, k.mk_strict], writes=[P0])
                    c.op("pool", lambda e: e.tensor_sub(out=P0[:], in0=P0[:], in1=B0[:]), reads=[P0, B0], writes=[P0])
                    for lv in range(5):
                        Ak, Bk, Pk = Ab[lv % 2], Bb[lv % 2], Pb[lv % 2]
                        An, Bn, Pn = Ab[(lv + 1) % 2], Bb[(lv + 1) % 2], Pb[(lv + 1) % 2]
                        pa_ = nextm()
                        for h in range(4):
                            c.op("pe", lambda e: e.matmul(pa_[0:64, h * 64:(h + 1) * 64], lhsT=Bk[:, h, :], rhs=Ak[:, h, :], start=True, stop=True),
                                 reads=[Ak, Bk], writes=[pa_])
                        if lv < 4:
                            pb_ = nextm()
                            for h in range(4):
                                c.op("pe", lambda e: e.matmul(pb_[0:64, h * 64:(h + 1) * 64], lhsT=Ak[:, h, :], rhs=Bk[:, h, :], start=True, stop=True),
                                     reads=[Ak, Bk], writes=[pb_])
                        c.op("act", lambda e: e.activation(out=An[:].rearrange("s h t -> s (h t)"), in_=pa_[0:64, 0:256], func=AF.Copy),
                             reads=[pa_], writes=[An])
                        if lv < 4:
                            c.op("dve", lambda e: e.tensor_copy(out=Bn[:].rearrange("s h t -> s (h t)"), in_=pb_[0:64, 0:256]),
                                 reads=[pb_], writes=[Bn])
                        pp_ = nextm()
                        for h in range(4):
                            c.op("pe", lambda e: e.matmul(pp_[0:64, h * 64:(h + 1) * 64], lhsT=An[:, h, :], rhs=Pk[:, h, :], start=True, stop=True),
                                 reads=[An, Pk], writes=[pp_])
                        c.op("dve", lambda e: e.tensor_tensor(out=Pn[:].rearrange("s h t -> s (h t)"), in0=pp_[0:64, 0:256],
                                                              in1=Pk[:].rearrange("s h t -> s (h t)"), op=ALU.add),
                             reads=[pp_, Pk], writes=[Pn])
                    Pf = Pb[5 % 2]
                    c.op("act", lambda e: e.activation(out=TT[:], in_=Pf[:], func=AF.Copy), reads=[Pf], writes=[TT])
                    for h in range(4):
                        c.op("pe", lambda e: e.transpose(out=k.pT[0:64, h, :], in_=gvF[:, h, cs], identity=k.idb[:]),
                             reads=[gvF, k.idb], writes=[k.pT])
                    c.op("act", lambda e: e.activation(out=vtok[:], in_=k.pT[0:64, 0:4, :], func=AF.Copy), reads=[k.pT], writes=[vtok])
                    for h in range(4):
                        c.op("pe", lambda e: e.transpose(out=k.pT[0:64, 4 + h, :], in_=gkT[:, h, cs], identity=k.idb[:]),
                             reads=[gkT, k.idb], writes=[k.pT])
                    c.op("act", lambda e: e.activation(out=ktk[:], in_=k.pT[0:64, 4:8, :], func=AF.Copy), reads=[k.pT], writes=[ktk])
                    c.op("dve", lambda e: e.tensor_tensor(out=ke[:], in0=ktk[:], in1=gb[:, 8:12].unsqueeze(2).to_broadcast([64, 4, P]),
                                                          op=ALU.mult), reads=[ktk, gb], writes=[ke])
                    c.op("pool", lambda e: e.tensor_tensor(out=kdec[:], in0=ktk[:], in1=gb[:, 12:16].unsqueeze(2).to_broadcast([64, 4, P]),
                                                           op=ALU.mult), reads=[ktk, gb], writes=[kdec])
                    for hp in range(2):
                        pu = nextm()
                        for hh in range(2):
                            h = hp * 2 + hh
                            c.op("pe", lambda e: e.matmul(pu[0:64, hh * P:(hh + 1) * P], lhsT=TT[:, h, :], rhs=vtok[:, h, :], start=True, stop=True),
                                 reads=[TT, vtok], writes=[pu])
                        c.op("dve", lambda e: e.tensor_tensor(out=ub[:, hp * 2:hp * 2 + 2, :],
                                                              in0=pu[0:64, 0:256].rearrange("s (h v) -> s h v", h=2),
                                                              in1=ga[:, 12 + hp * 2:14 + hp * 2].unsqueeze(2).to_broadcast([64, 2, P]),
                                                              op=ALU.mult), reads=[pu, ga], writes=[ub])
                    pw = nextm()
                    for h in range(4):
                        c.op("pe", lambda e: e.matmul(pw[:, h * 64:(h + 1) * 64], lhsT=ke[:, h, :], rhs=TT[:, h, :], start=True, stop=True),
                             reads=[ke, TT], writes=[pw])
                    c.op("act", lambda e: e.activation(out=wT[:].rearrange("p h t -> p (h t)"), in_=pw[:, 0:256], func=AF.Copy),
                         reads=[pw], writes=[wT])
                    for h in range(4):
                        pws = nextm()
                        c.op("pe", lambda e: e.matmul(pws[0:64, 0:P], lhsT=wT[:, h, :], rhs=Sgb[:, h, :], start=True, stop=True),
                             reads=[wT, Sgb], writes=[pws])
                        c.op("dve", lambda e: e.scalar_tensor_tensor(out=vnew[:], in0=pws[0:64, 0:P], scalar=gb[:, h:h + 1], in1=ub[:, h, :],
                                                                     op0=ALU.mult, op1=ALU.add), reads=[pws, gb, ub], writes=[vnew])
                        po = nextm()
                        c.op("pe", lambda e: e.matmul(po[:, 0:64], lhsT=Sgb[:, h, :], rhs=qeT[:, h, :], start=True, stop=False),
                             reads=[Sgb, qeT], writes=[po])
                        c.op("pe", lambda e: e.matmul(po[:, 0:64], lhsT=vnew[:], rhs=qkT[:, h, :], start=False, stop=True),
                             reads=[vnew, qkT], writes=[po])
                        c.op("act", lambda e: e.activation(out=og[:, h, cs], in_=po[:, 0:64], func=AF.Copy), reads=[po], writes=[og])
                        pd = nextm()
                        c.op("pe", lambda e: e.matmul(pd[:, 0:P], lhsT=kdec[:, h, :], rhs=vnew[:], start=True, stop=True),
                             reads=[kdec, vnew], writes=[pd])
                        c.op("dve", lambda e: e.scalar_tensor_tensor(out=Sg[:, h, :], in0=Sg[:, h, :], scalar=ecl[:, 4 + h:5 + h],
                                                                     in1=pd[:, 0:P], op0=ALU.mult, op1=ALU.add),
                             reads=[Sg, ecl, pd], writes=[Sg])
                        c.op("act", lambda e: e.activation(out=Sgb[:, h, :], in_=Sg[:, h, :], func=AF.Copy), reads=[Sg], writes=[Sgb])
                for h in range(4):
                    rms_gate(og[:, h, :], og, 3592 + h * P, AF.Silu, 1, yT[:, 4 + h, :])
                for i in range(ST // P):
                    pa = k.PA[0]
                    for half in range(2):
                        for fb in range(8):
                            c.op("pe", lambda e: e.matmul(pa[:, half, :], lhsT=yT[:, fb, i * P:(i + 1) * P],
                                                          rhs=wout[:, fb, half * 512:(half + 1) * 512],
                                                          start=(fb == 0), stop=(fb == 7)), reads=[yT, wout], writes=[pa])
                    epilogue(k, pa[:].rearrange("p a b -> p (a b)"), pa, t0 // P + i, xin, xout, xtout)
        c.barrier()


_NC_CACHE = {}


def kernel(**inputs):
    n = 8
    if "nc" not in _NC_CACHE:
        _NC_CACHE["nc"] = build()
    nc = _NC_CACHE["nc"]
    x = np.ascontiguousarray(inputs["x"], dtype=np.float32)
    mem = np.ascontiguousarray(inputs["mem"], dtype=np.float32)
    in_maps = []
    for ci in range(n):
        m = {kk: np.ascontiguousarray(vv, dtype=np.float32) for kk, vv in inputs.items() if kk not in ("x", "mem")}
        m["x"] = x[2 * ci:2 * ci + 2].reshape(2 * 2048, D)
        m["mem"] = mem[2 * ci:2 * ci + 2].reshape(2 * MEM_LEN, D)
        in_maps.append(m)
    res = run_bass_kernel_spmd(nc, in_maps, core_ids=list(range(n)))
    out = np.concatenate([r["out"].reshape(2, 2048, D) for r in res.results], axis=0)
    return out.astype(np.float32)
```

```python
from contextlib import ExitStack
import numpy as np
import concourse.bass as bass
import concourse.mybir as mybir
from concourse.bass_utils import run_bass_kernel_spmd

F32 = mybir.dt.float32
BF16 = mybir.dt.bfloat16
AF = mybir.ActivationFunctionType
ALU = mybir.AluOpType
AX = mybir.AxisListType

P = 128
D = 1024
KD = 8
DEPTH = 4
MEM_LEN = 256
IN_WIDTH = 4104
N_EXP = 32
D_EXP = 512
ALPHA = (2.0 * DEPTH) ** 0.25
LN_EPS = 1e-5
RMS_EPS = 1e-6
L2_EPS = 1e-6
BIG = 1.0e30


class Buf:
    __slots__ = ("t", "w", "r", "sem", "cnt", "name")

    def __init__(self, t, name=""):
        self.t = t
        self.w = None
        self.r = []
        self.sem = None
        self.cnt = 0
        self.name = name

    def __getitem__(self, k):
        return self.t[k]


class Ctx:
    ENG = ("pe", "act", "dve", "pool", "sp")

    def __init__(self, nc):
        self.nc = nc
        self.engs = {"pe": nc.tensor, "act": nc.scalar, "dve": nc.vector,
                     "pool": nc.gpsimd, "sp": nc.sync}
        self.sem = {e: nc.alloc_semaphore("s_" + e) for e in self.ENG}
        self.cnt = {e: 0 for e in self.ENG}
        self.seen = {e: {} for e in self.ENG}
        self.dma_bufs = []
        self.nsem = 5
        self.ninst = 0
        self.sched = None

    def _wait(self, eng, ev):
        if ev is None:
            return
        sem, val = ev
        if eng == "pe" and sem is self.sem["pe"]:
            return
        if self.seen[eng].get(sem.num, 0) >= val:
            return
        self.engs[eng].wait_ge(sem, val)
        self.seen[eng][sem.num] = val
        self.ninst += 1

    def _deps(self, eng, reads, writes):
        for b in reads:
            self._wait(eng, b.w)
        for b in writes:
            self._wait(eng, b.w)
            for ev in b.r:
                self._wait(eng, ev)

    def _commit(self, ev, reads, writes):
        for b in reads:
            b.r.append(ev)
            if len(b.r) > 40:
                last = {}
                for s, v in b.r:
                    if v > last.get(s.num, (None, 0))[1]:
                        last[s.num] = (s, v)
                b.r = list(last.values())
        for b in writes:
            b.w = ev
            b.r = []

    def op(self, eng, fn, reads=(), writes=()):
        self._deps(eng, reads, writes)
        ins = fn(self.engs[eng])
        self.cnt[eng] += 1
        self.ninst += 1
        ins.then_inc(self.sem[eng], 1)
        self._commit((self.sem[eng], self.cnt[eng]), reads, writes)
        if self.sched is not None:
            self.sched.tick()
        return ins

    def dma(self, eng, out, in_, dst, src, **kw):
        self._deps(eng, [src], [dst])
        if dst.sem is None:
            dst.sem = self.nc.alloc_semaphore("d%d" % self.nsem)
            self.nsem += 1
            self.dma_bufs.append(dst)
        ins = self.engs[eng].dma_start(out=out, in_=in_, **kw)
        dst.cnt += 16
        self.ninst += 1
        ins.then_inc(dst.sem, 16)
        self._commit((dst.sem, dst.cnt), [src], [dst])
        if self.sched is not None:
            self.sched.tick()
        return ins

    def barrier(self):
        for e in self.ENG:
            for e2 in self.ENG:
                if e2 != e and self.cnt[e2] > 0:
                    self._wait(e, (self.sem[e2], self.cnt[e2]))
            for b in self.dma_bufs:
                if b.cnt:
                    self._wait(e, (b.sem, b.cnt))


class K:
    pass


def build(NSEQ=2, S=2048, L=4, phases=("mix", "attn", "moe"), MEM=MEM_LEN):
    nc = bass.Bass("TRN2", target_bir_lowering=False)
    T = NSEQ * S
    NT = T // P
    c = Ctx(nc)
    k = K()
    k.nc, k.c, k.NSEQ, k.S, k.L, k.T, k.NT, k.MEM = nc, c, NSEQ, S, L, T, NT, MEM

    def din(name, shape):
        return Buf(nc.dram_tensor(name, list(shape), F32, kind="ExternalInput"), name)

    k.x = din("x", [T, D])
    k.mem = din("mem", [NSEQ * MEM, D])
    k.w_in = din("w_in", [L, D, IN_WIDTH])
    k.hg_lb_logits = din("hg_lb_logits", [DEPTH, 512])
    k.hg_norm_w = din("hg_norm_w", [L, 128])
    k.gd_conv_w = din("gd_conv_w", [L, 4, 1536])
    k.gd_a_log = din("gd_a_log", [L, 4])
    k.gd_dt_bias = din("gd_dt_bias", [L, 4])
    k.gd_norm_w = din("gd_norm_w", [L, 128])
    k.w_out = din("w_out", [L, D, D])
    k.mem_ln_g = din("mem_ln_g", [D])
    k.mem_ln_b = din("mem_ln_b", [D])
    k.w_mq = din("w_mq", [L, D, D])
    k.w_mk = din("w_mk", [L, D, D])
    k.w_mv = din("w_mv", [L, D, D])
    k.w_mo = din("w_mo", [L, D, D])
    k.w_group = din("w_group", [L, D, 4])
    k.b_group = din("b_group", [L, 4])
    k.w_router = din("w_router", [L, D, N_EXP])
    k.b_router = din("b_router", [L, N_EXP])
    k.w_gate = din("w_gate", [L, N_EXP, D, D_EXP])
    k.w_up = din("w_up", [L, N_EXP, D, D_EXP])
    k.w_down = din("w_down", [L, N_EXP, D_EXP, D])
    k.ln_g = din("ln_g", [L, 3, D])
    k.ln_b = din("ln_b", [L, 3, D])
    k.out = Buf(nc.dram_tensor("out", [T, D], F32, kind="ExternalOutput"), "out")
    k.XA = Buf(nc.dram_tensor("XA", [T, D], F32, kind="Internal"), "XA")
    k.XB = Buf(nc.dram_tensor("XB", [T, D], F32, kind="Internal"), "XB")
    k.XTA = Buf(nc.dram_tensor("XTA", [NT, P, KD, P], BF16, kind="Internal"), "XTA")
    k.XTB = Buf(nc.dram_tensor("XTB", [NT, P, KD, P], BF16, kind="Internal"), "XTB")

    def sb(name, shape, dt=F32):
        return Buf(nc.alloc_sbuf_tensor(name, list(shape), dt), name)

    k.idf = sb("idf", [P, P])
    k.idb = sb("idb", [P, P], BF16)
    k.onesf = sb("onesf", [P, P])
    k.onesb = sb("onesb", [P, P], BF16)
    k.cst = sb("cst", [P, 8])
    k.memT = sb("memT", [P, KD, NSEQ * MEM], BF16)
    c.op("pool", lambda e: e.memset(k.idf[:], 0.0), writes=[k.idf])
    c.op("pool", lambda e: e.affine_select(out=k.idf[:], in_=k.idf[:], pattern=[[-1, P]], base=0,
                                           channel_multiplier=1, compare_op=ALU.not_equal, fill=1.0),
         reads=[k.idf], writes=[k.idf])
    c.op("dve", lambda e: e.tensor_copy(out=k.idb[:], in_=k.idf[:]), reads=[k.idf], writes=[k.idb])
    c.op("pool", lambda e: e.memset(k.onesf[:], 1.0), writes=[k.onesf])
    c.op("pool", lambda e: e.memset(k.onesb[:], 1.0), writes=[k.onesb])
    for j, v in enumerate([LN_EPS, RMS_EPS, L2_EPS, 1.0, 0.0]):
        c.op("pool", lambda e: e.memset(k.cst[:, j:j + 1], v), writes=[k.cst])

    k.ep_xo = [sb("ep_xo%d" % i, [P, D]) for i in range(2)]
    k.ep_y = sb("ep_y", [P, D])
    k.ep_xn = [sb("ep_xn%d" % i, [P, D]) for i in range(2)]
    k.ep_xb = sb("ep_xb", [P, D], BF16)
    k.ep_xT = [sb("ep_xT%d" % i, [P, KD, P], BF16) for i in range(2)]
    k.ep_st = sb("ep_st", [P, 2, 6])
    k.ep_mv = sb("ep_mv", [P, 4])
    k.ep_g = sb("ep_g", [P, D])
    k.ep_b = sb("ep_b", [P, D])
    k.ep_n = 0
    k.pT = Buf(nc.alloc_psum_tensor("pT", [P, KD, P], BF16), "pT")
    k.PA = [Buf(nc.alloc_psum_tensor("PA0", [P, 2, 512], F32), "PA0")]

    prologue(k)
    if "mix" in phases:
        mix_consts(k)
    xin, xtin = k.x, k.XTA
    cur = 0
    xs = [k.XA, k.XB]
    xts = [k.XTB, k.XTA]
    for l in range(L):
        for pi, ph in enumerate(("mix", "attn", "moe")):
            if ph not in phases:
                continue
            last = (l == L - 1) and ph == [p_ for p_ in ("mix", "attn", "moe") if p_ in phases][-1]
            xout = k.out if last else xs[cur]
            xtout = xts[cur]
            c.barrier()
            load_ln(k, l, pi)
            if ph == "mix":
                phase_mix(k, l, xin, xtin, xout, xtout)
            elif ph == "attn":
                phase_attn(k, l, xin, xtin, xout, xtout)
            else:
                phase_moe(k, l, xin, xtin, xout, xtout)
            xin, xtin = xout, xtout
            cur ^= 1
    c.barrier()
    return nc


def load_ln(k, l, idx):
    c = k.c
    c.dma("sp", k.ep_g[:], k.ln_g[l, idx, :].partition_broadcast(P), k.ep_g, k.ln_g)
    c.dma("sp", k.ep_b[:], k.ln_b[l, idx, :].partition_broadcast(P), k.ep_b, k.ln_b)


def ln_rows(k, src_ap, src_buf, dst, g, b, mv, st):
    c = k.c
    for h in range(2):
        c.op("dve", lambda e: e.bn_stats(out=st[:, h, :], in_=src_ap[:, h * 512:(h + 1) * 512]),
             reads=[src_buf], writes=[st])
    c.op("dve", lambda e: e.bn_aggr(out=mv[:, 0:2], in_=st[:].rearrange("p a b -> p (a b)")),
         reads=[st], writes=[mv])
    c.op("act", lambda e: e.activation(out=mv[:, 2:3], in_=mv[:, 1:2], func=AF.Ln, bias=k.cst[:, 0:1]),
         reads=[mv, k.cst], writes=[mv])
    c.op("act", lambda e: e.activation(out=mv[:, 3:4], in_=mv[:, 2:3], func=AF.Exp, scale=-0.5), reads=[mv], writes=[mv])
    c.op("dve", lambda e: e.tensor_scalar(out=dst[:], in0=src_ap, scalar1=mv[:, 0:1], scalar2=mv[:, 3:4],
                                          op0=ALU.subtract, op1=ALU.mult),
         reads=[src_buf, mv], writes=[dst])
    c.op("pool", lambda e: e.tensor_mul(out=dst[:], in0=dst[:], in1=g[:]), reads=[dst, g], writes=[dst])
    c.op("pool", lambda e: e.tensor_add(out=dst[:], in0=dst[:], in1=b[:]), reads=[dst, b], writes=[dst])


def to_xT(k, xn, ti, xtout):
    c = k.c
    n = k.ep_n
    xT = k.ep_xT[n % 2]
    c.op("act", lambda e: e.activation(out=k.ep_xb[:], in_=xn[:], func=AF.Copy), reads=[xn], writes=[k.ep_xb])
    for kk in range(KD):
        c.op("pe", lambda e: e.transpose(out=k.pT[:, kk, :], in_=k.ep_xb[:, kk * P:(kk + 1) * P], identity=k.idb[:]),
             reads=[k.ep_xb, k.idb], writes=[k.pT])
    c.op("dve", lambda e: e.tensor_copy(out=xT[:], in_=k.pT[:]), reads=[k.pT], writes=[xT])
    c.dma("sp", xtout[ti], xT[:], xtout, xT)


def epilogue(k, sub_ap, sub_buf, ti, xin, xout, xtout):
    c = k.c
    n = k.ep_n
    xo = k.ep_xo[n % 2]
    xn = k.ep_xn[n % 2]
    c.dma("sp", xo[:], xin[ti * P:(ti + 1) * P, :], xo, xin)
    c.op("dve", lambda e: e.scalar_tensor_tensor(out=k.ep_y[:], in0=xo[:], scalar=ALPHA, in1=sub_ap,
                                                 op0=ALU.mult, op1=ALU.add),
         reads=[xo, sub_buf], writes=[k.ep_y])
    ln_rows(k, k.ep_y[:], k.ep_y, xn, k.ep_g, k.ep_b, k.ep_mv, k.ep_st)
    c.dma("sp", xout[ti * P:(ti + 1) * P, :], xn[:], xout, xn)
    to_xT(k, xn, ti, xtout)
    k.ep_n += 1


def prologue(k):
    c, nc = k.c, k.nc
    for ti in range(k.NT):
        xn = k.ep_xn[k.ep_n % 2]
        c.dma("sp", xn[:], k.x[ti * P:(ti + 1) * P, :], xn, k.x)
        to_xT(k, xn, ti, k.XTA)
        k.ep_n += 1
    c.dma("sp", k.ep_g[:], k.mem_ln_g[:].partition_broadcast(P), k.ep_g, k.mem_ln_g)
    c.dma("sp", k.ep_b[:], k.mem_ln_b[:].partition_broadcast(P), k.ep_b, k.mem_ln_b)
    for mi in range(k.NSEQ * k.MEM // P):
        xo = k.ep_xo[mi % 2]
        xn = k.ep_xn[mi % 2]
        c.dma("sp", xo[:], k.mem[mi * P:(mi + 1) * P, :], xo, k.mem)
        ln_rows(k, xo[:], xo, xn, k.ep_g, k.ep_b, k.ep_mv, k.ep_st)
        c.op("act", lambda e: e.activation(out=k.ep_xb[:], in_=xn[:], func=AF.Copy), reads=[xn], writes=[k.ep_xb])
        for kk in range(KD):
            c.op("pe", lambda e: e.transpose(out=k.pT[:, kk, :], in_=k.ep_xb[:, kk * P:(kk + 1) * P], identity=k.idb[:]),
                 reads=[k.ep_xb, k.idb], writes=[k.pT])
        c.op("dve", lambda e: e.tensor_copy(out=k.memT[:, :, mi * P:(mi + 1) * P], in_=k.pT[:]),
             reads=[k.pT], writes=[k.memT])


def load_xT(k, dst, xtin, t0, n):
    c = k.c
    nt = n // P
    src = xtin[t0 // P:t0 // P + nt].rearrange("i p k t -> p i k t")
    for i in range(nt):
        c.dma("sp", dst[:, :, i * P:(i + 1) * P], xtin[t0 // P + i], dst, xtin)


def load_w_bf(k, dst, dram_ap, src_buf):
    k.c.dma("pool", dst[:], dram_ap.rearrange("(k p) n -> p k n", p=P), dst, src_buf)


def phase_attn(k, l, xin, xtin, xout, xtout):
    c, nc = k.c, k.nc
    MEM, NSEQ, S = k.MEM, k.NSEQ, k.S
    MC = MEM // P
    with ExitStack() as es_:
        def sbt(name, shape, dt=F32):
            return Buf(es_.enter_context(nc.sbuf_tensor("%s_L%d" % (name, l), list(shape), dt)), name)

        def pst(name, shape, dt=F32):
            return Buf(es_.enter_context(nc.psum_tensor("%s_L%d" % (name, l), list(shape), dt)), name)

        wq, wo = sbt("a_wq", [P, KD, D], BF16), sbt("a_wo", [P, KD, D], BF16)
        kT = sbt("a_kT", [P, NSEQ, KD, MEM], BF16)
        v = sbt("a_v", [P, NSEQ * MC, D], BF16)
        xTs = [sbt("a_xT0", [P, KD, 512], BF16), sbt("a_xT1", [P, KD, 512], BF16)]
        qT = sbt("a_qT", [P, KD, 512], BF16)
        es = [sbt("a_e0", [P, MC, 512], BF16), sbt("a_e1", [P, MC, 512], BF16)]
        rs = sbt("a_rs", [P, 512], F32)
        oT = sbt("a_oT", [P, KD, 512], BF16)
        ps = [pst("a_p0", [P, 512]), pst("a_p1", [P, 512]), pst("a_p2", [P, 512])]
        PAs = [k.PA[0], pst("a_PA1", [P, 2, 512])]
        pn = [0]

        def nextp():
            pn[0] += 1
            return ps[pn[0] % 3]

        load_w_bf(k, wq, k.w_mk[l], k.w_mk)
        load_w_bf(k, wo, k.w_mv[l], k.w_mv)
        for s in range(NSEQ):
            for fb in range(KD):
                pp = nextp()
                for kk in range(KD):
                    c.op("pe", lambda e: e.matmul(pp[:, 0:MEM], lhsT=wq[:, kk, fb * P:(fb + 1) * P],
                                                  rhs=k.memT[:, kk, s * MEM:(s + 1) * MEM],
                                                  start=(kk == 0), stop=(kk == KD - 1)),
                         reads=[wq, k.memT], writes=[pp])
                c.op("act", lambda e: e.activation(out=kT[:, s, fb, :], in_=pp[:, 0:MEM], func=AF.Copy, scale=1.0 / 16.0),
                     reads=[pp], writes=[kT])
            for mc in range(MC):
                for half in range(2):
                    pp = nextp()
                    for kk in range(KD):
                        c.op("pe", lambda e: e.matmul(pp[:], lhsT=k.memT[:, kk, s * MEM + mc * P:s * MEM + (mc + 1) * P],
                                                      rhs=wo[:, kk, half * 512:(half + 1) * 512],
                                                      start=(kk == 0), stop=(kk == KD - 1)),
                             reads=[wo, k.memT], writes=[pp])
                    c.op("dve", lambda e: e.tensor_copy(out=v[:, s * MC + mc, half * 512:(half + 1) * 512], in_=pp[:]),
                         reads=[pp], writes=[v])
        load_w_bf(k, wq, k.w_mq[l], k.w_mq)
        load_w_bf(k, wo, k.w_mo[l], k.w_mo)
        nst = k.T // 512
        for st in range(nst):
            s = (st * 512) // S
            xT = xTs[st % 2]
            load_xT(k, xT, xtin, st * 512, 512)
            for fb in range(KD):
                pp = nextp()
                for kk in range(KD):
                    c.op("pe", lambda e: e.matmul(pp[:], lhsT=wq[:, kk, fb * P:(fb + 1) * P], rhs=xT[:, kk, :],
                                                  start=(kk == 0), stop=(kk == KD - 1)),
                         reads=[wq, xT], writes=[pp])
                if fb % 2 == 0:
                    c.op("act", lambda e: e.activation(out=qT[:, fb, :], in_=pp[:], func=AF.Copy), reads=[pp], writes=[qT])
                else:
                    c.op("dve", lambda e: e.tensor_copy(out=qT[:, fb, :], in_=pp[:]), reads=[pp], writes=[qT])
            for h in range(4):
                ee = es[h % 2]
                for mc in range(MC):
                    pp = nextp()
                    for j in range(2):
                        c.op("pe", lambda e: e.matmul(pp[:], lhsT=kT[:, s, 2 * h + j, mc * P:(mc + 1) * P],
                                                      rhs=qT[:, 2 * h + j, :], start=(j == 0), stop=(j == 1)),
                             reads=[kT, qT], writes=[pp])
                    c.op("act", lambda e: e.activation(out=ee[:, mc, :], in_=pp[:], func=AF.Exp), reads=[pp], writes=[ee])
                pp = nextp()
                for mc in range(MC):
                    c.op("pe", lambda e: e.matmul(pp[:], lhsT=k.onesb[:], rhs=ee[:, mc, :], start=(mc == 0), stop=(mc == MC - 1)),
                         reads=[k.onesb, ee], writes=[pp])
                c.op("dve", lambda e: e.reciprocal(out=rs[:], in_=pp[:]), reads=[pp], writes=[rs])
                for j in range(2):
                    pp = nextp()
                    for mc in range(MC):
                        c.op("pe", lambda e: e.matmul(pp[:], lhsT=v[:, s * MC + mc, (2 * h + j) * P:(2 * h + j + 1) * P],
                                                      rhs=ee[:, mc, :], start=(mc == 0), stop=(mc == MC - 1)),
                             reads=[v, ee], writes=[pp])
                    c.op("dve", lambda e: e.tensor_tensor(out=oT[:, 2 * h + j, :], in0=pp[:], in1=rs[:], op=ALU.mult),
                         reads=[pp, rs], writes=[oT])
            for i in range(4):
                pa = PAs[(st * 4 + i) % 2]
                for half in range(2):
                    for fb in range(KD):
                        c.op("pe", lambda e: e.matmul(pa[:, half, :], lhsT=oT[:, fb, i * P:(i + 1) * P],
                                                      rhs=wo[:, fb, half * 512:(half + 1) * 512],
                                                      start=(fb == 0), stop=(fb == KD - 1)),
                             reads=[oT, wo], writes=[pa])
                epilogue(k, pa[:].rearrange("p a b -> p (a b)"), pa, st * 4 + i, xin, xout, xtout)
        c.barrier()


def phase_moe(k, l, xin, xtin, xout, xtout):
    c, nc = k.c, k.nc
    TB = min(1024, k.T)
    NTB = TB // P
    with ExitStack() as es_:
        def sbt(name, shape, dt=F32):
            return Buf(es_.enter_context(nc.sbuf_tensor("%s_L%d" % (name, l), list(shape), dt)), name)

        def pst(name, shape, dt=F32):
            return Buf(es_.enter_context(nc.psum_tensor("%s_L%d" % (name, l), list(shape), dt)), name)

        xT = sbt("m_xT", [P, KD, TB], BF16)
        yacc = sbt("m_yacc", [P, NTB, D], F32)
        wgs = [sbt("m_wg0", [P, KD, D_EXP], BF16), sbt("m_wg1", [P, KD, D_EXP], BF16)]
        wus = [sbt("m_wu0", [P, KD, D_EXP], BF16), sbt("m_wu1", [P, KD, D_EXP], BF16)]
        wds = [sbt("m_wd0", [P, 4, D], BF16), sbt("m_wd1", [P, 4, D], BF16)]
        wr = sbt("m_wr", [P, KD, 36], BF16)
        br, lg, sm = sbt("m_br", [P, 36]), sbt("m_lg", [P, 36]), sbt("m_sm", [P, 16])
        t0, t1, t2 = sbt("m_t0", [P, 32]), sbt("m_t1", [P, 32]), sbt("m_t2", [P, 32])
        gate = sbt("m_gate", [P, NTB, 32])
        sg = sbt("m_sg", [P, 512])
        hTs = [sbt("m_hT0", [P, 4, 512], BF16), sbt("m_hT1", [P, 4, 512], BF16)]
        ps = [pst("m_p0", [P, 512]), pst("m_p1", [P, 512]), pst("m_p2", [P, 512])]
        PAs = [k.PA[0], pst("m_PA1", [P, 2, 512])]
        pn = [0]

        def nextp():
            pn[0] += 1
            return ps[pn[0] % 3]

        c.dma("pool", wr[:, :, 0:4], k.w_group[l].rearrange("(k p) n -> p k n", p=P), wr, k.w_group)
        c.dma("pool", wr[:, :, 4:36], k.w_router[l].rearrange("(k p) n -> p k n", p=P), wr, k.w_router)
        c.dma("sp", br[:, 0:4], k.b_group[l, :].partition_broadcast(P), br, k.b_group)
        c.dma("sp", br[:, 4:36], k.b_router[l, :].partition_broadcast(P), br, k.b_router)
        for blk in range(k.T // TB):
            tb0 = blk * TB
            load_xT(k, xT, xtin, tb0, TB)
            for ti in range(NTB):
                pp = nextp()
                for kk in range(KD):
                    c.op("pe", lambda e: e.matmul(pp[:, 0:36], lhsT=xT[:, kk, ti * P:(ti + 1) * P], rhs=wr[:, kk, :],
                                                  start=(kk == 0), stop=(kk == KD - 1)), reads=[xT, wr], writes=[pp])
                c.op("dve", lambda e: e.tensor_tensor(out=lg[:], in0=pp[:, 0:36], in1=br[:], op=ALU.add),
                     reads=[pp, br], writes=[lg])
                c.op("dve", lambda e: e.reduce_max(out=sm[:, 0:1], in_=lg[:, 0:4], axis=AX.X), reads=[lg], writes=[sm])
                c.op("dve", lambda e: e.tensor_scalar(out=t0[:, 0:4], in0=lg[:, 0:4], scalar1=sm[:, 0:1], scalar2=None,
                                                      op0=ALU.is_equal), reads=[lg, sm], writes=[t0])
                c.op("dve", lambda e: e.tensor_scalar(out=sm[:, 1:2], in0=sm[:, 0:1], scalar1=-1.0, scalar2=None,
                                                      op0=ALU.mult), reads=[sm], writes=[sm])
                c.op("act", lambda e: e.activation(out=t1[:, 0:4], in_=lg[:, 0:4], func=AF.Exp, bias=sm[:, 1:2],
                                                   accum_out=sm[:, 2:3]), reads=[lg, sm], writes=[t1, sm])
                c.op("dve", lambda e: e.reciprocal(out=sm[:, 3:4], in_=sm[:, 2:3]), reads=[sm], writes=[sm])
                c.op("dve", lambda e: e.tensor_scalar(out=t0[:, 4:8], in0=t0[:, 0:4], scalar1=-1.0, scalar2=BIG,
                                                      op0=ALU.add, op1=ALU.mult), reads=[t0], writes=[t0])
                c.op("dve", lambda e: e.tensor_tensor(out=t1[:].rearrange("p (g e) -> p g e", g=4),
                                                      in0=lg[:, 4:36].rearrange("p (g e) -> p g e", g=4),
                                                      in1=t0[:, 4:8].unsqueeze(2).to_broadcast([P, 4, 8]), op=ALU.add),
                     reads=[lg, t0], writes=[t1])
                c.op("dve", lambda e: e.reduce_max(out=sm[:, 4:5], in_=t1[:], axis=AX.X), reads=[t1], writes=[sm])
                c.op("dve", lambda e: e.tensor_scalar(out=t0[:], in0=t1[:], scalar1=sm[:, 4:5], scalar2=None,
                                                      op0=ALU.is_equal), reads=[t1, sm], writes=[t0])
                c.op("dve", lambda e: e.scalar_tensor_tensor(out=t1[:], in0=t0[:], scalar=-BIG, in1=t1[:],
                                                             op0=ALU.mult, op1=ALU.add), reads=[t0, t1], writes=[t1])
                c.op("dve", lambda e: e.reduce_max(out=sm[:, 5:6], in_=t1[:], axis=AX.X), reads=[t1], writes=[sm])
                c.op("dve", lambda e: e.tensor_scalar(out=t2[:], in0=t1[:], scalar1=sm[:, 5:6], scalar2=None,
                                                      op0=ALU.is_equal), reads=[t1, sm], writes=[t2])
                c.op("dve", lambda e: e.tensor_tensor(out=sm[:, 6:7], in0=sm[:, 5:6], in1=sm[:, 4:5], op=ALU.subtract),
                     reads=[sm], writes=[sm])
                c.op("act", lambda e: e.activation(out=sm[:, 7:8], in_=sm[:, 6:7], func=AF.Exp), reads=[sm], writes=[sm])
                c.op("dve", lambda e: e.tensor_scalar(out=sm[:, 7:8], in0=sm[:, 7:8], scalar1=1.0, scalar2=None, op0=ALU.add),
                     reads=[sm], writes=[sm])
                c.op("dve", lambda e: e.reciprocal(out=sm[:, 7:8], in_=sm[:, 7:8]), reads=[sm], writes=[sm])
                c.op("dve", lambda e: e.tensor_tensor(out=sm[:, 8:9], in0=sm[:, 7:8], in1=sm[:, 3:4], op=ALU.mult),
                     reads=[sm], writes=[sm])
                c.op("dve", lambda e: e.tensor_tensor(out=sm[:, 9:10], in0=sm[:, 3:4], in1=sm[:, 8:9], op=ALU.subtract),
                     reads=[sm], writes=[sm])
                c.op("dve", lambda e: e.tensor_scalar(out=t0[:], in0=t0[:], scalar1=sm[:, 8:9], scalar2=None, op0=ALU.mult),
                     reads=[t0, sm], writes=[t0])
                c.op("dve", lambda e: e.scalar_tensor_tensor(out=gate[:, ti, :], in0=t2[:], scalar=sm[:, 9:10], in1=t0[:],
                                                             op0=ALU.mult, op1=ALU.add), reads=[t2, sm, t0], writes=[gate])
            for ex in range(N_EXP):
                wg, wu, wd = wgs[ex % 2], wus[ex % 2], wds[ex % 2]
                load_w_bf(k, wg, k.w_gate[l, ex], k.w_gate)
                load_w_bf(k, wu, k.w_up[l, ex], k.w_up)
                load_w_bf(k, wd, k.w_down[l, ex], k.w_down)
                for j in range(TB // 512):
                    hT = hTs[j % 2]
                    for fb in range(4):
                        pg = nextp()
                        for kk in range(KD):
                            c.op("pe", lambda e: e.matmul(pg[:], lhsT=wg[:, kk, fb * P:(fb + 1) * P],
                                                          rhs=xT[:, kk, j * 512:(j + 1) * 512],
                                                          start=(kk == 0), stop=(kk == KD - 1)), reads=[wg, xT], writes=[pg])
                        c.op("act", lambda e: e.activation(out=sg[:], in_=pg[:], func=AF.Silu), reads=[pg], writes=[sg])
                        pu = nextp()
                        for kk in range(KD):
                            c.op("pe", lambda e: e.matmul(pu[:], lhsT=wu[:, kk, fb * P:(fb + 1) * P],
                                                          rhs=xT[:, kk, j * 512:(j + 1) * 512],
                                                          start=(kk == 0), stop=(kk == KD - 1)), reads=[wu, xT], writes=[pu])
                        c.op("dve", lambda e: e.tensor_tensor(out=hT[:, fb, :], in0=pu[:], in1=sg[:], op=ALU.mult),
                             reads=[pu, sg], writes=[hT])
                    for i in range(4):
                        ti = j * 4 + i
                        pa = PAs[ti % 2]
                        for half in range(2):
                            for fb in range(4):
                                c.op("pe", lambda e: e.matmul(pa[:, half, :], lhsT=hT[:, fb, i * P:(i + 1) * P],
                                                              rhs=wd[:, fb, half * 512:(half + 1) * 512],
                                                              start=(fb == 0), stop=(fb == 3)), reads=[hT, wd], writes=[pa])
                        pav = pa[:].rearrange("p a b -> p (a b)")
                        if ex == 0:
                            c.op("dve", lambda e: e.tensor_scalar(out=yacc[:, ti, :], in0=pav, scalar1=gate[:, ti, ex:ex + 1],
                                                                  scalar2=None, op0=ALU.mult),
                                 reads=[pa, gate], writes=[yacc])
                        else:
                            c.op("dve", lambda e: e.scalar_tensor_tensor(out=yacc[:, ti, :], in0=pav,
                                                                         scalar=gate[:, ti, ex:ex + 1], in1=yacc[:, ti, :],
                                                                         op0=ALU.mult, op1=ALU.add),
                                 reads=[pa, gate, yacc], writes=[yacc])
            for ti in range(NTB):
                epilogue(k, yacc[:, ti, :], yacc, tb0 // P + ti, xin, xout, xtout)
        c.barrier()


def mix_consts(k):
    c, nc = k.c, k.nc

    def sb(name, shape, dt=F32):
        return Buf(nc.alloc_sbuf_tensor(name, list(shape), dt), name)

    k.lb = sb("lb", [P, DEPTH, 4])
    k.oml = sb("oml", [P, DEPTH, 4])
    k.mk_incl = sb("mk_incl", [64, 4, 64])
    k.mk_strict = sb("mk_strict", [64, 4, 64])
    k.ones256 = sb("ones256", [P, 256])
    lgt = sb("lgt", [P, DEPTH, 4])
    tmp = sb("lbtmp", [P, 4])
    with nc.allow_non_contiguous_dma(reason="tiny param transposes"):
        c.dma("sp", lgt[:], k.hg_lb_logits[:, :].rearrange("l (h d) -> d l h", h=4), lgt, k.hg_lb_logits)
    c.op("dve", lambda e: e.tensor_tensor(out=tmp[:], in0=lgt[:, 0, :], in1=lgt[:, 1, :], op=ALU.max), reads=[lgt], writes=[tmp])
    c.op("dve", lambda e: e.tensor_tensor(out=tmp[:], in0=tmp[:], in1=lgt[:, 2, :], op=ALU.max), reads=[lgt, tmp], writes=[tmp])
    c.op("dve", lambda e: e.tensor_tensor(out=tmp[:], in0=tmp[:], in1=lgt[:, 3, :], op=ALU.max), reads=[lgt, tmp], writes=[tmp])
    c.op("dve", lambda e: e.tensor_tensor(out=lgt[:], in0=lgt[:], in1=tmp[:].unsqueeze(1).to_broadcast([P, DEPTH, 4]),
                                          op=ALU.subtract), reads=[lgt, tmp], writes=[lgt])
    c.op("act", lambda e: e.activation(out=lgt[:], in_=lgt[:], func=AF.Exp), reads=[lgt], writes=[lgt])
    c.op("dve", lambda e: e.tensor_tensor(out=tmp[:], in0=lgt[:, 0, :], in1=lgt[:, 1, :], op=ALU.add), reads=[lgt], writes=[tmp])
    c.op("dve", lambda e: e.tensor_tensor(out=tmp[:], in0=tmp[:], in1=lgt[:, 2, :], op=ALU.add), reads=[lgt, tmp], writes=[tmp])
    c.op("dve", lambda e: e.tensor_tensor(out=tmp[:], in0=tmp[:], in1=lgt[:, 3, :], op=ALU.add), reads=[lgt, tmp], writes=[tmp])
    c.op("dve", lambda e: e.reciprocal(out=tmp[:], in_=tmp[:]), reads=[tmp], writes=[tmp])
    c.op("dve", lambda e: e.tensor_tensor(out=lgt[:], in0=lgt[:], in1=tmp[:].unsqueeze(1).to_broadcast([P, DEPTH, 4]),
                                          op=ALU.mult), reads=[lgt, tmp], writes=[lgt])
    c.op("dve", lambda e: e.memset(k.lb[:, 0, :], 0.0), writes=[k.lb])
    c.op("dve", lambda e: e.tensor_copy(out=k.lb[:, 1, :], in_=lgt[:, 1, :]), reads=[lgt], writes=[k.lb])
    c.op("dve", lambda e: e.tensor_tensor(out=k.lb[:, 2, :], in0=k.lb[:, 1, :], in1=lgt[:, 2, :], op=ALU.add),
         reads=[lgt, k.lb], writes=[k.lb])
    c.op("dve", lambda e: e.tensor_tensor(out=k.lb[:, 3, :], in0=k.lb[:, 2, :], in1=lgt[:, 3, :], op=ALU.add),
         reads=[lgt, k.lb], writes=[k.lb])
    c.op("dve", lambda e: e.tensor_scalar(out=k.oml[:], in0=k.lb[:], scalar1=-1.0, scalar2=1.0, op0=ALU.mult, op1=ALU.add),
         reads=[k.lb], writes=[k.oml])
    for mk, op_ in ((k.mk_incl, ALU.is_ge), (k.mk_strict, ALU.is_gt)):
        c.op("pool", lambda e: e.memset(mk[:], 1.0), writes=[mk])
        c.op("pool", lambda e: e.affine_select(out=mk[:], in_=mk[:], pattern=[[0, 4], [1, 64]], base=0,
                                               channel_multiplier=-1, compare_op=op_, fill=0.0),
             reads=[mk], writes=[mk])
    c.op("pool", lambda e: e.memset(k.ones256[:], 1.0), writes=[k.ones256])


class Sched:
    def __init__(self):
        import threading
        self.th = threading
        self.cv = threading.Condition()
        self.live = []
        self.turn = None
        self.posted = set()
        self.err = None
        self.spin = 0

    def tick(self):
        me = self.th.get_ident()
        if me not in self.live:
            return
        with self.cv:
            if len(self.live) > 1:
                idx = self.live.index(me)
                self.turn = self.live[(idx + 1) % len(self.live)]
                self.cv.notify_all()
                while self.turn != me and self.err is None:
                    self.cv.wait()
            if self.err is not None and self.th.get_ident() != self.err[0]:
                raise RuntimeError("sibling stream failed")

    def post(self, key):
        self.posted.add(key)

    def need(self, key):
        n = 0
        while key not in self.posted:
            n += 1
            if n > 200000:
                raise RuntimeError("emission deadlock on %r" % (key,))
            self.tick()

    def run(self, fns):
        def body(fn):
            me = self.th.get_ident()
            with self.cv:
                while self.turn != me and self.err is None:
                    self.cv.wait()
            try:
                if self.err is None:
                    fn()
            except BaseException as ex:
                with self.cv:
                    if self.err is None:
                        self.err = (me, ex)
            finally:
                with self.cv:
                    idx = self.live.index(me)
                    self.live.remove(me)
                    if self.live:
                        self.turn = self.live[idx % len(self.live)]
                    self.cv.notify_all()

        import os as _os
        if _os.environ.get("SEQ_EMIT"):
            self.need = lambda key: None
            for fn in fns:
                fn()
            return
        ths = [self.th.Thread(target=body, args=(fn,)) for fn in fns]
        with self.cv:
            for t in ths:
                t.start()
            self.live = [t.ident for t in ths]
            self.turn = self.live[0]
            self.cv.notify_all()
        for t in ths:
            t.join()
        if self.err is not None:
            raise self.err[1]


def phase_mix(k, l, xin, xtin, xout, xtout):
    c, nc = k.c, k.nc
    S, NSEQ = k.S, k.NSEQ
    ST = 256
    NCH = ST // 64
    sch = Sched()
    c.sched = sch
    with ExitStack() as es_:
        def sbt(name, shape, dt=F32):
            return Buf(es_.enter_context(nc.sbuf_tensor("%s_L%d" % (name, l), list(shape), dt)), name)

        def pst(name, shape, dt=F32):
            return Buf(es_.enter_context(nc.psum_tensor("%s_L%d" % (name, l), list(shape), dt)), name)

        class Pool_:
            def __init__(self, bufs):
                self.b, self.n = bufs, 0

            def next(self):
                self.n += 1
                return self.b[self.n % len(self.b)]

        win = sbt("x_win", [P, KD, IN_WIDTH], BF16)
        wout = sbt("x_wout", [P, KD, D], BF16)
        xTs = [sbt("x_xT0", [P, KD, ST], BF16), sbt("x_xT1", [P, KD, ST], BF16)]
        yTs = [sbt("x_yT0", [P, 8, ST], BF16), sbt("x_yT1", [P, 8, ST], BF16)]
        nw = sbt("x_nw", [P, 2])
        cw = sbt("x_cw", [P, 12, 4])
        dtb = sbt("x_dtb", [P, 4])
        negA = sbt("x_negA", [P, 4])
        fa, flf, fk, fcum, fd, fe = (sbt("x_fa", [P, ST]), sbt("x_flf", [P, ST]), sbt("x_fk", [P, ST]),
                                     sbt("x_fcum", [P, ST]), sbt("x_fd", [P, ST]), sbt("x_fe", [P, ST]))
        qt, kt = sbt("x_qt", [P, ST], BF16), sbt("x_kt", [P, ST], BF16)
        Vt = sbt("x_Vt", [64, NCH, 512], BF16)
        hs = sbt("x_hs", [P, 16])
        hsc = sbt("x_hsc", [P, 12])
        AT = sbt("x_AT", [64, 64], BF16)
        ktok = sbt("x_ktok", [64, P], BF16)
        Ssc = sbt("x_Ssc", [P, P], BF16)
        Sh = sbt("x_Sh", [P, 4, P])
        Sg = sbt("x_Sg", [P, 4, P])
        Sgb = sbt("x_Sgb", [P, 4, P], BF16)
        tmpS = sbt("x_tmpS", [P, P])
        osb = sbt("x_osb", [P, ST])
        rsH = (sbt("x_sqH", [P, ST]), sbt("x_rstH", [P, ST]), sbt("x_gzH", [P, ST]))
        rsG = (sbt("x_sqG", [P, ST]), sbt("x_rstG", [P, ST]), sbt("x_gzG", [P, ST]))
        sqC, rstC = sbt("x_sqC", [P, ST]), sbt("x_rstC", [P, ST])
        hist = sbt("x_hist", [P, 12, 3])
        xp = sbt("x_xp", [P, ST + 3])
        yc = sbt("x_yc", [P, ST])
        ys = sbt("x_ys", [P, ST])
        gqT = sbt("x_gqT", [P, 4, ST], BF16)
        gkT = sbt("x_gkT", [P, 4, ST], BF16)
        gvF = sbt("x_gvF", [P, 4, ST], BF16)
        og = sbt("x_og", [P, 4, ST])
        Dg = sbt("x_Dg", [64, 4, 64])
        expR = sbt("x_expR", [P, 4, 64], BF16)
        decS = sbt("x_decS", [64, 4, 64])
        decI = sbt("x_decI", [64, 4, 64])
        ABt = [sbt("x_AB0", [64, 8, 64]), sbt("x_AB1", [64, 8, 64])]
        Ab = [Buf(ABt[0].t[:, 0:4, :]), Buf(ABt[1].t[:, 0:4, :])]
        Bb = [Buf(ABt[0].t[:, 4:8, :]), Buf(ABt[1].t[:, 4:8, :])]
        Pb = [sbt("x_P0", [64, 4, 64]), sbt("x_P1", [64, 4, 64])]
        vtok = sbt("x_vtok", [64, 4, P], BF16)
        ktk = sbt("x_ktk", [64, 4, P], BF16)
        ke = sbt("x_ke", [64, 4, P], BF16)
        vnew = sbt("x_vnew", [64, P], BF16)
        pre = []
        for i in range(2):
            pre.append(dict(
                ga=sbt("x_ga%d" % i, [64, 16]), gb=sbt("x_gb%d" % i, [64, 16]), ecl=sbt("x_ecl%d" % i, [P, 8]),
                qeT=sbt("x_qeT%d" % i, [P, 4, 64], BF16), qkT=sbt("x_qkT%d" % i, [64, 4, 64], BF16),
                TT=sbt("x_TT%d" % i, [64, 4, 64], BF16), kdec=sbt("x_kdec%d" % i, [64, 4, P], BF16),
                ub=sbt("x_ub%d" % i, [64, 4, P]), wT=sbt("x_wT%d" % i, [P, 4, 64], BF16)))
        hP = Pool_([pst("x_hp0", [P, 512]), pst("x_hp1", [P, 512])])
        gP = Pool_([pst("x_gp0", [P, 512]), pst("x_gp1", [P, 512])])
        sP = Pool_([pst("x_sp0", [P, 512])])
        idf64 = k.idf[0:64, 0:64]

        load_w_bf(k, win, k.w_in[l], k.w_in)
        load_w_bf(k, wout, k.w_out[l], k.w_out)
        with nc.allow_non_contiguous_dma(reason="tiny param transposes"):
            c.dma("sp", nw[:, 0:1], k.hg_norm_w[l:l + 1, :].rearrange("o v -> v o"), nw, k.hg_norm_w)
            c.dma("sp", nw[:, 1:2], k.gd_norm_w[l:l + 1, :].rearrange("o v -> v o"), nw, k.gd_norm_w)
            for j in range(4):
                c.dma("sp", cw[:, :, j], k.gd_conv_w[l, j, :].rearrange("(b c) -> c b", c=P), cw, k.gd_conv_w)
        c.dma("sp", dtb[:], k.gd_dt_bias[l, :].partition_broadcast(P), dtb, k.gd_dt_bias)
        c.dma("sp", negA[:], k.gd_a_log[l, :].partition_broadcast(P), negA, k.gd_a_log)
        c.op("act", lambda e: e.activation(out=negA[:], in_=negA[:], func=AF.Exp), reads=[negA], writes=[negA])
        c.op("dve", lambda e: e.tensor_scalar(out=negA[:], in0=negA[:], scalar1=-1.0, scalar2=None, op0=ALU.mult),
             reads=[negA], writes=[negA])

        def sigm(dst, dst_buf, src_ap, src_buf):
            c.op("act", lambda e: e.activation(out=dst, in_=src_ap, func=AF.Exp, scale=-1.0), reads=[src_buf], writes=[dst_buf])
            c.op("act", lambda e: e.activation(out=dst, in_=dst, func=AF.Ln, bias=k.cst[0:dst.shape[0], 3:4]),
                 reads=[dst_buf, k.cst], writes=[dst_buf])
            c.op("act", lambda e: e.activation(out=dst, in_=dst, func=AF.Exp, scale=-1.0), reads=[dst_buf], writes=[dst_buf])

        def proj_F(col0, xT, pool):
            pp = pool.next()
            for kk in range(KD):
                c.op("pe", lambda e: e.matmul(pp[:, 0:ST], lhsT=win[:, kk, col0:col0 + P], rhs=xT[:, kk, :],
                                              start=(kk == 0), stop=(kk == KD - 1)), reads=[win, xT], writes=[pp])
            return pp

        def rms_gate(o_ap, o_buf, gate_col0, silu, nwcol, ydst, yT, xT, pool, rs):
            sq, rst, gz = rs
            c.op("act", lambda e: e.activation(out=sq[:], in_=o_ap, func=AF.Square), reads=[o_buf], writes=[sq])
            pm = pool.next()
            c.op("pe", lambda e: e.matmul(pm[:, 0:ST], lhsT=k.onesf[:], rhs=sq[:], start=True, stop=True),
                 reads=[k.onesf, sq], writes=[pm])
            c.op("act", lambda e: e.activation(out=rst[:], in_=pm[:, 0:ST], func=AF.Ln, bias=k.cst[:, 1:2], scale=1.0 / 128.0),
                 reads=[pm, k.cst], writes=[rst])
            c.op("act", lambda e: e.activation(out=rst[:], in_=rst[:], func=AF.Exp, scale=-0.5), reads=[rst], writes=[rst])
            pz = proj_F(gate_col0, xT, pool)
            sigm(gz[:], gz, pz[:, 0:ST], pz)
            if silu:
                c.op("dve", lambda e: e.tensor_tensor(out=gz[:], in0=pz[:, 0:ST], in1=gz[:], op=ALU.mult), reads=[pz, gz], writes=[gz])
            c.op("pool", lambda e: e.tensor_mul(out=rst[:], in0=rst[:], in1=gz[:]), reads=[rst, gz], writes=[rst])
            c.op("dve", lambda e: e.scalar_tensor_tensor(out=ydst, in0=o_ap, scalar=nw[:, nwcol:nwcol + 1], in1=rst[:],
                                                         op0=ALU.mult, op1=ALU.mult), reads=[o_buf, nw, rst], writes=[yT])

        def hgrn(xT, yT):
            for ch in range(NCH):
                pp = hP.next()
                for kk in range(KD):
                    c.op("pe", lambda e: e.matmul(pp[0:64, :], lhsT=xT[:, kk, ch * 64:(ch + 1) * 64], rhs=win[:, kk, 1024:1536],
                                                  start=(kk == 0), stop=(kk == KD - 1)), reads=[win, xT], writes=[pp])
                c.op("act", lambda e: e.activation(out=Vt[:, ch, :], in_=pp[0:64, :], func=AF.Copy), reads=[pp], writes=[Vt])
            for h in range(4):
                pf = proj_F(512 + h * P, xT, hP)
                sigm(fa[:], fa, pf[:, 0:ST], pf)
                c.op("dve", lambda e: e.tensor_scalar(out=fa[:], in0=fa[:], scalar1=k.oml[:, l, h:h + 1], scalar2=None,
                                                      op0=ALU.mult), reads=[fa, k.oml], writes=[fa])
                c.op("act", lambda e: e.activation(out=flf[:], in_=fa[:], func=AF.Ln, bias=k.lb[:, l, h:h + 1]),
                     reads=[fa, k.lb], writes=[flf])
                c.op("pool", lambda e: e.tensor_scalar(out=fk[:], in0=fa[:], scalar1=-1.0, scalar2=k.oml[:, l, h:h + 1],
                                                       op0=ALU.mult, op1=ALU.add), reads=[fa, k.oml], writes=[fk])
                c.op("dve", lambda e: e.tensor_tensor_scan(out=fcum[:], data0=k.ones256[:, 0:ST], data1=flf[:], initial=0.0,
                                                           op0=ALU.mult, op1=ALU.add), reads=[k.ones256, flf], writes=[fcum])
                cv = fcum[:].rearrange("p (c t) -> p c t", t=64)
                c.op("dve", lambda e: e.memset(hs[:, 0:1], 0.0), writes=[hs])
                c.op("dve", lambda e: e.tensor_copy(out=hs[:, 1:NCH], in_=cv[:, 0:NCH - 1, 63]), reads=[fcum], writes=[hs])
                c.op("dve", lambda e: e.tensor_tensor(out=hs[:, 4:4 + NCH], in0=cv[:, :, 31], in1=hs[:, 0:NCH], op=ALU.subtract),
                     reads=[fcum, hs], writes=[hs])
                c.op("dve", lambda e: e.tensor_tensor(out=hs[:, 8:8 + NCH], in0=cv[:, :, 63], in1=hs[:, 0:NCH], op=ALU.subtract),
                     reads=[fcum, hs], writes=[hs])
                c.op("dve", lambda e: e.tensor_tensor(out=hs[:, 12:12 + NCH], in0=cv[:, :, 63], in1=cv[:, :, 31], op=ALU.subtract),
                     reads=[fcum, hs], writes=[hs])
                c.op("act", lambda e: e.activation(out=hsc[:], in_=hs[:, 4:16], func=AF.Exp), reads=[hs], writes=[hsc])
                c.op("dve", lambda e: e.tensor_tensor(out=fd[:].rearrange("p (c t) -> p c t", t=64), in0=cv,
                                                      in1=cv[:, :, 31:32].to_broadcast([P, NCH, 64]), op=ALU.subtract),
                     reads=[fcum], writes=[fd])
                c.op("act", lambda e: e.activation(out=fe[:], in_=fd[:], func=AF.Exp), reads=[fd], writes=[fe])
                pq = proj_F(h * P, xT, hP)
                c.op("dve", lambda e: e.tensor_tensor(out=qt[:], in0=pq[:, 0:ST], in1=fe[:], op=ALU.mult),
                     reads=[pq, fe], writes=[qt])
                c.op("act", lambda e: e.activation(out=fe[:], in_=fd[:], func=AF.Exp, scale=-1.0), reads=[fd], writes=[fe])
                c.op("dve", lambda e: e.tensor_tensor(out=kt[:], in0=fk[:], in1=fe[:], op=ALU.mult),
                     reads=[fk, fe], writes=[kt])
                for ch in range(NCH):
                    cs = slice(ch * 64, (ch + 1) * 64)
                    pm = hP.next()
                    c.op("pe", lambda e: e.matmul(pm[0:64, 0:64], lhsT=kt[:, cs], rhs=qt[:, cs], start=True, stop=True),
                         reads=[kt, qt], writes=[pm])
                    c.op("dve", lambda e: e.tensor_tensor(out=AT[:], in0=pm[0:64, 0:64], in1=k.mk_incl[:, 0, :], op=ALU.mult),
                         reads=[pm, k.mk_incl], writes=[AT])
                    pt2 = hP.next()
                    c.op("pe", lambda e: e.matmul(pt2[0:64, 0:P], lhsT=kt[:, cs], rhs=k.idb[:], start=True, stop=True),
                         reads=[kt, k.idb], writes=[pt2])
                    c.op("act", lambda e: e.activation(out=ktok[:], in_=pt2[0:64, 0:P], func=AF.Copy), reads=[pt2], writes=[ktok])
                    c.op("dve", lambda e: e.tensor_scalar(out=Ssc[:], in0=Sh[:, h, :], scalar1=hsc[:, ch:ch + 1], scalar2=None,
                                                          op0=ALU.mult), reads=[Sh, hsc], writes=[Ssc])
                    po = hP.next()
                    c.op("pe", lambda e: e.matmul(po[:, 0:64], lhsT=Vt[:, ch, h * P:(h + 1) * P], rhs=AT[:], start=True, stop=False),
                         reads=[Vt, AT], writes=[po])
                    c.op("pe", lambda e: e.matmul(po[:, 0:64], lhsT=Ssc[:], rhs=qt[:, cs], start=False, stop=True),
                         reads=[Ssc, qt], writes=[po])
                    c.op("act", lambda e: e.activation(out=osb[:, cs], in_=po[:, 0:64], func=AF.Copy), reads=[po], writes=[osb])
                    pd = hP.next()
                    c.op("pe", lambda e: e.matmul(pd[:, 0:P], lhsT=ktok[:], rhs=Vt[:, ch, h * P:(h + 1) * P], start=True, stop=True),
                         reads=[ktok, Vt], writes=[pd])
                    c.op("act", lambda e: e.activation(out=tmpS[:], in_=pd[:, 0:P], func=AF.Copy, scale=hsc[:, 8 + ch:9 + ch]),
                         reads=[pd, hsc], writes=[tmpS])
                    c.op("dve", lambda e: e.scalar_tensor_tensor(out=Sh[:, h, :], in0=Sh[:, h, :], scalar=hsc[:, 4 + ch:5 + ch],
                                                                 in1=tmpS[:], op0=ALU.mult, op1=ALU.add),
                         reads=[Sh, hsc, tmpS], writes=[Sh])
                rms_gate(osb[:], osb, 1536 + h * P, False, 0, yT[:, h, :], yT, xT, hP, rsH)

        def gconv(xT, key):
            for b in range(12):
                pp = proj_F(2048 + b * P, xT, gP)
                c.op("act", lambda e: e.activation(out=xp[:, 3:3 + ST], in_=pp[:, 0:ST], func=AF.Copy), reads=[pp], writes=[xp])
                c.op("pool", lambda e: e.tensor_copy(out=xp[:, 0:3], in_=hist[:, b, :]), reads=[hist], writes=[xp])
                c.op("dve", lambda e: e.tensor_scalar(out=yc[:], in0=xp[:, 3:3 + ST], scalar1=cw[:, b, 3:4], scalar2=None,
                                                      op0=ALU.mult), reads=[xp, cw], writes=[yc])
                for j in (2, 1, 0):
                    c.op("dve", lambda e: e.scalar_tensor_tensor(out=yc[:], in0=xp[:, j:j + ST], scalar=cw[:, b, j:j + 1], in1=yc[:],
                                                                 op0=ALU.mult, op1=ALU.add), reads=[xp, cw, yc], writes=[yc])
                c.op("pool", lambda e: e.tensor_copy(out=hist[:, b, :], in_=xp[:, ST:ST + 3]), reads=[xp], writes=[hist])
                sigm(ys[:], ys, yc[:], yc)
                if b >= 8:
                    c.op("dve", lambda e: e.tensor_tensor(out=gvF[:, b - 8, :], in0=yc[:], in1=ys[:], op=ALU.mult), reads=[yc, ys], writes=[gvF])
                else:
                    dst = gqT if b < 4 else gkT
                    c.op("dve", lambda e: e.tensor_tensor(out=ys[:], in0=yc[:], in1=ys[:], op=ALU.mult), reads=[yc, ys], writes=[ys])
                    c.op("act", lambda e: e.activation(out=sqC[:], in_=ys[:], func=AF.Square), reads=[ys], writes=[sqC])
                    pm = gP.next()
                    c.op("pe", lambda e: e.matmul(pm[:, 0:ST], lhsT=k.onesf[:], rhs=sqC[:], start=True, stop=True),
                         reads=[k.onesf, sqC], writes=[pm])
                    c.op("act", lambda e: e.activation(out=rstC[:], in_=pm[:, 0:ST], func=AF.Ln, bias=k.cst[:, 2:3]),
                         reads=[pm, k.cst], writes=[rstC])
                    c.op("act", lambda e: e.activation(out=rstC[:], in_=rstC[:], func=AF.Exp, scale=-0.5), reads=[rstC], writes=[rstC])
                    sc = (128.0 ** -0.5) if b < 4 else 1.0
                    c.op("dve", lambda e: e.scalar_tensor_tensor(out=dst[:, b % 4, :], in0=ys[:], scalar=sc, in1=rstC[:],
                                                                 op0=ALU.mult, op1=ALU.mult), reads=[ys, rstC], writes=[dst])
            for ch in range(NCH):
                if ch >= 2:
                    sch.need((key, "seq", ch - 2))
                gpre(xT, ch, pre[ch % 2])
                sch.post((key, "pre", ch))

        def gpre(xT, ch, pr):
            ga, gb, ecl, qeT, qkT, TT, kdec, ub, wT = (pr["ga"], pr["gb"], pr["ecl"], pr["qeT"], pr["qkT"], pr["TT"],
                                                       pr["kdec"], pr["ub"], pr["wT"])
            cs = slice(ch * 64, (ch + 1) * 64)
            pg = gP.next()
            for kk in range(KD):
                c.op("pe", lambda e: e.matmul(pg[0:64, 0:8], lhsT=xT[:, kk, cs], rhs=win[:, kk, 3584:3592],
                                              start=(kk == 0), stop=(kk == KD - 1)), reads=[win, xT], writes=[pg])
            c.op("dve", lambda e: e.tensor_tensor(out=ga[:, 0:4], in0=pg[0:64, 0:4], in1=dtb[0:64, :], op=ALU.add),
                 reads=[pg, dtb], writes=[ga])
            c.op("act", lambda e: e.activation(out=ga[:, 4:8], in_=ga[:, 0:4], func=AF.Exp), reads=[ga], writes=[ga])
            c.op("act", lambda e: e.activation(out=ga[:, 4:8], in_=ga[:, 4:8], func=AF.Ln, bias=k.cst[0:64, 3:4]),
                 reads=[ga, k.cst], writes=[ga])
            c.op("dve", lambda e: e.tensor_tensor(out=ga[:, 8:12], in0=ga[:, 4:8], in1=negA[0:64, :], op=ALU.mult),
                 reads=[ga, negA], writes=[ga])
            sigm(ga[:, 12:16], ga, pg[0:64, 4:8], pg)
            c.op("dve", lambda e: e.tensor_scalar(out=gb[:, 0:4], in0=ga[:, 12:16], scalar1=-1.0, scalar2=None, op0=ALU.mult),
                 reads=[ga], writes=[gb])
            pc = gP.next()
            c.op("pe", lambda e: e.matmul(pc[0:64, 0:4], lhsT=k.mk_incl[:, 0, :], rhs=ga[:, 8:12], start=True, stop=True),
                 reads=[k.mk_incl, ga], writes=[pc])
            c.op("dve", lambda e: e.tensor_copy(out=gb[:, 4:8], in_=pc[0:64, 0:4]), reads=[pc], writes=[gb])
            ptt = gP.next()
            c.op("pe", lambda e: e.matmul(ptt[:, 0:4], lhsT=k.onesf[0:64, :], rhs=ga[:, 8:12], start=True, stop=True),
                 reads=[k.onesf, ga], writes=[ptt])
            c.op("dve", lambda e: e.tensor_copy(out=ecl[:, 0:4], in_=ptt[:, 0:4]), reads=[ptt], writes=[ecl])
            c.op("act", lambda e: e.activation(out=ecl[:, 4:8], in_=ecl[:, 0:4], func=AF.Exp), reads=[ecl], writes=[ecl])
            c.op("act", lambda e: e.activation(out=gb[:, 8:12], in_=gb[:, 4:8], func=AF.Exp), reads=[gb], writes=[gb])
            c.op("dve", lambda e: e.tensor_tensor(out=gb[:, 12:16], in0=ecl[0:64, 0:4], in1=gb[:, 4:8], op=ALU.subtract),
                 reads=[ecl, gb], writes=[gb])
            c.op("act", lambda e: e.activation(out=gb[:, 12:16], in_=gb[:, 12:16], func=AF.Exp), reads=[gb], writes=[gb])
            for h in range(4):
                c.op("dve", lambda e: e.tensor_scalar(out=Dg[:, h, :], in0=idf64, scalar1=gb[:, 4 + h:5 + h], scalar2=None,
                                                       op0=ALU.mult), reads=[k.idf, gb], writes=[Dg])
            pr_ = gP.next()
            c.op("pe", lambda e: e.matmul(pr_[:, 0:256], lhsT=k.onesf[0:64, :], rhs=Dg[:].rearrange("s h t -> s (h t)"),
                                          start=True, stop=True), reads=[k.onesf, Dg], writes=[pr_])
            c.op("act", lambda e: e.activation(out=expR[:].rearrange("p h t -> p (h t)"), in_=pr_[:, 0:256], func=AF.Exp),
                 reads=[pr_], writes=[expR])
            c.op("dve", lambda e: e.tensor_tensor(out=qeT[:], in0=gqT[:, :, cs], in1=expR[:], op=ALU.mult),
                 reads=[gqT, expR], writes=[qeT])
            for h in range(4):
                c.op("dve", lambda e: e.tensor_scalar(out=decS[:, h, :], in0=pr_[0:64, h * 64:(h + 1) * 64],
                                                      scalar1=gb[:, 4 + h:5 + h], scalar2=0.0, op0=ALU.subtract, op1=ALU.min),
                     reads=[pr_, gb], writes=[decS])
            c.op("act", lambda e: e.activation(out=decS[:], in_=decS[:], func=AF.Exp), reads=[decS], writes=[decS])
            c.op("pool", lambda e: e.tensor_mul(out=decI[:], in0=decS[:], in1=k.mk_incl[:]), reads=[decS, k.mk_incl], writes=[decI])
            c.op("pool", lambda e: e.tensor_mul(out=decS[:], in0=decS[:], in1=k.mk_strict[:]), reads=[decS, k.mk_strict], writes=[decS])
            pG = gP.next()
            for h in range(4):
                c.op("pe", lambda e: e.matmul(pG[0:64, h * 64:(h + 1) * 64], lhsT=gkT[:, h, cs], rhs=gkT[:, h, cs], start=True, stop=True),
                     reads=[gkT], writes=[pG])
            pQ = gP.next()
            for h in range(4):
                c.op("pe", lambda e: e.matmul(pQ[0:64, h * 64:(h + 1) * 64], lhsT=gkT[:, h, cs], rhs=gqT[:, h, cs], start=True, stop=True),
                     reads=[gkT, gqT], writes=[pQ])
            B0, A0, P0 = Bb[0], Ab[0], Pb[0]
            c.op("dve", lambda e: e.tensor_tensor(out=B0[:], in0=pG[0:64, 0:256].rearrange("s (h t) -> s h t", h=4),
                                                  in1=decS[:], op=ALU.mult),
                 reads=[pG, decS], writes=[B0])
            c.op("dve", lambda e: e.tensor_tensor(out=B0[:], in0=B0[:], in1=ga[:, 12:16].unsqueeze(2).to_broadcast([64, 4, 64]),
                                                  op=ALU.mult), reads=[B0, ga], writes=[B0])
            c.op("dve", lambda e: e.tensor_tensor(out=qkT[:].rearrange("s h t -> s (h t)"), in0=pQ[0:64, 0:256],
                                                  in1=decI[:].rearrange("s h t -> s (h t)"), op=ALU.mult),
                 reads=[pQ, decI], writes=[qkT])
            pA = gP.next()
            for h in range(4):
                c.op("pe", lambda e: e.transpose(out=pA[0:64, h * 64:(h + 1) * 64], in_=B0[:, h, :], identity=idf64),
                     reads=[B0, k.idf], writes=[pA])
            c.op("act", lambda e: e.activation(out=A0[:], in_=pA[0:64, 0:256].rearrange("s (h t) -> s h t", h=4), func=AF.Copy),
                 reads=[pA], writes=[A0])
            c.op("pool", lambda e: e.tensor_sub(out=P0[:], in0=k.mk_incl[:], in1=k.mk_strict[:]), reads=[k.mk_incl, k.mk_strict], writes=[P0])
            c.op("pool", lambda e: e.tensor_sub(out=P0[:], in0=P0[:], in1=B0[:]), reads=[P0, B0], writes=[P0])
            for lv in range(5):
                Ak, Bk, Pk = Ab[lv % 2], Bb[lv % 2], Pb[lv % 2]
                An, Bn, Pn = Ab[(lv + 1) % 2], Bb[(lv + 1) % 2], Pb[(lv + 1) % 2]
                pab = gP.next()
                for h in range(4):
                    c.op("pe", lambda e: e.matmul(pab[0:64, h * 64:(h + 1) * 64], lhsT=Bk[:, h, :], rhs=Ak[:, h, :], start=True, stop=True),
                         reads=[Ak, Bk], writes=[pab])
                c.op("act", lambda e: e.activation(out=An[:], in_=pab[0:64, 0:256].rearrange("s (h t) -> s h t", h=4), func=AF.Copy),
                     reads=[pab], writes=[An])
                if lv < 4:
                    pbb = gP.next()
                    for h in range(4):
                        c.op("pe", lambda e: e.matmul(pbb[0:64, h * 64:(h + 1) * 64], lhsT=Ak[:, h, :], rhs=Bk[:, h, :], start=True, stop=True),
                             reads=[Ak, Bk], writes=[pbb])
                    c.op("dve", lambda e: e.tensor_copy(out=Bn[:], in_=pbb[0:64, 0:256].rearrange("s (h t) -> s h t", h=4)),
                         reads=[pbb], writes=[Bn])
                pp_ = gP.next()
                for h in range(4):
                    c.op("pe", lambda e: e.matmul(pp_[0:64, h * 64:(h + 1) * 64], lhsT=An[:, h, :], rhs=Pk[:, h, :], start=True, stop=True),
                         reads=[An, Pk], writes=[pp_])
                c.op("dve", lambda e: e.tensor_tensor(out=Pn[:].rearrange("s h t -> s (h t)"), in0=pp_[0:64, 0:256],
                                                      in1=Pk[:].rearrange("s h t -> s (h t)"), op=ALU.add),
                     reads=[pp_, Pk], writes=[Pn])
            Pf = Pb[5 % 2]
            c.op("act", lambda e: e.activation(out=TT[:], in_=Pf[:], func=AF.Copy), reads=[Pf], writes=[TT])
            for src_, dst_ in ((gvF, vtok), (gkT, ktk)):
                for hp in range(2):
                    pv = gP.next()
                    for hh in range(2):
                        h = hp * 2 + hh
                        c.op("pe", lambda e: e.matmul(pv[0:64, hh * P:(hh + 1) * P], lhsT=src_[:, h, cs], rhs=k.idb[:], start=True, stop=True),
                             reads=[src_, k.idb], writes=[pv])
                    c.op("act", lambda e: e.activation(out=dst_[:, hp * 2:hp * 2 + 2, :], in_=pv[0:64, 0:256].rearrange("s (h v) -> s h v", h=2),
                                                       func=AF.Copy), reads=[pv], writes=[dst_])
            c.op("dve", lambda e: e.tensor_tensor(out=ke[:], in0=ktk[:], in1=gb[:, 8:12].unsqueeze(2).to_broadcast([64, 4, P]),
                                                  op=ALU.mult), reads=[ktk, gb], writes=[ke])
            c.op("pool", lambda e: e.tensor_tensor(out=kdec[:], in0=ktk[:], in1=gb[:, 12:16].unsqueeze(2).to_broadcast([64, 4, P]),
                                                   op=ALU.mult), reads=[ktk, gb], writes=[kdec])
            for hp in range(2):
                pu = gP.next()
                for hh in range(2):
                    h = hp * 2 + hh
                    c.op("pe", lambda e: e.matmul(pu[0:64, hh * P:(hh + 1) * P], lhsT=TT[:, h, :], rhs=vtok[:, h, :], start=True, stop=True),
                         reads=[TT, vtok], writes=[pu])
                c.op("dve", lambda e: e.tensor_tensor(out=ub[:, hp * 2:hp * 2 + 2, :],
                                                      in0=pu[0:64, 0:256].rearrange("s (h v) -> s h v", h=2),
                                                      in1=ga[:, 12 + hp * 2:14 + hp * 2].unsqueeze(2).to_broadcast([64, 2, P]),
                                                      op=ALU.mult), reads=[pu, ga], writes=[ub])
            pw = gP.next()
            for h in range(4):
                c.op("pe", lambda e: e.matmul(pw[:, h * 64:(h + 1) * 64], lhsT=ke[:, h, :], rhs=TT[:, h, :], start=True, stop=True),
                     reads=[ke, TT], writes=[pw])
            c.op("act", lambda e: e.activation(out=wT[:].rearrange("p h t -> p (h t)"), in_=pw[:, 0:256], func=AF.Copy),
                 reads=[pw], writes=[wT])

        def gseq(xT, yT, key):
            for ch in range(NCH):
                sch.need((key, "pre", ch))
                pr = pre[ch % 2]
                gb, ecl, qeT, qkT, kdec, ub, wT = pr["gb"], pr["ecl"], pr["qeT"], pr["qkT"], pr["kdec"], pr["ub"], pr["wT"]
                cs = slice(ch * 64, (ch + 1) * 64)
                for h in range(4):
                    pws = sP.next()
                    c.op("pe", lambda e: e.matmul(pws[0:64, 0:P], lhsT=wT[:, h, :], rhs=Sgb[:, h, :], start=True, stop=True),
                         reads=[wT, Sgb], writes=[pws])
                    c.op("dve", lambda e: e.scalar_tensor_tensor(out=vnew[:], in0=pws[0:64, 0:P], scalar=gb[:, h:h + 1], in1=ub[:, h, :],
                                                                 op0=ALU.mult, op1=ALU.add), reads=[pws, gb, ub], writes=[vnew])
                    c.op("pe", lambda e: e.matmul(pws[:, 0:64], lhsT=Sgb[:, h, :], rhs=qeT[:, h, :], start=True, stop=False),
                         reads=[Sgb, qeT], writes=[pws])
                    c.op("pe", lambda e: e.matmul(pws[:, 0:64], lhsT=vnew[:], rhs=qkT[:, h, :], start=False, stop=True),
                         reads=[vnew, qkT], writes=[pws])
                    c.op("act", lambda e: e.activation(out=og[:, h, cs], in_=pws[:, 0:64], func=AF.Copy), reads=[pws], writes=[og])
                    c.op("pe", lambda e: e.matmul(pws[:, 0:128], lhsT=kdec[:, h, :], rhs=vnew[:], start=True, stop=True),
                         reads=[kdec, vnew], writes=[pws])
                    c.op("dve", lambda e: e.scalar_tensor_tensor(out=Sg[:, h, :], in0=Sg[:, h, :], scalar=ecl[:, 4 + h:5 + h],
                                                                 in1=pws[:, 0:128], op0=ALU.mult, op1=ALU.add),
                         reads=[Sg, ecl, pws], writes=[Sg])
                    c.op("act", lambda e: e.activation(out=Sgb[:, h, :], in_=Sg[:, h, :], func=AF.Copy), reads=[Sg], writes=[Sgb])
                sch.post((key, "seq", ch))
            for h in range(4):
                rms_gate(og[:, h, :], og, 3592 + h * P, True, 1, yT[:, 4 + h, :], yT, xT, sP, rsG)

        def epi(yT, t0):
            for i in range(ST // P):
                pa = k.PA[0]
                for half in range(2):
                    for fb in range(8):
                        c.op("pe", lambda e: e.matmul(pa[:, half, :], lhsT=yT[:, fb, i * P:(i + 1) * P],
                                                      rhs=wout[:, fb, half * 512:(half + 1) * 512],
                                                      start=(fb == 0), stop=(fb == 7)), reads=[yT, wout], writes=[pa])
                epilogue(k, pa[:].rearrange("p a b -> p (a b)"), pa, t0 // P + i, xin, xout, xtout)

        nst_seq = S // ST
        rnd = 0
        for s in range(NSEQ):
            c.op("pool", lambda e: e.memset(Sh[:], 0.0), writes=[Sh])
            c.op("pool", lambda e: e.memset(Sg[:], 0.0), writes=[Sg])
            c.op("pool", lambda e: e.memset(Sgb[:], 0.0), writes=[Sgb])
            c.op("pool", lambda e: e.memset(hist[:], 0.0), writes=[hist])
            prev = None
            for sti in range(nst_seq):
                t0 = s * S + sti * ST
                xT, yT = xTs[rnd % 2], yTs[rnd % 2]
                load_xT(k, xT, xtin, t0, ST)
                key = rnd
                fns = [lambda xT=xT, yT=yT: hgrn(xT, yT),
                       lambda xT=xT, key=key: gconv(xT, key),
                       lambda xT=xT, yT=yT, key=key: gseq(xT, yT, key)]
                if prev is not None:
                    fns.append(lambda pv=prev: epi(pv[0], pv[1]))
                import os as _os
                _ms = _os.environ.get("MIXSTREAMS")
                if _ms:
                    fns = [f for f, tag in zip(fns, "hcs") if tag in _ms]
                sch.run(fns)
                prev = (yT, t0)
                rnd += 1
            epi(prev[0], prev[1])
        c.sched = None
        c.barrier()


_NC_CACHE = {}


def kernel(**inputs):
    n = 8
    if "nc" not in _NC_CACHE:
        _NC_CACHE["nc"] = build()
    nc = _NC_CACHE["nc"]
    x = np.ascontiguousarray(inputs["x"], dtype=np.float32)
    mem = np.ascontiguousarray(inputs["mem"], dtype=np.float32)
    in_maps = []
    for ci in range(n):
        m = {kk: np.ascontiguousarray(vv, dtype=np.float32) for kk, vv in inputs.items() if kk not in ("x", "mem")}
        m["x"] = x[2 * ci:2 * ci + 2].reshape(2 * 2048, D)
        m["mem"] = mem[2 * ci:2 * ci + 2].reshape(2 * MEM_LEN, D)
        in_maps.append(m)
    res = run_bass_kernel_spmd(nc, in_maps, core_ids=list(range(n)))
    out = np.concatenate([r["out"].reshape(2, 2048, D) for r in res.results], axis=0)
    return out.astype(np.float32)
```

```python
from contextlib import ExitStack
import numpy as np
import concourse.bass as bass
import concourse.mybir as mybir
from concourse.bass_utils import run_bass_kernel_spmd

F32 = mybir.dt.float32
BF16 = mybir.dt.bfloat16
AF = mybir.ActivationFunctionType
ALU = mybir.AluOpType
AX = mybir.AxisListType

P = 128
D = 1024
KD = 8
DEPTH = 4
MEM_LEN = 256
IN_WIDTH = 4104
N_EXP = 32
D_EXP = 512
ALPHA = (2.0 * DEPTH) ** 0.25
LN_EPS = 1e-5
RMS_EPS = 1e-6
L2_EPS = 1e-6
BIG = 1.0e30


class Buf:
    __slots__ = ("t", "w", "r", "sem", "cnt", "name")

    def __init__(self, t, name=""):
        self.t = t
        self.w = None
        self.r = []
        self.sem = None
        self.cnt = 0
        self.name = name

    def __getitem__(self, k):
        return self.t[k]


class Ctx:
    ENG = ("pe", "act", "dve", "pool", "sp")

    def __init__(self, nc):
        self.nc = nc
        self.engs = {"pe": nc.tensor, "act": nc.scalar, "dve": nc.vector,
                     "pool": nc.gpsimd, "sp": nc.sync}
        self.sem = {e: nc.alloc_semaphore("s_" + e) for e in self.ENG}
        self.cnt = {e: 0 for e in self.ENG}
        self.seen = {e: {} for e in self.ENG}
        self.dma_bufs = []
        self.nsem = 5
        self.ninst = 0
        self.sched = None

    def _wait(self, eng, ev):
        if ev is None:
            return
        sem, val = ev
        if eng == "pe" and sem is self.sem["pe"]:
            return
        if self.seen[eng].get(sem.num, 0) >= val:
            return
        self.engs[eng].wait_ge(sem, val)
        self.seen[eng][sem.num] = val
        self.ninst += 1

    def _deps(self, eng, reads, writes):
        for b in reads:
            self._wait(eng, b.w)
        for b in writes:
            self._wait(eng, b.w)
            for ev in b.r:
                self._wait(eng, ev)

    def _commit(self, ev, reads, writes):
        for b in reads:
            b.r.append(ev)
            if len(b.r) > 40:
                last = {}
                for s, v in b.r:
                    if v > last.get(s.num, (None, 0))[1]:
                        last[s.num] = (s, v)
                b.r = list(last.values())
        for b in writes:
            b.w = ev
            b.r = []

    def op(self, eng, fn, reads=(), writes=()):
        self._deps(eng, reads, writes)
        ins = fn(self.engs[eng])
        self.cnt[eng] += 1
        self.ninst += 1
        ins.then_inc(self.sem[eng], 1)
        self._commit((self.sem[eng], self.cnt[eng]), reads, writes)
        if self.sched is not None:
            self.sched.tick()
        return ins

    def dma(self, eng, out, in_, dst, src, **kw):
        self._deps(eng, [src], [dst])
        if dst.sem is None:
            dst.sem = self.nc.alloc_semaphore("d%d" % self.nsem)
            self.nsem += 1
            self.dma_bufs.append(dst)
        ins = self.engs[eng].dma_start(out=out, in_=in_, **kw)
        dst.cnt += 16
        self.ninst += 1
        ins.then_inc(dst.sem, 16)
        self._commit((dst.sem, dst.cnt), [src], [dst])
        if self.sched is not None:
            self.sched.tick()
        return ins

    def barrier(self):
        for e in self.ENG:
            for e2 in self.ENG:
                if e2 != e and self.cnt[e2] > 0:
                    self._wait(e, (self.sem[e2], self.cnt[e2]))
            for b in self.dma_bufs:
                if b.cnt:
                    self._wait(e, (b.sem, b.cnt))


class K:
    pass


def build(NSEQ=2, S=2048, L=4, phases=("mix", "attn", "moe"), MEM=MEM_LEN):
    nc = bass.Bass("TRN2", target_bir_lowering=False)
    T = NSEQ * S
    NT = T // P
    c = Ctx(nc)
    k = K()
    k.nc, k.c, k.NSEQ, k.S, k.L, k.T, k.NT, k.MEM = nc, c, NSEQ, S, L, T, NT, MEM

    def din(name, shape):
        return Buf(nc.dram_tensor(name, list(shape), F32, kind="ExternalInput"), name)

    k.x = din("x", [T, D])
    k.mem = din("mem", [NSEQ * MEM, D])
    k.w_in = din("w_in", [L, D, IN_WIDTH])
    k.hg_lb_logits = din("hg_lb_logits", [DEPTH, 512])
    k.hg_norm_w = din("hg_norm_w", [L, 128])
    k.gd_conv_w = din("gd_conv_w", [L, 4, 1536])
    k.gd_a_log = din("gd_a_log", [L, 4])
    k.gd_dt_bias = din("gd_dt_bias", [L, 4])
    k.gd_norm_w = din("gd_norm_w", [L, 128])
    k.w_out = din("w_out", [L, D, D])
    k.mem_ln_g = din("mem_ln_g", [D])
    k.mem_ln_b = din("mem_ln_b", [D])
    k.w_mq = din("w_mq", [L, D, D])
    k.w_mk = din("w_mk", [L, D, D])
    k.w_mv = din("w_mv", [L, D, D])
    k.w_mo = din("w_mo", [L, D, D])
    k.w_group = din("w_group", [L, D, 4])
    k.b_group = din("b_group", [L, 4])
    k.w_router = din("w_router", [L, D, N_EXP])
    k.b_router = din("b_router", [L, N_EXP])
    k.w_gate = din("w_gate", [L, N_EXP, D, D_EXP])
    k.w_up = din("w_up", [L, N_EXP, D, D_EXP])
    k.w_down = din("w_down", [L, N_EXP, D_EXP, D])
    k.ln_g = din("ln_g", [L, 3, D])
    k.ln_b = din("ln_b", [L, 3, D])
    k.out = Buf(nc.dram_tensor("out", [T, D], F32, kind="ExternalOutput"), "out")
    k.XA = Buf(nc.dram_tensor("XA", [T, D], F32, kind="Internal"), "XA")
    k.XB = Buf(nc.dram_tensor("XB", [T, D], F32, kind="Internal"), "XB")
    k.XTA = Buf(nc.dram_tensor("XTA", [NT, P, KD, P], BF16, kind="Internal"), "XTA")
    k.XTB = Buf(nc.dram_tensor("XTB", [NT, P, KD, P], BF16, kind="Internal"), "XTB")

    def sb(name, shape, dt=F32):
        return Buf(nc.alloc_sbuf_tensor(name, list(shape), dt), name)

    k.idf = sb("idf", [P, P])
    k.idb = sb("idb", [P, P], BF16)
    k.onesf = sb("onesf", [P, P])
    k.onesb = sb("onesb", [P, P], BF16)
    k.cst = sb("cst", [P, 8])
    k.memT = sb("memT", [P, KD, NSEQ * MEM], BF16)
    c.op("pool", lambda e: e.memset(k.idf[:], 0.0), writes=[k.idf])
    c.op("pool", lambda e: e.affine_select(out=k.idf[:], in_=k.idf[:], pattern=[[-1, P]], base=0,
                                           channel_multiplier=1, compare_op=ALU.not_equal, fill=1.0),
         reads=[k.idf], writes=[k.idf])
    c.op("dve", lambda e: e.tensor_copy(out=k.idb[:], in_=k.idf[:]), reads=[k.idf], writes=[k.idb])
    c.op("pool", lambda e: e.memset(k.onesf[:], 1.0), writes=[k.onesf])
    c.op("pool", lambda e: e.memset(k.onesb[:], 1.0), writes=[k.onesb])
    for j, v in enumerate([LN_EPS, RMS_EPS, L2_EPS, 1.0, 0.0]):
        c.op("pool", lambda e: e.memset(k.cst[:, j:j + 1], v), writes=[k.cst])

    k.ep_xo = [sb("ep_xo%d" % i, [P, D]) for i in range(2)]
    k.ep_y = sb("ep_y", [P, D])
    k.ep_xn = [sb("ep_xn%d" % i, [P, D]) for i in range(2)]
    k.ep_xb = sb("ep_xb", [P, D], BF16)
    k.ep_xT = [sb("ep_xT%d" % i, [P, KD, P], BF16) for i in range(2)]
    k.ep_st = sb("ep_st", [P, 2, 6])
    k.ep_mv = sb("ep_mv", [P, 4])
    k.ep_g = sb("ep_g", [P, D])
    k.ep_b = sb("ep_b", [P, D])
    k.ep_n = 0
    k.pT = Buf(nc.alloc_psum_tensor("pT", [P, KD, P], BF16), "pT")
    k.PA = [Buf(nc.alloc_psum_tensor("PA0", [P, 2, 512], F32), "PA0")]

    prologue(k)
    if "mix" in phases:
        mix_consts(k)
    xin, xtin = k.x, k.XTA
    cur = 0
    xs = [k.XA, k.XB]
    xts = [k.XTB, k.XTA]
    for l in range(L):
        for pi, ph in enumerate(("mix", "attn", "moe")):
            if ph not in phases:
                continue
            last = (l == L - 1) and ph == [p_ for p_ in ("mix", "attn", "moe") if p_ in phases][-1]
            xout = k.out if last else xs[cur]
            xtout = xts[cur]
            c.barrier()
            load_ln(k, l, pi)
            if ph == "mix":
                phase_mix(k, l, xin, xtin, xout, xtout)
            elif ph == "attn":
                phase_attn(k, l, xin, xtin, xout, xtout)
            else:
                phase_moe(k, l, xin, xtin, xout, xtout)
            xin, xtin = xout, xtout
            cur ^= 1
    c.barrier()
    return nc


def load_ln(k, l, idx):
    c = k.c
    c.dma("sp", k.ep_g[:], k.ln_g[l, idx, :].partition_broadcast(P), k.ep_g, k.ln_g)
    c.dma("sp", k.ep_b[:], k.ln_b[l, idx, :].partition_broadcast(P), k.ep_b, k.ln_b)


def ln_rows(k, src_ap, src_buf, dst, g, b, mv, st):
    c = k.c
    for h in range(2):
        c.op("dve", lambda e: e.bn_stats(out=st[:, h, :], in_=src_ap[:, h * 512:(h + 1) * 512]),
             reads=[src_buf], writes=[st])
    c.op("dve", lambda e: e.bn_aggr(out=mv[:, 0:2], in_=st[:].rearrange("p a b -> p (a b)")),
         reads=[st], writes=[mv])
    c.op("act", lambda e: e.activation(out=mv[:, 2:3], in_=mv[:, 1:2], func=AF.Ln, bias=k.cst[:, 0:1]),
         reads=[mv, k.cst], writes=[mv])
    c.op("act", lambda e: e.activation(out=mv[:, 3:4], in_=mv[:, 2:3], func=AF.Exp, scale=-0.5), reads=[mv], writes=[mv])
    c.op("dve", lambda e: e.tensor_scalar(out=dst[:], in0=src_ap, scalar1=mv[:, 0:1], scalar2=mv[:, 3:4],
                                          op0=ALU.subtract, op1=ALU.mult),
         reads=[src_buf, mv], writes=[dst])
    c.op("pool", lambda e: e.tensor_mul(out=dst[:], in0=dst[:], in1=g[:]), reads=[dst, g], writes=[dst])
    c.op("pool", lambda e: e.tensor_add(out=dst[:], in0=dst[:], in1=b[:]), reads=[dst, b], writes=[dst])


def to_xT(k, xn, ti, xtout):
    c = k.c
    n = k.ep_n
    xT = k.ep_xT[n % 2]
    c.op("act", lambda e: e.activation(out=k.ep_xb[:], in_=xn[:], func=AF.Copy), reads=[xn], writes=[k.ep_xb])
    for kk in range(KD):
        c.op("pe", lambda e: e.transpose(out=k.pT[:, kk, :], in_=k.ep_xb[:, kk * P:(kk + 1) * P], identity=k.idb[:]),
             reads=[k.ep_xb, k.idb], writes=[k.pT])
    c.op("dve", lambda e: e.tensor_copy(out=xT[:], in_=k.pT[:]), reads=[k.pT], writes=[xT])
    c.dma("sp", xtout[ti], xT[:], xtout, xT)


def epilogue(k, sub_ap, sub_buf, ti, xin, xout, xtout):
    c = k.c
    n = k.ep_n
    xo = k.ep_xo[n % 2]
    xn = k.ep_xn[n % 2]
    c.dma("sp", xo[:], xin[ti * P:(ti + 1) * P, :], xo, xin)
    c.op("dve", lambda e: e.scalar_tensor_tensor(out=k.ep_y[:], in0=xo[:], scalar=ALPHA, in1=sub_ap,
                                                 op0=ALU.mult, op1=ALU.add),
         reads=[xo, sub_buf], writes=[k.ep_y])
    ln_rows(k, k.ep_y[:], k.ep_y, xn, k.ep_g, k.ep_b, k.ep_mv, k.ep_st)
    c.dma("sp", xout[ti * P:(ti + 1) * P, :], xn[:], xout, xn)
    to_xT(k, xn, ti, xtout)
    k.ep_n += 1


def prologue(k):
    c, nc = k.c, k.nc
    for ti in range(k.NT):
        xn = k.ep_xn[k.ep_n % 2]
        c.dma("sp", xn[:], k.x[ti * P:(ti + 1) * P, :], xn, k.x)
        to_xT(k, xn, ti, k.XTA)
        k.ep_n += 1
    c.dma("sp", k.ep_g[:], k.mem_ln_g[:].partition_broadcast(P), k.ep_g, k.mem_ln_g)
    c.dma("sp", k.ep_b[:], k.mem_ln_b[:].partition_broadcast(P), k.ep_b, k.mem_ln_b)
    for mi in range(k.NSEQ * k.MEM // P):
        xo = k.ep_xo[mi % 2]
        xn = k.ep_xn[mi % 2]
        c.dma("sp", xo[:], k.mem[mi * P:(mi + 1) * P, :], xo, k.mem)
        ln_rows(k, xo[:], xo, xn, k.ep_g, k.ep_b, k.ep_mv, k.ep_st)
        c.op("act", lambda e: e.activation(out=k.ep_xb[:], in_=xn[:], func=AF.Copy), reads=[xn], writes=[k.ep_xb])
        for kk in range(KD):
            c.op("pe", lambda e: e.transpose(out=k.pT[:, kk, :], in_=k.ep_xb[:, kk * P:(kk + 1) * P], identity=k.idb[:]),
                 reads=[k.ep_xb, k.idb], writes=[k.pT])
        c.op("dve", lambda e: e.tensor_copy(out=k.memT[:, :, mi * P:(mi + 1) * P], in_=k.pT[:]),
             reads=[k.pT], writes=[k.memT])


def load_xT(k, dst, xtin, t0, n):
    c = k.c
    nt = n // P
    src = xtin[t0 // P:t0 // P + nt].rearrange("i p k t -> p i k t")
    for i in range(nt):
        c.dma("sp", dst[:, :, i * P:(i + 1) * P], xtin[t0 // P + i], dst, xtin)


def load_w_bf(k, dst, dram_ap, src_buf):
    k.c.dma("pool", dst[:], dram_ap.rearrange("(k p) n -> p k n", p=P), dst, src_buf)


def phase_attn(k, l, xin, xtin, xout, xtout):
    c, nc = k.c, k.nc
    MEM, NSEQ, S = k.MEM, k.NSEQ, k.S
    MC = MEM // P
    with ExitStack() as es_:
        def sbt(name, shape, dt=F32):
            return Buf(es_.enter_context(nc.sbuf_tensor("%s_L%d" % (name, l), list(shape), dt)), name)

        def pst(name, shape, dt=F32):
            return Buf(es_.enter_context(nc.psum_tensor("%s_L%d" % (name, l), list(shape), dt)), name)

        wq, wo = sbt("a_wq", [P, KD, D], BF16), sbt("a_wo", [P, KD, D], BF16)
        kT = sbt("a_kT", [P, NSEQ, KD, MEM], BF16)
        v = sbt("a_v", [P, NSEQ * MC, D], BF16)
        xTs = [sbt("a_xT0", [P, KD, 512], BF16), sbt("a_xT1", [P, KD, 512], BF16)]
        qT = sbt("a_qT", [P, KD, 512], BF16)
        es = [sbt("a_e0", [P, MC, 512], BF16), sbt("a_e1", [P, MC, 512], BF16)]
        rs = sbt("a_rs", [P, 512], F32)
        oTs = [sbt("a_oT0", [P, KD, 512], BF16), sbt("a_oT1", [P, KD, 512], BF16)]
        ps = [pst("a_p0", [P, 512]), pst("a_p1", [P, 512]), pst("a_p2", [P, 512])]
        PAs = [k.PA[0], pst("a_PA1", [P, 2, 512])]
        pn = [0]

        def nextp():
            pn[0] += 1
            return ps[pn[0] % 3]

        load_w_bf(k, wq, k.w_mk[l], k.w_mk)
        load_w_bf(k, wo, k.w_mv[l], k.w_mv)
        for s in range(NSEQ):
            for fb in range(KD):
                pp = nextp()
                for kk in range(KD):
                    c.op("pe", lambda e: e.matmul(pp[:, 0:MEM], lhsT=wq[:, kk, fb * P:(fb + 1) * P],
                                                  rhs=k.memT[:, kk, s * MEM:(s + 1) * MEM],
                                                  start=(kk == 0), stop=(kk == KD - 1)),
                         reads=[wq, k.memT], writes=[pp])
                c.op("act", lambda e: e.activation(out=kT[:, s, fb, :], in_=pp[:, 0:MEM], func=AF.Copy, scale=1.0 / 16.0),
                     reads=[pp], writes=[kT])
            for mc in range(MC):
                for half in range(2):
                    pp = nextp()
                    for kk in range(KD):
                        c.op("pe", lambda e: e.matmul(pp[:], lhsT=k.memT[:, kk, s * MEM + mc * P:s * MEM + (mc + 1) * P],
                                                      rhs=wo[:, kk, half * 512:(half + 1) * 512],
                                                      start=(kk == 0), stop=(kk == KD - 1)),
                             reads=[wo, k.memT], writes=[pp])
                    c.op("dve", lambda e: e.tensor_copy(out=v[:, s * MC + mc, half * 512:(half + 1) * 512], in_=pp[:]),
                         reads=[pp], writes=[v])
        load_w_bf(k, wq, k.w_mq[l], k.w_mq)
        load_w_bf(k, wo, k.w_mo[l], k.w_mo)
        nst = k.T // 512
        sch = Sched()
        c.sched = sch

        def comp(st, xT, oT):
            s = (st * 512) // S
            for fb in range(KD):
                pp = nextp()
                for kk in range(KD):
                    c.op("pe", lambda e: e.matmul(pp[:], lhsT=wq[:, kk, fb * P:(fb + 1) * P], rhs=xT[:, kk, :],
                                                  start=(kk == 0), stop=(kk == KD - 1)),
                         reads=[wq, xT], writes=[pp])
                if fb % 2 == 0:
                    c.op("act", lambda e: e.activation(out=qT[:, fb, :], in_=pp[:], func=AF.Copy), reads=[pp], writes=[qT])
                else:
                    c.op("dve", lambda e: e.tensor_copy(out=qT[:, fb, :], in_=pp[:]), reads=[pp], writes=[qT])
            for h in range(4):
                ee = es[h % 2]
                for mc in range(MC):
                    pp = nextp()
                    for j in range(2):
                        c.op("pe", lambda e: e.matmul(pp[:], lhsT=kT[:, s, 2 * h + j, mc * P:(mc + 1) * P],
                                                      rhs=qT[:, 2 * h + j, :], start=(j == 0), stop=(j == 1)),
                             reads=[kT, qT], writes=[pp])
                    c.op("act", lambda e: e.activation(out=ee[:, mc, :], in_=pp[:], func=AF.Exp), reads=[pp], writes=[ee])
                pp = nextp()
                for mc in range(MC):
                    c.op("pe", lambda e: e.matmul(pp[:], lhsT=k.onesb[:], rhs=ee[:, mc, :], start=(mc == 0), stop=(mc == MC - 1)),
                         reads=[k.onesb, ee], writes=[pp])
                c.op("dve", lambda e: e.reciprocal(out=rs[:], in_=pp[:]), reads=[pp], writes=[rs])
                for j in range(2):
                    pp = nextp()
                    for mc in range(MC):
                        c.op("pe", lambda e: e.matmul(pp[:], lhsT=v[:, s * MC + mc, (2 * h + j) * P:(2 * h + j + 1) * P],
                                                      rhs=ee[:, mc, :], start=(mc == 0), stop=(mc == MC - 1)),
                             reads=[v, ee], writes=[pp])
                    c.op("dve", lambda e: e.tensor_tensor(out=oT[:, 2 * h + j, :], in0=pp[:], in1=rs[:], op=ALU.mult),
                         reads=[pp, rs], writes=[oT])

        def outp(st, oT):
            for i in range(4):
                pa = PAs[(st * 4 + i) % 2]
                for half in range(2):
                    for fb in range(KD):
                        c.op("pe", lambda e: e.matmul(pa[:, half, :], lhsT=oT[:, fb, i * P:(i + 1) * P],
                                                      rhs=wo[:, fb, half * 512:(half + 1) * 512],
                                                      start=(fb == 0), stop=(fb == KD - 1)),
                             reads=[oT, wo], writes=[pa])
                epilogue(k, pa[:].rearrange("p a b -> p (a b)"), pa, st * 4 + i, xin, xout, xtout)

        for st in range(nst):
            xT = xTs[st % 2]
            load_xT(k, xT, xtin, st * 512, 512)
            fns = [lambda st=st, xT=xT: comp(st, xT, oTs[st % 2])]
            if st > 0:
                fns.append(lambda st=st: outp(st - 1, oTs[(st - 1) % 2]))
            sch.run(fns)
        outp(nst - 1, oTs[(nst - 1) % 2])
        c.sched = None
        c.barrier()


def phase_moe(k, l, xin, xtin, xout, xtout):
    c, nc = k.c, k.nc
    TB = min(1024, k.T)
    NTB = TB // P
    with ExitStack() as es_:
        def sbt(name, shape, dt=F32):
            return Buf(es_.enter_context(nc.sbuf_tensor("%s_L%d" % (name, l), list(shape), dt)), name)

        def pst(name, shape, dt=F32):
            return Buf(es_.enter_context(nc.psum_tensor("%s_L%d" % (name, l), list(shape), dt)), name)

        xT = sbt("m_xT", [P, KD, TB], BF16)
        yacc = sbt("m_yacc", [P, NTB, D], F32)
        wgs = [sbt("m_wg0", [P, KD, D_EXP], BF16), sbt("m_wg1", [P, KD, D_EXP], BF16)]
        wus = [sbt("m_wu0", [P, KD, D_EXP], BF16), sbt("m_wu1", [P, KD, D_EXP], BF16)]
        wds = [sbt("m_wd0", [P, 4, D], BF16), sbt("m_wd1", [P, 4, D], BF16)]
        wr = sbt("m_wr", [P, KD, 36], BF16)
        br, lg, sm = sbt("m_br", [P, 36]), sbt("m_lg", [P, 36]), sbt("m_sm", [P, 16])
        t0, t1, t2 = sbt("m_t0", [P, 32]), sbt("m_t1", [P, 32]), sbt("m_t2", [P, 32])
        gate = sbt("m_gate", [P, NTB, 32])
        sg = sbt("m_sg", [P, 512])
        hTs = [sbt("m_hT0", [P, 4, 512], BF16), sbt("m_hT1", [P, 4, 512], BF16)]
        ps = [pst("m_p0", [P, 512]), pst("m_p1", [P, 512]), pst("m_p2", [P, 512])]
        PAs = [k.PA[0], pst("m_PA1", [P, 2, 512])]
        pn = [0]

        def nextp():
            pn[0] += 1
            return ps[pn[0] % 3]

        c.dma("pool", wr[:, :, 0:4], k.w_group[l].rearrange("(k p) n -> p k n", p=P), wr, k.w_group)
        c.dma("pool", wr[:, :, 4:36], k.w_router[l].rearrange("(k p) n -> p k n", p=P), wr, k.w_router)
        c.dma("sp", br[:, 0:4], k.b_group[l, :].partition_broadcast(P), br, k.b_group)
        c.dma("sp", br[:, 4:36], k.b_router[l, :].partition_broadcast(P), br, k.b_router)
        for blk in range(k.T // TB):
            tb0 = blk * TB
            load_xT(k, xT, xtin, tb0, TB)
            for ti in range(NTB):
                pp = nextp()
                for kk in range(KD):
                    c.op("pe", lambda e: e.matmul(pp[:, 0:36], lhsT=xT[:, kk, ti * P:(ti + 1) * P], rhs=wr[:, kk, :],
                                                  start=(kk == 0), stop=(kk == KD - 1)), reads=[xT, wr], writes=[pp])
                c.op("dve", lambda e: e.tensor_tensor(out=lg[:], in0=pp[:, 0:36], in1=br[:], op=ALU.add),
                     reads=[pp, br], writes=[lg])
                c.op("dve", lambda e: e.reduce_max(out=sm[:, 0:1], in_=lg[:, 0:4], axis=AX.X), reads=[lg], writes=[sm])
                c.op("dve", lambda e: e.tensor_scalar(out=t0[:, 0:4], in0=lg[:, 0:4], scalar1=sm[:, 0:1], scalar2=None,
                                                      op0=ALU.is_equal), reads=[lg, sm], writes=[t0])
                c.op("dve", lambda e: e.tensor_scalar(out=sm[:, 1:2], in0=sm[:, 0:1], scalar1=-1.0, scalar2=None,
                                                      op0=ALU.mult), reads=[sm], writes=[sm])
                c.op("act", lambda e: e.activation(out=t1[:, 0:4], in_=lg[:, 0:4], func=AF.Exp, bias=sm[:, 1:2],
                                                   accum_out=sm[:, 2:3]), reads=[lg, sm], writes=[t1, sm])
                c.op("dve", lambda e: e.reciprocal(out=sm[:, 3:4], in_=sm[:, 2:3]), reads=[sm], writes=[sm])
                c.op("dve", lambda e: e.tensor_scalar(out=t0[:, 4:8], in0=t0[:, 0:4], scalar1=-1.0, scalar2=BIG,
                                                      op0=ALU.add, op1=ALU.mult), reads=[t0], writes=[t0])
                c.op("dve", lambda e: e.tensor_tensor(out=t1[:].rearrange("p (g e) -> p g e", g=4),
                                                      in0=lg[:, 4:36].rearrange("p (g e) -> p g e", g=4),
                                                      in1=t0[:, 4:8].unsqueeze(2).to_broadcast([P, 4, 8]), op=ALU.add),
                     reads=[lg, t0], writes=[t1])
                c.op("dve", lambda e: e.reduce_max(out=sm[:, 4:5], in_=t1[:], axis=AX.X), reads=[t1], writes=[sm])
                c.op("dve", lambda e: e.tensor_scalar(out=t0[:], in0=t1[:], scalar1=sm[:, 4:5], scalar2=None,
                                                      op0=ALU.is_equal), reads=[t1, sm], writes=[t0])
                c.op("dve", lambda e: e.scalar_tensor_tensor(out=t1[:], in0=t0[:], scalar=-BIG, in1=t1[:],
                                                             op0=ALU.mult, op1=ALU.add), reads=[t0, t1], writes=[t1])
                c.op("dve", lambda e: e.reduce_max(out=sm[:, 5:6], in_=t1[:], axis=AX.X), reads=[t1], writes=[sm])
                c.op("dve", lambda e: e.tensor_scalar(out=t2[:], in0=t1[:], scalar1=sm[:, 5:6], scalar2=None,
                                                      op0=ALU.is_equal), reads=[t1, sm], writes=[t2])
                c.op("dve", lambda e: e.tensor_tensor(out=sm[:, 6:7], in0=sm[:, 5:6], in1=sm[:, 4:5], op=ALU.subtract),
                     reads=[sm], writes=[sm])
                c.op("act", lambda e: e.activation(out=sm[:, 7:8], in_=sm[:, 6:7], func=AF.Exp), reads=[sm], writes=[sm])
                c.op("dve", lambda e: e.tensor_scalar(out=sm[:, 7:8], in0=sm[:, 7:8], scalar1=1.0, scalar2=None, op0=ALU.add),
                     reads=[sm], writes=[sm])
                c.op("dve", lambda e: e.reciprocal(out=sm[:, 7:8], in_=sm[:, 7:8]), reads=[sm], writes=[sm])
                c.op("dve", lambda e: e.tensor_tensor(out=sm[:, 8:9], in0=sm[:, 7:8], in1=sm[:, 3:4], op=ALU.mult),
                     reads=[sm], writes=[sm])
                c.op("dve", lambda e: e.tensor_tensor(out=sm[:, 9:10], in0=sm[:, 3:4], in1=sm[:, 8:9], op=ALU.subtract),
                     reads=[sm], writes=[sm])
                c.op("dve", lambda e: e.tensor_scalar(out=t0[:], in0=t0[:], scalar1=sm[:, 8:9], scalar2=None, op0=ALU.mult),
                     reads=[t0, sm], writes=[t0])
                c.op("dve", lambda e: e.scalar_tensor_tensor(out=gate[:, ti, :], in0=t2[:], scalar=sm[:, 9:10], in1=t0[:],
                                                             op0=ALU.mult, op1=ALU.add), reads=[t2, sm, t0], writes=[gate])
            for ex in range(N_EXP):
                wg, wu, wd = wgs[ex % 2], wus[ex % 2], wds[ex % 2]
                load_w_bf(k, wg, k.w_gate[l, ex], k.w_gate)
                load_w_bf(k, wu, k.w_up[l, ex], k.w_up)
                load_w_bf(k, wd, k.w_down[l, ex], k.w_down)
                for j in range(TB // 512):
                    hT = hTs[j % 2]
                    for fb in range(4):
                        pg = nextp()
                        for kk in range(KD):
                            c.op("pe", lambda e: e.matmul(pg[:], lhsT=wg[:, kk, fb * P:(fb + 1) * P],
                                                          rhs=xT[:, kk, j * 512:(j + 1) * 512],
                                                          start=(kk == 0), stop=(kk == KD - 1)), reads=[wg, xT], writes=[pg])
                        c.op("act", lambda e: e.activation(out=sg[:], in_=pg[:], func=AF.Silu), reads=[pg], writes=[sg])
                        pu = nextp()
                        for kk in range(KD):
                            c.op("pe", lambda e: e.matmul(pu[:], lhsT=wu[:, kk, fb * P:(fb + 1) * P],
                                                          rhs=xT[:, kk, j * 512:(j + 1) * 512],
                                                          start=(kk == 0), stop=(kk == KD - 1)), reads=[wu, xT], writes=[pu])
                        c.op("dve", lambda e: e.tensor_tensor(out=hT[:, fb, :], in0=pu[:], in1=sg[:], op=ALU.mult),
                             reads=[pu, sg], writes=[hT])
                    for i in range(4):
                        ti = j * 4 + i
                        pa = PAs[ti % 2]
                        for half in range(2):
                            for fb in range(4):
                                c.op("pe", lambda e: e.matmul(pa[:, half, :], lhsT=hT[:, fb, i * P:(i + 1) * P],
                                                              rhs=wd[:, fb, half * 512:(half + 1) * 512],
                                                              start=(fb == 0), stop=(fb == 3)), reads=[hT, wd], writes=[pa])
                        pav = pa[:].rearrange("p a b -> p (a b)")
                        if ex == 0:
                            c.op("dve", lambda e: e.tensor_scalar(out=yacc[:, ti, :], in0=pav, scalar1=gate[:, ti, ex:ex + 1],
                                                                  scalar2=None, op0=ALU.mult),
                                 reads=[pa, gate], writes=[yacc])
                        else:
                            c.op("dve", lambda e: e.scalar_tensor_tensor(out=yacc[:, ti, :], in0=pav,
                                                                         scalar=gate[:, ti, ex:ex + 1], in1=yacc[:, ti, :],
                                                                         op0=ALU.mult, op1=ALU.add),
                                 reads=[pa, gate, yacc], writes=[yacc])
            for ti in range(NTB):
                epilogue(k, yacc[:, ti, :], yacc, tb0 // P + ti, xin, xout, xtout)
        c.barrier()


def mix_consts(k):
    c, nc = k.c, k.nc

    def sb(name, shape, dt=F32):
        return Buf(nc.alloc_sbuf_tensor(name, list(shape), dt), name)

    k.lb = sb("lb", [P, DEPTH, 4])
    k.oml = sb("oml", [P, DEPTH, 4])
    k.mk_incl = sb("mk_incl", [64, 4, 64])
    k.mk_strict = sb("mk_strict", [64, 4, 64])
    k.ones256 = sb("ones256", [P, 256])
    lgt = sb("lgt", [P, DEPTH, 4])
    tmp = sb("lbtmp", [P, 4])
    with nc.allow_non_contiguous_dma(reason="tiny param transposes"):
        c.dma("sp", lgt[:], k.hg_lb_logits[:, :].rearrange("l (h d) -> d l h", h=4), lgt, k.hg_lb_logits)
    c.op("dve", lambda e: e.tensor_tensor(out=tmp[:], in0=lgt[:, 0, :], in1=lgt[:, 1, :], op=ALU.max), reads=[lgt], writes=[tmp])
    c.op("dve", lambda e: e.tensor_tensor(out=tmp[:], in0=tmp[:], in1=lgt[:, 2, :], op=ALU.max), reads=[lgt, tmp], writes=[tmp])
    c.op("dve", lambda e: e.tensor_tensor(out=tmp[:], in0=tmp[:], in1=lgt[:, 3, :], op=ALU.max), reads=[lgt, tmp], writes=[tmp])
    c.op("dve", lambda e: e.tensor_tensor(out=lgt[:], in0=lgt[:], in1=tmp[:].unsqueeze(1).to_broadcast([P, DEPTH, 4]),
                                          op=ALU.subtract), reads=[lgt, tmp], writes=[lgt])
    c.op("act", lambda e: e.activation(out=lgt[:], in_=lgt[:], func=AF.Exp), reads=[lgt], writes=[lgt])
    c.op("dve", lambda e: e.tensor_tensor(out=tmp[:], in0=lgt[:, 0, :], in1=lgt[:, 1, :], op=ALU.add), reads=[lgt], writes=[tmp])
    c.op("dve", lambda e: e.tensor_tensor(out=tmp[:], in0=tmp[:], in1=lgt[:, 2, :], op=ALU.add), reads=[lgt, tmp], writes=[tmp])
    c.op("dve", lambda e: e.tensor_tensor(out=tmp[:], in0=tmp[:], in1=lgt[:, 3, :], op=ALU.add), reads=[lgt, tmp], writes=[tmp])
    c.op("dve", lambda e: e.reciprocal(out=tmp[:], in_=tmp[:]), reads=[tmp], writes=[tmp])
    c.op("dve", lambda e: e.tensor_tensor(out=lgt[:], in0=lgt[:], in1=tmp[:].unsqueeze(1).to_broadcast([P, DEPTH, 4]),
                                          op=ALU.mult), reads=[lgt, tmp], writes=[lgt])
    c.op("dve", lambda e: e.memset(k.lb[:, 0, :], 0.0), writes=[k.lb])
    c.op("dve", lambda e: e.tensor_copy(out=k.lb[:, 1, :], in_=lgt[:, 1, :]), reads=[lgt], writes=[k.lb])
    c.op("dve", lambda e: e.tensor_tensor(out=k.lb[:, 2, :], in0=k.lb[:, 1, :], in1=lgt[:, 2, :], op=ALU.add),
         reads=[lgt, k.lb], writes=[k.lb])
    c.op("dve", lambda e: e.tensor_tensor(out=k.lb[:, 3, :], in0=k.lb[:, 2, :], in1=lgt[:, 3, :], op=ALU.add),
         reads=[lgt, k.lb], writes=[k.lb])
    c.op("dve", lambda e: e.tensor_scalar(out=k.oml[:], in0=k.lb[:], scalar1=-1.0, scalar2=1.0, op0=ALU.mult, op1=ALU.add),
         reads=[k.lb], writes=[k.oml])
    for mk, op_ in ((k.mk_incl, ALU.is_ge), (k.mk_strict, ALU.is_gt)):
        c.op("pool", lambda e: e.memset(mk[:], 1.0), writes=[mk])
        c.op("pool", lambda e: e.affine_select(out=mk[:], in_=mk[:], pattern=[[0, 4], [1, 64]], base=0,
                                               channel_multiplier=-1, compare_op=op_, fill=0.0),
             reads=[mk], writes=[mk])
    c.op("pool", lambda e: e.memset(k.ones256[:], 1.0), writes=[k.ones256])


class Sched:
    def __init__(self):
        import threading
        self.th = threading
        self.cv = threading.Condition()
        self.live = []
        self.turn = None
        self.posted = set()
        self.err = None
        self.spin = 0
        self.left = {}
        self.wts = {}

    def tick(self):
        me = self.th.get_ident()
        if me not in self.live:
            return
        left = self.left.get(me, 1) - 1
        if left > 0:
            self.left[me] = left
            return
        self.left[me] = self.wts.get(me, 1)
        with self.cv:
            if len(self.live) > 1:
                idx = self.live.index(me)
                self.turn = self.live[(idx + 1) % len(self.live)]
                self.cv.notify_all()
                while self.turn != me and self.err is None:
                    self.cv.wait()
            if self.err is not None and self.th.get_ident() != self.err[0]:
                raise RuntimeError("sibling stream failed")

    def post(self, key):
        self.posted.add(key)

    def need(self, key):
        n = 0
        while key not in self.posted:
            n += 1
            if n > 200000:
                raise RuntimeError("emission deadlock on %r" % (key,))
            self.tick()

    def run(self, fns, weights=None):
        def body(fn):
            me = self.th.get_ident()
            with self.cv:
                while self.turn != me and self.err is None:
                    self.cv.wait()
            try:
                if self.err is None:
                    fn()
            except BaseException as ex:
                with self.cv:
                    if self.err is None:
                        self.err = (me, ex)
            finally:
                with self.cv:
                    idx = self.live.index(me)
                    self.live.remove(me)
                    if self.live:
                        self.turn = self.live[idx % len(self.live)]
                    self.cv.notify_all()

        import os as _os
        if _os.environ.get("SEQ_EMIT"):
            self.need = lambda key: None
            for fn in fns:
                fn()
            return
        ths = [self.th.Thread(target=body, args=(fn,)) for fn in fns]
        with self.cv:
            for t in ths:
                t.start()
            self.live = [t.ident for t in ths]
            self.wts = {t.ident: (weights[i] if weights else 1) for i, t in enumerate(ths)}
            self.left = dict(self.wts)
            self.turn = self.live[0]
            self.cv.notify_all()
        for t in ths:
            t.join()
        if self.err is not None:
            raise self.err[1]


def phase_mix(k, l, xin, xtin, xout, xtout):
    c, nc = k.c, k.nc
    S, NSEQ = k.S, k.NSEQ
    ST = 256
    NCH = ST // 64
    sch = Sched()
    c.sched = sch
    with ExitStack() as es_:
        def sbt(name, shape, dt=F32):
            return Buf(es_.enter_context(nc.sbuf_tensor("%s_L%d" % (name, l), list(shape), dt)), name)

        def pst(name, shape, dt=F32):
            return Buf(es_.enter_context(nc.psum_tensor("%s_L%d" % (name, l), list(shape), dt)), name)

        class Pool_:
            def __init__(self, bufs):
                self.b, self.n = bufs, 0

            def next(self):
                self.n += 1
                return self.b[self.n % len(self.b)]

        win = sbt("x_win", [P, KD, IN_WIDTH], BF16)
        wout = sbt("x_wout", [P, KD, D], BF16)
        xTs = [sbt("x_xT0", [P, KD, ST], BF16), sbt("x_xT1", [P, KD, ST], BF16)]
        yTs = [sbt("x_yT0", [P, 8, ST], BF16), sbt("x_yT1", [P, 8, ST], BF16)]
        nw = sbt("x_nw", [P, 2])
        cw = sbt("x_cw", [P, 12, 4])
        dtb = sbt("x_dtb", [P, 4])
        negA = sbt("x_negA", [P, 4])
        fa, flf, fk, fcum, fd, fe = (sbt("x_fa", [P, ST]), sbt("x_flf", [P, ST]), sbt("x_fk", [P, ST]),
                                     sbt("x_fcum", [P, ST]), sbt("x_fd", [P, ST]), sbt("x_fe", [P, ST]))
        qt, kt = sbt("x_qt", [P, ST], BF16), sbt("x_kt", [P, ST], BF16)
        Vt = sbt("x_Vt", [64, NCH, 512], BF16)
        hs = sbt("x_hs", [P, 16])
        hsc = sbt("x_hsc", [P, 12])
        AT = sbt("x_AT", [64, 64], BF16)
        ktok = sbt("x_ktok", [64, P], BF16)
        Ssc = sbt("x_Ssc", [P, P], BF16)
        Sh = sbt("x_Sh", [P, 4, P])
        Sg = sbt("x_Sg", [P, 4, P])
        Sgb = sbt("x_Sgb", [P, 4, P], BF16)
        tmpS = sbt("x_tmpS", [P, P])
        osb = sbt("x_osb", [P, ST])
        rsH = (sbt("x_sqH", [P, ST]), sbt("x_rstH", [P, ST]), sbt("x_gzH", [P, ST]))
        rsG = (sbt("x_sqG", [P, ST]), sbt("x_rstG", [P, ST]), sbt("x_gzG", [P, ST]))
        sqC, rstC = sbt("x_sqC", [P, ST]), sbt("x_rstC", [P, ST])
        hist = sbt("x_hist", [P, 12, 3])
        xp = sbt("x_xp", [P, ST + 3])
        yc = sbt("x_yc", [P, ST])
        ys = sbt("x_ys", [P, ST])
        gqT = sbt("x_gqT", [P, 4, ST], BF16)
        gkT = sbt("x_gkT", [P, 4, ST], BF16)
        gvF = sbt("x_gvF", [P, 4, ST], BF16)
        og = sbt("x_og", [P, 4, ST])
        Dg = sbt("x_Dg", [64, 4, 64])
        expR = sbt("x_expR", [P, 4, 64], BF16)
        decS = sbt("x_decS", [64, 4, 64])
        decI = sbt("x_decI", [64, 4, 64])
        ABt = [sbt("x_AB0", [64, 8, 64]), sbt("x_AB1", [64, 8, 64])]
        Ab = [Buf(ABt[0].t[:, 0:4, :]), Buf(ABt[1].t[:, 0:4, :])]
        Bb = [Buf(ABt[0].t[:, 4:8, :]), Buf(ABt[1].t[:, 4:8, :])]
        Pb = [sbt("x_P0", [64, 4, 64]), sbt("x_P1", [64, 4, 64])]
        vtok = sbt("x_vtok", [64, 4, P], BF16)
        ktk = sbt("x_ktk", [64, 4, P], BF16)
        ke = sbt("x_ke", [64, 4, P], BF16)
        vnew = sbt("x_vnew", [64, P], BF16)
        pre = []
        for i in range(2):
            pre.append(dict(
                ga=sbt("x_ga%d" % i, [64, 16]), gb=sbt("x_gb%d" % i, [64, 16]), ecl=sbt("x_ecl%d" % i, [P, 8]),
                qeT=sbt("x_qeT%d" % i, [P, 4, 64], BF16), qkT=sbt("x_qkT%d" % i, [64, 4, 64], BF16),
                TT=sbt("x_TT%d" % i, [64, 4, 64], BF16), kdec=sbt("x_kdec%d" % i, [64, 4, P], BF16),
                ub=sbt("x_ub%d" % i, [64, 4, P]), wT=sbt("x_wT%d" % i, [P, 4, 64], BF16)))
        hP = Pool_([pst("x_hp0", [P, 512]), pst("x_hp1", [P, 512])])
        gP = Pool_([pst("x_gp0", [P, 512]), pst("x_gp1", [P, 512])])
        sP = Pool_([pst("x_sp0", [P, 512])])
        idf64 = k.idf[0:64, 0:64]

        load_w_bf(k, win, k.w_in[l], k.w_in)
        load_w_bf(k, wout, k.w_out[l], k.w_out)
        with nc.allow_non_contiguous_dma(reason="tiny param transposes"):
            c.dma("sp", nw[:, 0:1], k.hg_norm_w[l:l + 1, :].rearrange("o v -> v o"), nw, k.hg_norm_w)
            c.dma("sp", nw[:, 1:2], k.gd_norm_w[l:l + 1, :].rearrange("o v -> v o"), nw, k.gd_norm_w)
            for j in range(4):
                c.dma("sp", cw[:, :, j], k.gd_conv_w[l, j, :].rearrange("(b c) -> c b", c=P), cw, k.gd_conv_w)
        c.dma("sp", dtb[:], k.gd_dt_bias[l, :].partition_broadcast(P), dtb, k.gd_dt_bias)
        c.dma("sp", negA[:], k.gd_a_log[l, :].partition_broadcast(P), negA, k.gd_a_log)
        c.op("act", lambda e: e.activation(out=negA[:], in_=negA[:], func=AF.Exp), reads=[negA], writes=[negA])
        c.op("dve", lambda e: e.tensor_scalar(out=negA[:], in0=negA[:], scalar1=-1.0, scalar2=None, op0=ALU.mult),
             reads=[negA], writes=[negA])

        def sigm(dst, dst_buf, src_ap, src_buf):
            c.op("act", lambda e: e.activation(out=dst, in_=src_ap, func=AF.Exp, scale=-1.0), reads=[src_buf], writes=[dst_buf])
            c.op("act", lambda e: e.activation(out=dst, in_=dst, func=AF.Ln, bias=k.cst[0:dst.shape[0], 3:4]),
                 reads=[dst_buf, k.cst], writes=[dst_buf])
            c.op("act", lambda e: e.activation(out=dst, in_=dst, func=AF.Exp, scale=-1.0), reads=[dst_buf], writes=[dst_buf])

        def proj_F(col0, xT, pool):
            pp = pool.next()
            for kk in range(KD):
                c.op("pe", lambda e: e.matmul(pp[:, 0:ST], lhsT=win[:, kk, col0:col0 + P], rhs=xT[:, kk, :],
                                              start=(kk == 0), stop=(kk == KD - 1)), reads=[win, xT], writes=[pp])
            return pp

        def rms_gate(o_ap, o_buf, gate_col0, silu, nwcol, ydst, yT, xT, pool, rs):
            sq, rst, gz = rs
            c.op("act", lambda e: e.activation(out=sq[:], in_=o_ap, func=AF.Square), reads=[o_buf], writes=[sq])
            pm = pool.next()
            c.op("pe", lambda e: e.matmul(pm[:, 0:ST], lhsT=k.onesf[:], rhs=sq[:], start=True, stop=True),
                 reads=[k.onesf, sq], writes=[pm])
            c.op("act", lambda e: e.activation(out=rst[:], in_=pm[:, 0:ST], func=AF.Ln, bias=k.cst[:, 1:2], scale=1.0 / 128.0),
                 reads=[pm, k.cst], writes=[rst])
            c.op("act", lambda e: e.activation(out=rst[:], in_=rst[:], func=AF.Exp, scale=-0.5), reads=[rst], writes=[rst])
            pz = proj_F(gate_col0, xT, pool)
            sigm(gz[:], gz, pz[:, 0:ST], pz)
            if silu:
                c.op("dve", lambda e: e.tensor_tensor(out=gz[:], in0=pz[:, 0:ST], in1=gz[:], op=ALU.mult), reads=[pz, gz], writes=[gz])
            c.op("pool", lambda e: e.tensor_mul(out=rst[:], in0=rst[:], in1=gz[:]), reads=[rst, gz], writes=[rst])
            c.op("dve", lambda e: e.scalar_tensor_tensor(out=ydst, in0=o_ap, scalar=nw[:, nwcol:nwcol + 1], in1=rst[:],
                                                         op0=ALU.mult, op1=ALU.mult), reads=[o_buf, nw, rst], writes=[yT])

        def hgrn(xT, yT):
            for ch in range(NCH):
                pp = hP.next()
                for kk in range(KD):
                    c.op("pe", lambda e: e.matmul(pp[0:64, :], lhsT=xT[:, kk, ch * 64:(ch + 1) * 64], rhs=win[:, kk, 1024:1536],
                                                  start=(kk == 0), stop=(kk == KD - 1)), reads=[win, xT], writes=[pp])
                c.op("act", lambda e: e.activation(out=Vt[:, ch, :], in_=pp[0:64, :], func=AF.Copy), reads=[pp], writes=[Vt])
            for h in range(4):
                pf = proj_F(512 + h * P, xT, hP)
                sigm(fa[:], fa, pf[:, 0:ST], pf)
                c.op("dve", lambda e: e.tensor_scalar(out=fa[:], in0=fa[:], scalar1=k.oml[:, l, h:h + 1], scalar2=None,
                                                      op0=ALU.mult), reads=[fa, k.oml], writes=[fa])
                c.op("act", lambda e: e.activation(out=flf[:], in_=fa[:], func=AF.Ln, bias=k.lb[:, l, h:h + 1]),
                     reads=[fa, k.lb], writes=[flf])
                c.op("pool", lambda e: e.tensor_scalar(out=fk[:], in0=fa[:], scalar1=-1.0, scalar2=k.oml[:, l, h:h + 1],
                                                       op0=ALU.mult, op1=ALU.add), reads=[fa, k.oml], writes=[fk])
                c.op("dve", lambda e: e.tensor_tensor_scan(out=fcum[:], data0=k.ones256[:, 0:ST], data1=flf[:], initial=0.0,
                                                           op0=ALU.mult, op1=ALU.add), reads=[k.ones256, flf], writes=[fcum])
                cv = fcum[:].rearrange("p (c t) -> p c t", t=64)
                c.op("dve", lambda e: e.memset(hs[:, 0:1], 0.0), writes=[hs])
                c.op("dve", lambda e: e.tensor_copy(out=hs[:, 1:NCH], in_=cv[:, 0:NCH - 1, 63]), reads=[fcum], writes=[hs])
                c.op("dve", lambda e: e.tensor_tensor(out=hs[:, 4:4 + NCH], in0=cv[:, :, 31], in1=hs[:, 0:NCH], op=ALU.subtract),
                     reads=[fcum, hs], writes=[hs])
                c.op("dve", lambda e: e.tensor_tensor(out=hs[:, 8:8 + NCH], in0=cv[:, :, 63], in1=hs[:, 0:NCH], op=ALU.subtract),
                     reads=[fcum, hs], writes=[hs])
                c.op("dve", lambda e: e.tensor_tensor(out=hs[:, 12:12 + NCH], in0=cv[:, :, 63], in1=cv[:, :, 31], op=ALU.subtract),
                     reads=[fcum, hs], writes=[hs])
                c.op("act", lambda e: e.activation(out=hsc[:], in_=hs[:, 4:16], func=AF.Exp), reads=[hs], writes=[hsc])
                c.op("dve", lambda e: e.tensor_tensor(out=fd[:].rearrange("p (c t) -> p c t", t=64), in0=cv,
                                                      in1=cv[:, :, 31:32].to_broadcast([P, NCH, 64]), op=ALU.subtract),
                     reads=[fcum], writes=[fd])
                c.op("act", lambda e: e.activation(out=fe[:], in_=fd[:], func=AF.Exp), reads=[fd], writes=[fe])
                pq = proj_F(h * P, xT, hP)
                c.op("dve", lambda e: e.tensor_tensor(out=qt[:], in0=pq[:, 0:ST], in1=fe[:], op=ALU.mult),
                     reads=[pq, fe], writes=[qt])
                c.op("act", lambda e: e.activation(out=fe[:], in_=fd[:], func=AF.Exp, scale=-1.0), reads=[fd], writes=[fe])
                c.op("dve", lambda e: e.tensor_tensor(out=kt[:], in0=fk[:], in1=fe[:], op=ALU.mult),
                     reads=[fk, fe], writes=[kt])
                for ch in range(NCH):
                    cs = slice(ch * 64, (ch + 1) * 64)
                    pm = hP.next()
                    c.op("pe", lambda e: e.matmul(pm[0:64, 0:64], lhsT=kt[:, cs], rhs=qt[:, cs], start=True, stop=True),
                         reads=[kt, qt], writes=[pm])
                    c.op("dve", lambda e: e.tensor_tensor(out=AT[:], in0=pm[0:64, 0:64], in1=k.mk_incl[:, 0, :], op=ALU.mult),
                         reads=[pm, k.mk_incl], writes=[AT])
                    pt2 = hP.next()
                    c.op("pe", lambda e: e.matmul(pt2[0:64, 0:P], lhsT=kt[:, cs], rhs=k.idb[:], start=True, stop=True),
                         reads=[kt, k.idb], writes=[pt2])
                    c.op("act", lambda e: e.activation(out=ktok[:], in_=pt2[0:64, 0:P], func=AF.Copy), reads=[pt2], writes=[ktok])
                    c.op("dve", lambda e: e.tensor_scalar(out=Ssc[:], in0=Sh[:, h, :], scalar1=hsc[:, ch:ch + 1], scalar2=None,
                                                          op0=ALU.mult), reads=[Sh, hsc], writes=[Ssc])
                    po = hP.next()
                    c.op("pe", lambda e: e.matmul(po[:, 0:64], lhsT=Vt[:, ch, h * P:(h + 1) * P], rhs=AT[:], start=True, stop=False),
                         reads=[Vt, AT], writes=[po])
                    c.op("pe", lambda e: e.matmul(po[:, 0:64], lhsT=Ssc[:], rhs=qt[:, cs], start=False, stop=True),
                         reads=[Ssc, qt], writes=[po])
                    c.op("act", lambda e: e.activation(out=osb[:, cs], in_=po[:, 0:64], func=AF.Copy), reads=[po], writes=[osb])
                    pd = hP.next()
                    c.op("pe", lambda e: e.matmul(pd[:, 0:P], lhsT=ktok[:], rhs=Vt[:, ch, h * P:(h + 1) * P], start=True, stop=True),
                         reads=[ktok, Vt], writes=[pd])
                    c.op("act", lambda e: e.activation(out=tmpS[:], in_=pd[:, 0:P], func=AF.Copy, scale=hsc[:, 8 + ch:9 + ch]),
                         reads=[pd, hsc], writes=[tmpS])
                    c.op("dve", lambda e: e.scalar_tensor_tensor(out=Sh[:, h, :], in0=Sh[:, h, :], scalar=hsc[:, 4 + ch:5 + ch],
                                                                 in1=tmpS[:], op0=ALU.mult, op1=ALU.add),
                         reads=[Sh, hsc, tmpS], writes=[Sh])
                rms_gate(osb[:], osb, 1536 + h * P, False, 0, yT[:, h, :], yT, xT, hP, rsH)

        def gconv(xT, key):
            for b in range(12):
                pp = proj_F(2048 + b * P, xT, gP)
                c.op("act", lambda e: e.activation(out=xp[:, 3:3 + ST], in_=pp[:, 0:ST], func=AF.Copy), reads=[pp], writes=[xp])
                c.op("pool", lambda e: e.tensor_copy(out=xp[:, 0:3], in_=hist[:, b, :]), reads=[hist], writes=[xp])
                c.op("dve", lambda e: e.tensor_scalar(out=yc[:], in0=xp[:, 3:3 + ST], scalar1=cw[:, b, 3:4], scalar2=None,
                                                      op0=ALU.mult), reads=[xp, cw], writes=[yc])
                for j in (2, 1, 0):
                    c.op("dve", lambda e: e.scalar_tensor_tensor(out=yc[:], in0=xp[:, j:j + ST], scalar=cw[:, b, j:j + 1], in1=yc[:],
                                                                 op0=ALU.mult, op1=ALU.add), reads=[xp, cw, yc], writes=[yc])
                c.op("pool", lambda e: e.tensor_copy(out=hist[:, b, :], in_=xp[:, ST:ST + 3]), reads=[xp], writes=[hist])
                sigm(ys[:], ys, yc[:], yc)
                if b >= 8:
                    c.op("dve", lambda e: e.tensor_tensor(out=gvF[:, b - 8, :], in0=yc[:], in1=ys[:], op=ALU.mult), reads=[yc, ys], writes=[gvF])
                else:
                    dst = gqT if b < 4 else gkT
                    c.op("dve", lambda e: e.tensor_tensor(out=ys[:], in0=yc[:], in1=ys[:], op=ALU.mult), reads=[yc, ys], writes=[ys])
                    c.op("act", lambda e: e.activation(out=sqC[:], in_=ys[:], func=AF.Square), reads=[ys], writes=[sqC])
                    pm = gP.next()
                    c.op("pe", lambda e: e.matmul(pm[:, 0:ST], lhsT=k.onesf[:], rhs=sqC[:], start=True, stop=True),
                         reads=[k.onesf, sqC], writes=[pm])
                    c.op("act", lambda e: e.activation(out=rstC[:], in_=pm[:, 0:ST], func=AF.Ln, bias=k.cst[:, 2:3]),
                         reads=[pm, k.cst], writes=[rstC])
                    c.op("act", lambda e: e.activation(out=rstC[:], in_=rstC[:], func=AF.Exp, scale=-0.5), reads=[rstC], writes=[rstC])
                    sc = (128.0 ** -0.5) if b < 4 else 1.0
                    c.op("dve", lambda e: e.scalar_tensor_tensor(out=dst[:, b % 4, :], in0=ys[:], scalar=sc, in1=rstC[:],
                                                                 op0=ALU.mult, op1=ALU.mult), reads=[ys, rstC], writes=[dst])
            for ch in range(NCH):
                if ch >= 2:
                    sch.need((key, "seq", ch - 2))
                gpre(xT, ch, pre[ch % 2])
                sch.post((key, "pre", ch))

        def gpre(xT, ch, pr):
            ga, gb, ecl, qeT, qkT, TT, kdec, ub, wT = (pr["ga"], pr["gb"], pr["ecl"], pr["qeT"], pr["qkT"], pr["TT"],
                                                       pr["kdec"], pr["ub"], pr["wT"])
            cs = slice(ch * 64, (ch + 1) * 64)
            pg = gP.next()
            for kk in range(KD):
                c.op("pe", lambda e: e.matmul(pg[0:64, 0:8], lhsT=xT[:, kk, cs], rhs=win[:, kk, 3584:3592],
                                              start=(kk == 0), stop=(kk == KD - 1)), reads=[win, xT], writes=[pg])
            c.op("dve", lambda e: e.tensor_tensor(out=ga[:, 0:4], in0=pg[0:64, 0:4], in1=dtb[0:64, :], op=ALU.add),
                 reads=[pg, dtb], writes=[ga])
            c.op("act", lambda e: e.activation(out=ga[:, 4:8], in_=ga[:, 0:4], func=AF.Exp), reads=[ga], writes=[ga])
            c.op("act", lambda e: e.activation(out=ga[:, 4:8], in_=ga[:, 4:8], func=AF.Ln, bias=k.cst[0:64, 3:4]),
                 reads=[ga, k.cst], writes=[ga])
            c.op("dve", lambda e: e.tensor_tensor(out=ga[:, 8:12], in0=ga[:, 4:8], in1=negA[0:64, :], op=ALU.mult),
                 reads=[ga, negA], writes=[ga])
            sigm(ga[:, 12:16], ga, pg[0:64, 4:8], pg)
            c.op("dve", lambda e: e.tensor_scalar(out=gb[:, 0:4], in0=ga[:, 12:16], scalar1=-1.0, scalar2=None, op0=ALU.mult),
                 reads=[ga], writes=[gb])
            pc = gP.next()
            c.op("pe", lambda e: e.matmul(pc[0:64, 0:4], lhsT=k.mk_incl[:, 0, :], rhs=ga[:, 8:12], start=True, stop=True),
                 reads=[k.mk_incl, ga], writes=[pc])
            c.op("dve", lambda e: e.tensor_copy(out=gb[:, 4:8], in_=pc[0:64, 0:4]), reads=[pc], writes=[gb])
            ptt = gP.next()
            c.op("pe", lambda e: e.matmul(ptt[:, 0:4], lhsT=k.onesf[0:64, :], rhs=ga[:, 8:12], start=True, stop=True),
                 reads=[k.onesf, ga], writes=[ptt])
            c.op("dve", lambda e: e.tensor_copy(out=ecl[:, 0:4], in_=ptt[:, 0:4]), reads=[ptt], writes=[ecl])
            c.op("act", lambda e: e.activation(out=ecl[:, 4:8], in_=ecl[:, 0:4], func=AF.Exp), reads=[ecl], writes=[ecl])
            c.op("act", lambda e: e.activation(out=gb[:, 8:12], in_=gb[:, 4:8], func=AF.Exp), reads=[gb], writes=[gb])
            c.op("dve", lambda e: e.tensor_tensor(out=gb[:, 12:16], in0=ecl[0:64, 0:4], in1=gb[:, 4:8], op=ALU.subtract),
                 reads=[ecl, gb], writes=[gb])
            c.op("act", lambda e: e.activation(out=gb[:, 12:16], in_=gb[:, 12:16], func=AF.Exp), reads=[gb], writes=[gb])
            for h in range(4):
                c.op("dve", lambda e: e.tensor_scalar(out=Dg[:, h, :], in0=idf64, scalar1=gb[:, 4 + h:5 + h], scalar2=None,
                                                       op0=ALU.mult), reads=[k.idf, gb], writes=[Dg])
            pr_ = gP.next()
            c.op("pe", lambda e: e.matmul(pr_[:, 0:256], lhsT=k.onesf[0:64, :], rhs=Dg[:].rearrange("s h t -> s (h t)"),
                                          start=True, stop=True), reads=[k.onesf, Dg], writes=[pr_])
            c.op("act", lambda e: e.activation(out=expR[:].rearrange("p h t -> p (h t)"), in_=pr_[:, 0:256], func=AF.Exp),
                 reads=[pr_], writes=[expR])
            c.op("dve", lambda e: e.tensor_tensor(out=qeT[:], in0=gqT[:, :, cs], in1=expR[:], op=ALU.mult),
                 reads=[gqT, expR], writes=[qeT])
            for h in range(4):
                c.op("dve", lambda e: e.tensor_scalar(out=decS[:, h, :], in0=pr_[0:64, h * 64:(h + 1) * 64],
                                                      scalar1=gb[:, 4 + h:5 + h], scalar2=0.0, op0=ALU.subtract, op1=ALU.min),
                     reads=[pr_, gb], writes=[decS])
            c.op("act", lambda e: e.activation(out=decS[:], in_=decS[:], func=AF.Exp), reads=[decS], writes=[decS])
            c.op("pool", lambda e: e.tensor_mul(out=decI[:], in0=decS[:], in1=k.mk_incl[:]), reads=[decS, k.mk_incl], writes=[decI])
            c.op("pool", lambda e: e.tensor_mul(out=decS[:], in0=decS[:], in1=k.mk_strict[:]), reads=[decS, k.mk_strict], writes=[decS])
            pG = gP.next()
            for h in range(4):
                c.op("pe", lambda e: e.matmul(pG[0:64, h * 64:(h + 1) * 64], lhsT=gkT[:, h, cs], rhs=gkT[:, h, cs], start=True, stop=True),
                     reads=[gkT], writes=[pG])
            pQ = gP.next()
            for h in range(4):
                c.op("pe", lambda e: e.matmul(pQ[0:64, h * 64:(h + 1) * 64], lhsT=gkT[:, h, cs], rhs=gqT[:, h, cs], start=True, stop=True),
                     reads=[gkT, gqT], writes=[pQ])
            B0, A0, P0 = Bb[0], Ab[0], Pb[0]
            c.op("dve", lambda e: e.tensor_tensor(out=B0[:], in0=pG[0:64, 0:256].rearrange("s (h t) -> s h t", h=4),
                                                  in1=decS[:], op=ALU.mult),
                 reads=[pG, decS], writes=[B0])
            c.op("dve", lambda e: e.tensor_tensor(out=B0[:], in0=B0[:], in1=ga[:, 12:16].unsqueeze(2).to_broadcast([64, 4, 64]),
                                                  op=ALU.mult), reads=[B0, ga], writes=[B0])
            c.op("dve", lambda e: e.tensor_tensor(out=qkT[:].rearrange("s h t -> s (h t)"), in0=pQ[0:64, 0:256],
                                                  in1=decI[:].rearrange("s h t -> s (h t)"), op=ALU.mult),
                 reads=[pQ, decI], writes=[qkT])
            pA = gP.next()
            for h in range(4):
                c.op("pe", lambda e: e.transpose(out=pA[0:64, h * 64:(h + 1) * 64], in_=B0[:, h, :], identity=idf64),
                     reads=[B0, k.idf], writes=[pA])
            c.op("act", lambda e: e.activation(out=A0[:], in_=pA[0:64, 0:256].rearrange("s (h t) -> s h t", h=4), func=AF.Copy),
                 reads=[pA], writes=[A0])
            c.op("pool", lambda e: e.tensor_sub(out=P0[:], in0=k.mk_incl[:], in1=k.mk_strict[:]), reads=[k.mk_incl, k.mk_strict], writes=[P0])
            c.op("pool", lambda e: e.tensor_sub(out=P0[:], in0=P0[:], in1=B0[:]), reads=[P0, B0], writes=[P0])
            for lv in range(5):
                Ak, Bk, Pk = Ab[lv % 2], Bb[lv % 2], Pb[lv % 2]
                An, Bn, Pn = Ab[(lv + 1) % 2], Bb[(lv + 1) % 2], Pb[(lv + 1) % 2]
                pab = gP.next()
                for h in range(4):
                    c.op("pe", lambda e: e.matmul(pab[0:64, h * 64:(h + 1) * 64], lhsT=Bk[:, h, :], rhs=Ak[:, h, :], start=True, stop=True),
                         reads=[Ak, Bk], writes=[pab])
                c.op("act", lambda e: e.activation(out=An[:], in_=pab[0:64, 0:256].rearrange("s (h t) -> s h t", h=4), func=AF.Copy),
                     reads=[pab], writes=[An])
                if lv < 4:
                    pbb = gP.next()
                    for h in range(4):
                        c.op("pe", lambda e: e.matmul(pbb[0:64, h * 64:(h + 1) * 64], lhsT=Ak[:, h, :], rhs=Bk[:, h, :], start=True, stop=True),
                             reads=[Ak, Bk], writes=[pbb])
                    c.op("dve", lambda e: e.tensor_copy(out=Bn[:], in_=pbb[0:64, 0:256].rearrange("s (h t) -> s h t", h=4)),
                         reads=[pbb], writes=[Bn])
                pp_ = gP.next()
                for h in range(4):
                    c.op("pe", lambda e: e.matmul(pp_[0:64, h * 64:(h + 1) * 64], lhsT=An[:, h, :], rhs=Pk[:, h, :], start=True, stop=True),
                         reads=[An, Pk], writes=[pp_])
                c.op("dve", lambda e: e.tensor_tensor(out=Pn[:].rearrange("s h t -> s (h t)"), in0=pp_[0:64, 0:256],
                                                      in1=Pk[:].rearrange("s h t -> s (h t)"), op=ALU.add),
                     reads=[pp_, Pk], writes=[Pn])
            Pf = Pb[5 % 2]
            c.op("act", lambda e: e.activation(out=TT[:], in_=Pf[:], func=AF.Copy), reads=[Pf], writes=[TT])
            for src_, dst_ in ((gvF, vtok), (gkT, ktk)):
                for hp in range(2):
                    pv = gP.next()
                    for hh in range(2):
                        h = hp * 2 + hh
                        c.op("pe", lambda e: e.matmul(pv[0:64, hh * P:(hh + 1) * P], lhsT=src_[:, h, cs], rhs=k.idb[:], start=True, stop=True),
                             reads=[src_, k.idb], writes=[pv])
                    c.op("act", lambda e: e.activation(out=dst_[:, hp * 2:hp * 2 + 2, :], in_=pv[0:64, 0:256].rearrange("s (h v) -> s h v", h=2),
                                                       func=AF.Copy), reads=[pv], writes=[dst_])
            c.op("dve", lambda e: e.tensor_tensor(out=ke[:], in0=ktk[:], in1=gb[:, 8:12].unsqueeze(2).to_broadcast([64, 4, P]),
                                                  op=ALU.mult), reads=[ktk, gb], writes=[ke])
            c.op("pool", lambda e: e.tensor_tensor(out=kdec[:], in0=ktk[:], in1=gb[:, 12:16].unsqueeze(2).to_broadcast([64, 4, P]),
                                                   op=ALU.mult), reads=[ktk, gb], writes=[kdec])
            for hp in range(2):
                pu = gP.next()
                for hh in range(2):
                    h = hp * 2 + hh
                    c.op("pe", lambda e: e.matmul(pu[0:64, hh * P:(hh + 1) * P], lhsT=TT[:, h, :], rhs=vtok[:, h, :], start=True, stop=True),
                         reads=[TT, vtok], writes=[pu])
                c.op("dve", lambda e: e.tensor_tensor(out=ub[:, hp * 2:hp * 2 + 2, :],
                                                      in0=pu[0:64, 0:256].rearrange("s (h v) -> s h v", h=2),
                                                      in1=ga[:, 12 + hp * 2:14 + hp * 2].unsqueeze(2).to_broadcast([64, 2, P]),
                                                      op=ALU.mult), reads=[pu, ga], writes=[ub])
            pw = gP.next()
            for h in range(4):
                c.op("pe", lambda e: e.matmul(pw[:, h * 64:(h + 1) * 64], lhsT=ke[:, h, :], rhs=TT[:, h, :], start=True, stop=True),
                     reads=[ke, TT], writes=[pw])
            c.op("act", lambda e: e.activation(out=wT[:].rearrange("p h t -> p (h t)"), in_=pw[:, 0:256], func=AF.Copy),
                 reads=[pw], writes=[wT])

        def gseq(xT, yT, key):
            for ch in range(NCH):
                sch.need((key, "pre", ch))
                pr = pre[ch % 2]
                gb, ecl, qeT, qkT, kdec, ub, wT = pr["gb"], pr["ecl"], pr["qeT"], pr["qkT"], pr["kdec"], pr["ub"], pr["wT"]
                cs = slice(ch * 64, (ch + 1) * 64)
                for h in range(4):
                    pws = sP.next()
                    c.op("pe", lambda e: e.matmul(pws[0:64, 0:P], lhsT=wT[:, h, :], rhs=Sgb[:, h, :], start=True, stop=True),
                         reads=[wT, Sgb], writes=[pws])
                    c.op("dve", lambda e: e.scalar_tensor_tensor(out=vnew[:], in0=pws[0:64, 0:P], scalar=gb[:, h:h + 1], in1=ub[:, h, :],
                                                                 op0=ALU.mult, op1=ALU.add), reads=[pws, gb, ub], writes=[vnew])
                    c.op("pe", lambda e: e.matmul(pws[:, 0:64], lhsT=Sgb[:, h, :], rhs=qeT[:, h, :], start=True, stop=False),
                         reads=[Sgb, qeT], writes=[pws])
                    c.op("pe", lambda e: e.matmul(pws[:, 0:64], lhsT=vnew[:], rhs=qkT[:, h, :], start=False, stop=True),
                         reads=[vnew, qkT], writes=[pws])
                    c.op("act", lambda e: e.activation(out=og[:, h, cs], in_=pws[:, 0:64], func=AF.Copy), reads=[pws], writes=[og])
                    c.op("pe", lambda e: e.matmul(pws[:, 0:128], lhsT=kdec[:, h, :], rhs=vnew[:], start=True, stop=True),
                         reads=[kdec, vnew], writes=[pws])
                    c.op("dve", lambda e: e.scalar_tensor_tensor(out=Sg[:, h, :], in0=Sg[:, h, :], scalar=ecl[:, 4 + h:5 + h],
                                                                 in1=pws[:, 0:128], op0=ALU.mult, op1=ALU.add),
                         reads=[Sg, ecl, pws], writes=[Sg])
                    c.op("act", lambda e: e.activation(out=Sgb[:, h, :], in_=Sg[:, h, :], func=AF.Copy), reads=[Sg], writes=[Sgb])
                sch.post((key, "seq", ch))
            for h in range(4):
                rms_gate(og[:, h, :], og, 3592 + h * P, True, 1, yT[:, 4 + h, :], yT, xT, sP, rsG)

        def epi(yT, t0):
            for i in range(ST // P):
                pa = k.PA[0]
                for half in range(2):
                    for fb in range(8):
                        c.op("pe", lambda e: e.matmul(pa[:, half, :], lhsT=yT[:, fb, i * P:(i + 1) * P],
                                                      rhs=wout[:, fb, half * 512:(half + 1) * 512],
                                                      start=(fb == 0), stop=(fb == 7)), reads=[yT, wout], writes=[pa])
                epilogue(k, pa[:].rearrange("p a b -> p (a b)"), pa, t0 // P + i, xin, xout, xtout)

        nst_seq = S // ST
        rnd = 0
        for s in range(NSEQ):
            c.op("pool", lambda e: e.memset(Sh[:], 0.0), writes=[Sh])
            c.op("pool", lambda e: e.memset(Sg[:], 0.0), writes=[Sg])
            c.op("pool", lambda e: e.memset(Sgb[:], 0.0), writes=[Sgb])
            c.op("pool", lambda e: e.memset(hist[:], 0.0), writes=[hist])
            prev = None
            for sti in range(nst_seq):
                t0 = s * S + sti * ST
                xT, yT = xTs[rnd % 2], yTs[rnd % 2]
                load_xT(k, xT, xtin, t0, ST)
                key = rnd
                fns = [lambda xT=xT, yT=yT: hgrn(xT, yT),
                       lambda xT=xT, key=key: gconv(xT, key),
                       lambda xT=xT, yT=yT, key=key: gseq(xT, yT, key)]
                if prev is not None:
                    fns.append(lambda pv=prev: epi(pv[0], pv[1]))
                import os as _os
                _ms = _os.environ.get("MIXSTREAMS")
                if _ms:
                    fns = [f for f, tag in zip(fns, "hcs") if tag in _ms]
                sch.run(fns, weights=None if _ms else [1, 2, 1, 1][:len(fns)])
                prev = (yT, t0)
                rnd += 1
            epi(prev[0], prev[1])
        c.sched = None
        c.barrier()


_NC_CACHE = {}


def kernel(**inputs):
    n = 8
    if "nc" not in _NC_CACHE:
        _NC_CACHE["nc"] = build()
    nc = _NC_CACHE["nc"]
    x = np.ascontiguousarray(inputs["x"], dtype=np.float32)
    mem = np.ascontiguousarray(inputs["mem"], dtype=np.float32)
    in_maps = []
    for ci in range(n):
        m = {kk: np.ascontiguousarray(vv, dtype=np.float32) for kk, vv in inputs.items() if kk not in ("x", "mem")}
        m["x"] = x[2 * ci:2 * ci + 2].reshape(2 * 2048, D)
        m["mem"] = mem[2 * ci:2 * ci + 2].reshape(2 * MEM_LEN, D)
        in_maps.append(m)
    res = run_bass_kernel_spmd(nc, in_maps, core_ids=list(range(n)))
    out = np.concatenate([r["out"].reshape(2, 2048, D) for r in res.results], axis=0)
    return out.astype(np.float32)
```
